# Optimizing a Trainium2 kernel written in Bass

```python
import math
import jax, jax.numpy as jnp
from jax import lax
import numpy as np

D_MODEL = 1024
BATCH = 16
SEQ = 4096
DEPTH = 1

D_MIX = D_MODEL
D_SSM = D_MIX // 2
D_FNO = D_MIX - D_SSM
SSM_GROUP = 16
N_SSM_GROUPS = D_SSM // SSM_GROUP
SSM_STATE = 64
N_FOURIER_GROUPS = 4
FOURIER_GROUP = D_FNO // N_FOURIER_GROUPS
DT_MIN = 1e-3
DT_MAX = 1e-1
PEER_HEADS = 8
PEER_NKEYS = 128
PEER_EXPERTS = PEER_NKEYS * PEER_NKEYS
PEER_TOPK = 16
PEER_QDIM = 256
PEER_HALF = PEER_QDIM // 2
PEER_BLOCK = 128
RMS_EPS = 1e-6

kernel_name = "hybrid_s5_fnet_peer_encoder_block"


def rmsnorm(x, g):
    xf = x.astype(jnp.float32)
    y = xf * lax.rsqrt(jnp.mean(xf * xf, axis=-1, keepdims=True) + RMS_EPS)
    return (y * g.astype(jnp.float32)).astype(x.dtype)


def _ssm_combine(e_i, e_j):
    a_i, b_i = e_i
    a_j, b_j = e_j
    return a_j * a_i, a_j * b_i + b_j


def s5_scan(u, a_re, a_im, log_step, b_re, b_im, c_re, c_im, reverse):
    f32 = jnp.float32
    lam = lax.complex(a_re.astype(f32), a_im.astype(f32))
    step = jnp.exp(log_step.astype(f32))[:, None]
    lam_bar = jnp.exp(lam * step)
    b = lax.complex(b_re.astype(f32), b_im.astype(f32))
    b_bar = ((lam_bar - 1.0) / lam)[..., None] * b
    c = lax.complex(c_re.astype(f32), c_im.astype(f32))
    bu = jnp.einsum("gpc,bsgc->bsgp", b_bar, u.astype(jnp.complex64))
    a = jnp.broadcast_to(lam_bar, (1, u.shape[1]) + lam_bar.shape)
    _, states = lax.associative_scan(_ssm_combine, (a, bu), axis=1, reverse=reverse)
    return jnp.einsum("gcp,bsgp->bsgc", c, states).real


def token_mixer(h, w_in, ssm_a_re, ssm_a_im, ssm_log_step, ssm_b_re, ssm_b_im,
                ssm_c_re, ssm_c_im, ssm_d, w_glu, w_fourier, w_out):
    bsz, seq, _ = h.shape
    f32 = jnp.float32
    z = h @ w_in
    u = z[..., :D_SSM].astype(f32).reshape(bsz, seq, N_SSM_GROUPS, SSM_GROUP)
    y = (s5_scan(u, ssm_a_re[0], ssm_a_im[0], ssm_log_step[0], ssm_b_re[0], ssm_b_im[0],
                 ssm_c_re[0], ssm_c_im[0], reverse=False)
         + s5_scan(u, ssm_a_re[1], ssm_a_im[1], ssm_log_step[1], ssm_b_re[1], ssm_b_im[1],
                   ssm_c_re[1], ssm_c_im[1], reverse=True)
         + ssm_d.astype(f32) * u)
    y = jax.nn.gelu(y.reshape(bsz, seq, D_SSM), approximate=False)
    y_ssm = (y * jax.nn.sigmoid(y @ w_glu.astype(f32))).astype(h.dtype)
    f = z[..., D_SSM:].astype(f32).reshape(bsz, seq, N_FOURIER_GROUPS, FOURIER_GROUP)
    f = jnp.fft.fft2(f, axes=(1, 3), norm="ortho").real.astype(h.dtype)
    y_fno = jnp.einsum("bsgc,gcd->bsgd", f, w_fourier).reshape(bsz, seq, D_FNO)
    return jnp.concatenate([y_ssm, y_fno], axis=-1) @ w_out


def peer_ffn(h, w_query, sub_keys, expert_u, expert_v):
    bsz, seq, d = h.shape
    tokens = h.reshape(-1, PEER_BLOCK, d)

    def block(xb):
        t = xb.shape[0]
        q = (xb @ w_query).reshape(t, PEER_HEADS, 2, PEER_HALF)
        scores = jnp.einsum("thpd,hpkd->thpk", q, sub_keys).astype(jnp.float32)
        s_val, s_idx = lax.top_k(scores, PEER_TOPK)
        cand = s_val[:, :, 0, :, None] + s_val[:, :, 1, None, :]
        cand_idx = s_idx[:, :, 0, :, None] * PEER_NKEYS + s_idx[:, :, 1, None, :]
        n_cand = PEER_TOPK * PEER_TOPK
        top_val, top_pos = lax.top_k(cand.reshape(t, PEER_HEADS, n_cand), PEER_TOPK)
        expert_idx = jnp.take_along_axis(cand_idx.reshape(t, PEER_HEADS, n_cand), top_pos, axis=-1)
        gate = jax.nn.softmax(top_val, axis=-1)
        u_sel = expert_u[expert_idx]
        v_sel = expert_v[expert_idx]
        pre = jnp.einsum("thkd,td->thk", u_sel, xb).astype(jnp.float32)
        act = (jax.nn.gelu(pre, approximate=False) * gate).astype(xb.dtype)
        return jnp.einsum("thk,thkd->td", act, v_sel)

    return lax.map(block, tokens).reshape(bsz, seq, d)


def setup_inputs(seed: int = 0) -> dict:
    key = jax.random.key(seed)
    ks = jax.random.split(key, 20)
    nrm = jax.random.normal
    G, P, Cg = N_SSM_GROUPS, SSM_STATE, SSM_GROUP
    n_idx = jnp.arange(P, dtype=jnp.float32)
    x = nrm(ks[0], (BATCH, SEQ, D_MODEL), jnp.float32)
    norm1_g = 1.0 + 0.02 * nrm(ks[1], (DEPTH, D_MODEL), jnp.float32)
    w_in = nrm(ks[2], (DEPTH, D_MODEL, D_MIX), jnp.float32) * D_MODEL ** -0.5
    ssm_a_re = -0.5 + 0.01 * nrm(ks[3], (DEPTH, 2, G, P), jnp.float32)
    ssm_a_im = math.pi * n_idx + 0.01 * nrm(ks[4], (DEPTH, 2, G, P), jnp.float32)
    ssm_log_step = jax.random.uniform(ks[5], (DEPTH, 2, G), jnp.float32,
                                      math.log(DT_MIN), math.log(DT_MAX))
    b_scale = (2.0 * Cg) ** -0.5
    ssm_b_re = nrm(ks[6], (DEPTH, 2, G, P, Cg), jnp.float32) * b_scale
    ssm_b_im = nrm(ks[7], (DEPTH, 2, G, P, Cg), jnp.float32) * b_scale
    c_scale = P ** -0.5
    ssm_c_re = nrm(ks[8], (DEPTH, 2, G, Cg, P), jnp.float32) * c_scale
    ssm_c_im = nrm(ks[9], (DEPTH, 2, G, Cg, P), jnp.float32) * c_scale
    ssm_d = nrm(ks[10], (DEPTH, G, Cg), jnp.float32)
    w_glu = nrm(ks[11], (DEPTH, D_SSM, D_SSM), jnp.float32) * D_SSM ** -0.5
    w_fourier = nrm(ks[12], (DEPTH, N_FOURIER_GROUPS, FOURIER_GROUP, FOURIER_GROUP),
                    jnp.float32) * FOURIER_GROUP ** -0.5
    w_out = nrm(ks[13], (DEPTH, D_MIX, D_MODEL), jnp.float32) * D_MIX ** -0.5
    norm2_g = 1.0 + 0.02 * nrm(ks[14], (DEPTH, D_MODEL), jnp.float32)
    w_query = nrm(ks[15], (DEPTH, D_MODEL, PEER_HEADS * PEER_QDIM), jnp.float32) * D_MODEL ** -0.5
    sub_keys = nrm(ks[16], (DEPTH, PEER_HEADS, 2, PEER_NKEYS, PEER_HALF), jnp.float32) * PEER_HALF ** -0.5
    expert_u = nrm(ks[17], (DEPTH, PEER_EXPERTS, D_MODEL), jnp.float32) * D_MODEL ** -0.5
    expert_v = nrm(ks[18], (DEPTH, PEER_EXPERTS, D_MODEL), jnp.float32) * PEER_HEADS ** -0.5
    final_g = 1.0 + 0.02 * nrm(ks[19], (D_MODEL,), jnp.float32)
    return {"x": x, "norm1_g": norm1_g, "w_in": w_in, "ssm_a_re": ssm_a_re,
            "ssm_a_im": ssm_a_im, "ssm_log_step": ssm_log_step, "ssm_b_re": ssm_b_re,
            "ssm_b_im": ssm_b_im, "ssm_c_re": ssm_c_re, "ssm_c_im": ssm_c_im,
            "ssm_d": ssm_d, "w_glu": w_glu, "w_fourier": w_fourier, "w_out": w_out,
            "norm2_g": norm2_g, "w_query": w_query, "sub_keys": sub_keys,
            "expert_u": expert_u, "expert_v": expert_v, "final_g": final_g}


def reference(x, norm1_g, w_in, ssm_a_re, ssm_a_im, ssm_log_step, ssm_b_re, ssm_b_im,
              ssm_c_re, ssm_c_im, ssm_d, w_glu, w_fourier, w_out, norm2_g, w_query,
              sub_keys, expert_u, expert_v, final_g):
    for layer in range(DEPTH):
        h = rmsnorm(x, norm1_g[layer])
        x = x + token_mixer(h, w_in[layer], ssm_a_re[layer], ssm_a_im[layer],
                            ssm_log_step[layer], ssm_b_re[layer], ssm_b_im[layer],
                            ssm_c_re[layer], ssm_c_im[layer], ssm_d[layer], w_glu[layer],
                            w_fourier[layer], w_out[layer])
        h = rmsnorm(x, norm2_g[layer])
        x = x + peer_ffn(h, w_query[layer], sub_keys[layer], expert_u[layer], expert_v[layer])
    return rmsnorm(x, final_g)
```

```python
import math
from contextlib import ExitStack
import numpy as np
import ml_dtypes
import concourse.bass as bass
import concourse.mybir as mybir
from concourse.bass_utils import run_bass_kernel_spmd

F32 = mybir.dt.float32
BF16 = mybir.dt.bfloat16
U32 = mybir.dt.uint32
ALU = mybir.AluOpType
AF = mybir.ActivationFunctionType
AX = mybir.AxisListType

NB = 2
S = 4096
D = 1024
L = 16
NCH = S // L
PADW = 2 * L - 1
EPS = 1e-6
ENG = ['pe', 'act', 'dve', 'pool', 'sp']


class Prog:
    def __init__(s, nc):
        s.nc = nc
        s.e = dict(pe=nc.tensor, act=nc.scalar, dve=nc.vector, pool=nc.gpsimd, sp=nc.sync)
        s.nsem = 0
        s.new_sems()
        s.dsem = {}
        s.lastw = {}
        s.readers = {}
        s.items = {k: [] for k in ENG}

    def new_sems(s):
        if s.nsem > 0:
            return
        s.sem = {}
        for k in ENG:
            s.sem[k] = s.nc.alloc_semaphore("es%d_%s" % (s.nsem, k))
        s.nsem += 1
        s.cnt = {k: 0 for k in ENG}
        s.waited = {}

    def _wait(s, eng, tok):
        kind, name, val = tok
        h = s.sem[name] if kind == 'e' else s.dsem[name][0]
        key = (eng, h.num)
        if s.waited.get(key, 0) >= val:
            return
        s.waited[key] = val
        s.items[eng].append(('w', h, val))

    def _deps(s, eng, r, w):
        deps = []
        for k in r:
            if k in s.lastw:
                deps.append((s.lastw[k], True))
        for k in w:
            if k in s.lastw:
                deps.append((s.lastw[k], False))
            for t in s.readers.get(k, ()):
                deps.append((t, False))
        for tok, raw in deps:
            if tok[0] == 'e' and tok[1] == eng:
                if raw and s.cnt[eng] - tok[2] < 2:
                    s._wait(eng, tok)
                continue
            s._wait(eng, tok)

    def _upd(s, tok, r, w):
        for k in r:
            s.readers.setdefault(k, []).append(tok)
        for k in w:
            s.lastw[k] = tok
            s.readers[k] = []

    def op(s, eng, fn, r=(), w=()):
        s._deps(eng, r, w)
        s.cnt[eng] += 1
        s.items[eng].append(('o', fn, s.sem[eng]))
        s._upd(('e', eng, s.cnt[eng]), r, w)

    def dma(s, eng, out, in_, sem, r=(), w=()):
        s._deps(eng, r, w)
        if sem not in s.dsem:
            if getattr(s, 'free_d', None):
                s.free_d.sort(key=lambda hc: hc[1])
                s.dsem[sem] = s.free_d.pop(0)
            else:
                s.ndsem = getattr(s, 'ndsem', 0) + 1
                s.dsem[sem] = [s.nc.alloc_semaphore("ds%d" % s.ndsem), 0]
        d = s.dsem[sem]
        d[1] += 16
        s.items[eng].append(('d', out, in_, d[0]))
        s._upd(('d', sem, d[1]), r, w)

    def barrier(s, final=False):
        toks = [('e', k, s.cnt[k]) for k in ENG if s.cnt[k] > 0]
        toks += [('d', n, d[1]) for n, d in s.dsem.items() if d[1] > 0]
        for eng in (['sp'] if final else ENG):
            for tok in toks:
                if tok[0] == 'e' and tok[1] == eng:
                    continue
                s._wait(eng, tok)
        s.lastw.clear()
        s.readers.clear()
        s.flush()
        if not hasattr(s, 'free_d'):
            s.free_d = []
        s.free_d.extend(s.dsem.values())
        s.dsem = {}

    def flush(s):
        def replay(items, embed=False):
            def f(e):
                pend = []
                for it in items:
                    if it[0] == 'w':
                        if embed:
                            pend.append(it)
                        else:
                            e.wait_ge(it[1], it[2])
                    elif it[0] == 'o':
                        for p in pend[:-1]:
                            e.wait_ge(p[1], p[2])
                        ins = it[1](e)
                        if pend:
                            ins._wait_ge(pend[-1][1], pend[-1][2])
                        pend = []
                        ins.then_inc(it[2], 1)
                    else:
                        e.dma_start(out=it[1], in_=it[2]).then_inc(it[3], 16)
            return f
        with s.nc.Block() as block:
            for k, dec in (('pe', block.tensor), ('act', block.scalar), ('dve', block.vector), ('pool', block.gpsimd), ('sp', block.sync)):
                if s.items[k]:
                    dec(replay(s.items[k], embed=False))
        s.items = {k: [] for k in ENG}


def AP(t, off, dims):
    return bass.AP(t, off, [list(d) for d in dims])


def build(debug=(), nblk_lim=None, stage=99, upto=99, quick=False):
    nc = bass.Bass("TRN2", target_bir_lowering=False)
    P = Prog(nc)

    def din(name, shape, dt=F32):
        return nc.dram_tensor(name, list(shape), dt, kind="ExternalInput").ap()

    dbg = {}

    def dscr(name, shape, dt=BF16):
        kind = "ExternalOutput" if name in debug else "Internal"
        a = nc.dram_tensor(name, list(shape), dt, kind=kind).ap()
        return a

    x = din("x", [NB, S, D])
    w1 = din("w1", [128, 8, D])
    g1 = din("g1", [128, 8])
    woutw = din("wout", [128, 8, D])
    wglu = din("wglu", [128, 4, 512])
    wq = din("wq", [128, 8, 2048])
    g2 = din("g2", [128, 8])
    gfin = din("gfin", [128, D])
    wf = din("wf", [128, 4, 128])
    ccsc = din("ccsc", [128, 2, 128])
    ident_in = din("ident", [128, 128])
    tab = din("tab", [16, 128, 2, 32, 256], BF16)
    y = nc.dram_tensor("y", [NB, S, D], F32, kind="ExternalOutput").ap()

    zS_d = dscr("zS_d", [NB, 4, 128, S])
    A_d = dscr("A_d", [NB, 32, 128, 1024])
    yF_d = dscr("yF_d", [NB, 4, 128, S])
    yG_d = dscr("yG_d", [NB, 4, 128, S])
    x1_d = dscr("x1_d", [NB, S, D], F32)
    h2T_d = dscr("h2T_d", [NB, 128, 8, S])

    def dump(name, tile, shape, dt, key):
        if name in debug:
            d = nc.dram_tensor(name, list(shape), dt, kind="ExternalOutput").ap()
            P.dma('sp', d, tile, 'dbg_' + name, r=[key], w=['dbg_' + name])

    pst = ExitStack()

    def SBp(name, shape, dt):
        return pst.enter_context(nc.sbuf_tensor(name, list(shape), dt))

    identb = SBp("identb", [128, 128], BF16)
    identf = SBp("identf", [128, 128], F32)
    gfin_s = SBp("gfin_s", [128, D], F32)
    epsc = SBp("epsc", [128, 1], F32)

    P.dma('sp', identf[:], ident_in[:, :], 'c_identf', w=['identf'])
    P.dma('sp', gfin_s[:], gfin[:, :], 'c_gfin', w=['gfin'])
    P.op('dve', lambda e: e.tensor_copy(out=identb[:], in_=identf[:]), r=['identf'], w=['identb'])
    P.op('dve', lambda e: e.memset(epsc[:], EPS), w=['epsc'])

    uT_in = din("uT_in", [128, 8, 16384])
    v_in = din("v_in", [128, 128, 1024])
    UTb_d = dscr("UTb_d", [128, 128, 8, 128])
    Vb_d = dscr("Vb_d", [128, 128, 1024])
    g2s = SBp("g2s", [128, 8], F32)
    P.dma('sp', g2s[:], g2[:, :], 'c_g2s', w=['g2s'])
    e0st = ExitStack()
    uf0 = e0st.enter_context(nc.sbuf_tensor("uf0", [128, 8, 512], F32))
    ub0 = e0st.enter_context(nc.sbuf_tensor("ub0", [128, 8, 512], BF16))
    vf0 = e0st.enter_context(nc.sbuf_tensor("vf0", [128, 4, 1024], F32))
    vb0 = e0st.enter_context(nc.sbuf_tensor("vb0", [128, 4, 1024], BF16))

    def e0_iter(it):
        P.dma('pool', uf0[:], uT_in[:, :, it * 512:(it + 1) * 512], 'uf0', w=['uf0'])
        for kc in range(8):
            P.op('dve' if kc % 2 else 'pool', lambda e, kc=kc: e.tensor_scalar(out=ub0[:, kc, :], in0=uf0[:, kc, :], scalar1=g2s[:, kc:kc + 1], scalar2=None, op0=ALU.mult),
                 r=['uf0', 'g2s'], w=['ub0_%d' % kc] + ['ubo0_%d' % i4 for i4 in range(4)])
        for i4 in range(4):
            P.dma('pool', UTb_d[it * 4 + i4], AP(ub0, i4 * 128, [[4096, 128], [512, 8], [1, 128]]),
                  'ubo0', r=['ub0_%d' % kc for kc in range(8)], w=['ubo0_%d' % i4])
        P.dma('pool', vf0[:], v_in[it * 4:(it + 1) * 4].rearrange("i p d -> p i d"), 'vf0', w=['vf0'])
        P.op('pool', lambda e: e.tensor_copy(out=vb0[:], in_=vf0[:]), r=['vf0'], w=['vb0'])
        P.dma('pool', Vb_d[it * 4:(it + 1) * 4].rearrange("i p d -> p i d"), vb0[:], 'vbo0', r=['vb0'], w=['vb0'])

    with ExitStack() as st:
        def SB(name, shape, dt):
            return st.enter_context(nc.sbuf_tensor(name, list(shape), dt))

        def PS(name, shape, dt=F32):
            return st.enter_context(nc.psum_tensor(name, list(shape), dt))

        w1f = SB("w1f", [128, 8, D], F32)
        w1b = SB("w1b", [128, 8, D], BF16)
        g1s = SB("g1s", [128, 8], F32)
        wfs = SB("wfs", [128, 4, 128], F32)
        ccs = SB("ccs", [128, 2, 128], F32)
        csw = SB("csw", [128, 4, 256], BF16)
        wfb = SB("wfb", [128, 4, 128], BF16)
        ccb = SB("ccb", [128, 2, 128], BF16)
        xs = [SB("xs%d" % i, [128, D], F32) for i in range(2)]
        sq = SB("sq", [128, D], F32)
        ss = [SB("ss%d" % i, [128, 4], F32) for i in range(2)]
        xn = [SB("xn%d" % i, [128, D], BF16) for i in range(2)]
        hT = [SB("hT%d" % i, [128, 8, 128], BF16) for i in range(2)]
        zT = [SB("zT%d" % i, [128, 8, 128], BF16) for i in range(2)]
        Ab = [SB("Ab%d" % i, [128, 1024], BF16) for i in range(2)]
        pT = [PS("pT%d" % i, [128, 8, 128], BF16) for i in range(2)]
        pz = [PS("pz%d" % i, [128, 8, 128], F32) for i in range(1)]
        pA = [PS("pA%d" % i, [128, 1024], F32) for i in range(1)]
        pw = PS("pw", [128, 512], F32)

        P.dma('sp', w1f[:], w1[:, :, :], 'c_w1f', w=['w1f'])
        P.dma('sp', g1s[:], g1[:, :], 'c_g1s', w=['g1s'])
        P.dma('sp', wfs[:], wf[:, :, :], 'c_wfs', w=['wfs'])
        P.dma('sp', ccs[:], ccsc[:, :, :], 'c_ccs', w=['ccs'])
        for kc in range(8):
            P.op('dve', lambda e, kc=kc: e.tensor_scalar(out=w1b[:, kc, :], in0=w1f[:, kc, :], scalar1=g1s[:, kc:kc + 1],
                                                        scalar2=None, op0=ALU.mult), r=['w1f', 'g1s'], w=['w1b'])
        P.op('dve', lambda e: e.tensor_copy(out=wfb[:], in_=wfs[:]), r=['wfs'], w=['wfb'])
        P.op('dve', lambda e: e.tensor_copy(out=ccb[:], in_=ccs[:]), r=['ccs'], w=['ccb'])
        for g in range(4 if stage >= 0 else 0):
            for t in range(2):
                P.op('pe', lambda e, g=g, t=t: e.matmul(pw[:, (g % 2) * 256 + t * 128:(g % 2) * 256 + t * 128 + 128],
                                                        lhsT=ccb[:, t, :], rhs=wfb[:, g, :], start=True, stop=True),
                     r=['ccb', 'wfb'], w=['pw'])
            P.op('dve', lambda e, g=g: e.tensor_copy(out=csw[:, g, :], in_=pw[:, (g % 2) * 256:(g % 2) * 256 + 256]),
                 r=['pw'], w=['csw'])

        nblk = NB * (S // 128) if nblk_lim is None else nblk_lim
        if stage == -2:
            nblk = 0
        for blk in range(nblk):
            b, tb = divmod(blk, S // 128)
            i = blk % 2
            t0 = tb * 128
            if blk % 2 == 0:
                e0_iter(blk // 2)
            P.dma('sp', xs[i][:], x[b, t0:t0 + 128, :], 'xs%d' % i, w=['xs%d' % i])
            P.op('act', lambda e, i=i: e.activation(out=sq[:], in_=xs[i][:], func=AF.Square, accum_out=ss[i][:, 0:1]),
                 r=['xs%d' % i], w=['sq', 'ssa%d' % i])
            P.op('act', lambda e, i=i: e.activation(out=ss[i][:, 1:2], in_=ss[i][:, 0:1], func=AF.Sqrt, scale=1.0 / D, bias=epsc[:, 0:1]),
                 r=['ssa%d' % i, 'epsc'], w=['ssb%d' % i])
            P.op('dve', lambda e, i=i: e.reciprocal(out=ss[i][:, 2:3], in_=ss[i][:, 1:2]), r=['ssb%d' % i], w=['ssc%d' % i])
            P.op('dve', lambda e, i=i: e.tensor_scalar(out=xn[i][:], in0=xs[i][:], scalar1=ss[i][:, 2:3], scalar2=None, op0=ALU.mult),
                 r=['xs%d' % i, 'ssc%d' % i], w=['xn%d' % i])

            if stage < 1:
                continue
            def tr(e, i=i):
                for kc in range(8):
                    ins = e.transpose(out=pT[i][:, kc, :], in_=xn[i][:, kc * 128:(kc + 1) * 128], identity=identb[:])
                return ins
            P.op('pe', tr, r=['xn%d' % i, 'identb'], w=['pT%d' % i])
            P.op('act', lambda e, i=i: e.copy(out=hT[i][:], in_=pT[i][:]), r=['pT%d' % i], w=['hT%d' % i])

            if stage < 2:
                continue
            def zmm(e, i=i):
                for n in range(8):
                    for kc in range(8):
                        ins = e.matmul(pz[0][:, n, :], lhsT=w1b[:, kc, n * 128:(n + 1) * 128], rhs=hT[i][:, kc, :],
                                       start=(kc == 0), stop=(kc == 7))
                return ins
            P.op('pe', zmm, r=['hT%d' % i, 'w1b'], w=['pz'])
            P.op('dve', lambda e, i=i: e.tensor_copy(out=zT[i][:], in_=pz[0][:]), r=['pz'], w=['zT%d' % i])
            if blk == 0:
                dump('d_xn', xn[i][:], [128, D], BF16, 'xn%d' % i)
                dump('d_hT', hT[i][:], [128, 8, 128], BF16, 'hT%d' % i)
                dump('d_zT', zT[i][:], [128, 8, 128], BF16, 'zT%d' % i)
                dump('d_w1b', w1b[:], [128, 8, D], BF16, 'w1b')
            P.dma('sp', zS_d[b, :, :, t0:t0 + 128].rearrange("t p s -> p t s"), zT[i][:, 0:4, :], 'zso%d' % i, r=['zT%d' % i], w=['zso%d' % i])

            if stage < 3:
                continue
            def amm(e, i=i):
                for g in range(4):
                    ins = e.matmul(pA[0][:, g * 256:(g + 1) * 256], lhsT=zT[i][:, 4 + g, :], rhs=csw[:, g, :], start=True, stop=True)
                return ins
            P.op('pe', amm, r=['zT%d' % i, 'csw'], w=['pA'])
            P.op('act', lambda e, i=i: e.copy(out=Ab[i][:], in_=pA[0][:]), r=['pA'], w=['Ab%d' % i])
            P.dma('sp', A_d[b, tb, :, :], Ab[i][:], 'ao%d' % i, r=['Ab%d' % i], w=['ao%d' % i])
        P.barrier()
    e0st.close()
    P.new_sems()
    if upto < 2:
        return nc, P, {}

    with ExitStack() as st:
        def SB(name, shape, dt):
            return st.enter_context(nc.sbuf_tensor(name, list(shape), dt))

        def PS(name, shape, dt=F32):
            return st.enter_context(nc.psum_tensor(name, list(shape), dt))
        Asb = SB("Asb", [128, 32, 1024], BF16)
        tbs = [SB("tb%d" % i, [128, 2, 32, 256], BF16) for i in range(2)]
        yst = [SB("yst%d" % i, [128, 4, 256], BF16) for i in range(2)]
        pf = [PS("pf%d" % i, [128, 512], F32) for i in range(4)]
        for b in range(NB):
            P.dma('sp', Asb[:], A_d[b].rearrange("s p n -> p s n"), 'Asb', w=['Asb'])
            for kt in range(16 if not quick else 1):
                it = (b * 16 + kt) % 2
                P.dma('sp', tbs[it][:], tab[kt], 'tb%d' % it, w=['tb%d' % it])
                for g in range(4):
                    def fmm(e, g=g, it=it):
                        n = 0
                        for cs in range(2):
                            for sc in range(32):
                                ins = e.matmul(pf[g][:, 0:256], lhsT=Asb[:, sc, g * 256 + cs * 128:g * 256 + cs * 128 + 128],
                                               rhs=tbs[it][:, cs, sc, :], start=(n == 0), stop=(n == 63))
                                n += 1
                        return ins
                    P.op('pe', fmm, r=['Asb', 'tb%d' % it], w=['pf%d' % g])
                    if g % 2:
                        P.op('act', lambda e, g=g, it=it: e.copy(out=yst[it][:, g, :], in_=pf[g][:, 0:256]), r=['pf%d' % g], w=['yst%d_%d' % (it, g)])
                    else:
                        P.op('dve', lambda e, g=g, it=it: e.tensor_copy(out=yst[it][:, g, :], in_=pf[g][:, 0:256]), r=['pf%d' % g], w=['yst%d_%d' % (it, g)])
                P.dma('sp', yF_d[b, :, :, kt * 256:(kt + 1) * 256].rearrange("g p s -> p g s"), yst[it][:], 'yfo%d' % it,
                      r=['yst%d_%d' % (it, g) for g in range(4)], w=['yst%d_%d' % (it, g) for g in range(4)])
        P.barrier()
    P.new_sems()
    if upto < 3:
        return nc, P, {}

    lam_are = din("lam_are", [128, 32]); lam_aim = din("lam_aim", [128, 32]); lam_lst = din("lam_lst", [128, 32])
    bA = din("bA", [2, 4, 128, 8, 128]); cA = din("cA", [2, 4, 128, 8, 128]); dD = din("dD", [128, 4])
    WLAG_d = dscr("WLAG_d", [4, 128, 31, 128])
    WIN_d = dscr("WIN_d", [4, 16, 128, 16, 128])
    WOUT_d = dscr("WOUT_d", [4, 16, 128, 16, 128])
    prt = SBp("prt", [128, 17, 32], F32)
    pit = SBp("pit", [128, 17, 32], F32)
    with ExitStack() as st:
        def SB(name, shape, dt):
            return st.enter_context(nc.sbuf_tensor(name, list(shape), dt))

        def PS(name, shape, dt=F32):
            return st.enter_context(nc.psum_tensor(name, list(shape), dt))
        names = ['are', 'aim', 'lst', 'stp', 'ar', 'ai', 'mag', 's16', 'cc', 'sn', 't1', 't2', 't3', 'lr', 'li', 'den', 'nr', 'cr', 'ci']
        T_ = {n: SB("l_" + n, [128, 32], F32) for n in names}
        P.dma('sp', T_['are'][:], lam_are[:, :], 'c_are', w=['are'])
        P.dma('sp', T_['aim'][:], lam_aim[:, :], 'c_aim', w=['aim'])
        P.dma('sp', T_['lst'][:], lam_lst[:, :], 'c_lst', w=['lst'])
        dDs = SB("dDs", [128, 4], F32)
        P.dma('sp', dDs[:], dD[:, :], 'c_dD', w=['dD'])

        def tt(o, a, b_, op, eng='dve'):
            P.op(eng, lambda e: e.tensor_tensor(out=T_[o][:], in0=T_[a][:], in1=T_[b_][:], op=op), r=[a, b_], w=[o])

        def ts(o, a, s1, s2, op0, op1=None):
            if op1 is None:
                P.op('dve', lambda e: e.tensor_scalar(out=T_[o][:], in0=T_[a][:], scalar1=s1, scalar2=None, op0=op0), r=[a], w=[o])
            else:
                P.op('dve', lambda e: e.tensor_scalar(out=T_[o][:], in0=T_[a][:], scalar1=s1, scalar2=s2, op0=op0, op1=op1), r=[a], w=[o])

        def act(o, a, func, scale=1.0):
            P.op('act', lambda e: e.activation(out=T_[o][:], in_=T_[a][:], func=func, scale=scale), r=[a], w=[o])
        act('stp', 'lst', AF.Exp)
        tt('ar', 'are', 'stp', ALU.mult)
        tt('ai', 'aim', 'stp', ALU.mult)
        act('mag', 'ar', AF.Exp)
        act('s16', 'ai', AF.Sin, 1.0 / 16)
        act('sn', 'ai', AF.Sin, 1.0 / 8)
        tt('t1', 's16', 's16', ALU.mult)
        ts('cc', 't1', -2.0, 1.0, ALU.mult, ALU.add)
        for _ in range(3):
            tt('t1', 'cc', 'cc', ALU.mult)
            tt('t2', 'sn', 'sn', ALU.mult)
            tt('t3', 'cc', 'sn', ALU.mult)
            tt('cc', 't1', 't2', ALU.subtract)
            ts('sn', 't3', 2.0, None, ALU.mult)
        tt('lr', 'mag', 'cc', ALU.mult)
        tt('li', 'mag', 'sn', ALU.mult)
        tt('t1', 'are', 'are', ALU.mult)
        tt('t2', 'aim', 'aim', ALU.mult)
        tt('den', 't1', 't2', ALU.add)
        P.op('dve', lambda e: e.reciprocal(out=T_['den'][:], in_=T_['den'][:]), r=['den'], w=['den'])
        ts('nr', 'lr', -1.0, None, ALU.add)
        tt('t1', 'nr', 'are', ALU.mult)
        tt('t2', 'li', 'aim', ALU.mult)
        tt('t3', 't1', 't2', ALU.add)
        tt('cr', 't3', 'den', ALU.mult)
        tt('t1', 'li', 'are', ALU.mult)
        tt('t2', 'nr', 'aim', ALU.mult)
        tt('t3', 't1', 't2', ALU.subtract)
        tt('ci', 't3', 'den', ALU.mult)
        P.op('dve', lambda e: e.memset(prt[:, 0, :], 1.0), w=['prt'])
        P.op('dve', lambda e: e.memset(pit[:, 0, :], 0.0), w=['pit'])
        for k in range(16):
            P.op('dve', lambda e, k=k: e.tensor_tensor(out=T_['t1'][:], in0=prt[:, k, :], in1=T_['lr'][:], op=ALU.mult), r=['prt', 'lr'], w=['t1'])
            P.op('dve', lambda e, k=k: e.tensor_tensor(out=T_['t2'][:], in0=pit[:, k, :], in1=T_['li'][:], op=ALU.mult), r=['pit', 'li'], w=['t2'])
            P.op('dve', lambda e, k=k: e.tensor_tensor(out=T_['t3'][:], in0=prt[:, k, :], in1=T_['li'][:], op=ALU.mult), r=['prt', 'li'], w=['t3'])
            P.op('dve', lambda e, k=k: e.tensor_tensor(out=T_['nr'][:], in0=pit[:, k, :], in1=T_['lr'][:], op=ALU.mult), r=['pit', 'lr'], w=['nr'])
            P.op('dve', lambda e, k=k: e.tensor_tensor(out=prt[:, k + 1, :], in0=T_['t1'][:], in1=T_['t2'][:], op=ALU.subtract), r=['t1', 't2'], w=['prt'])
            P.op('dve', lambda e, k=k: e.tensor_tensor(out=pit[:, k + 1, :], in0=T_['t3'][:], in1=T_['nr'][:], op=ALU.add), r=['t3', 'nr'], w=['pit'])

        bre = SB("bre", [128, 8, 128], F32); bim = SB("bim", [128, 8, 128], F32)
        cre = SB("cre", [128, 8, 128], F32); cim = SB("cim", [128, 8, 128], F32)
        Br = SB("Br", [128, 8, 128], F32); Bi = SB("Bi", [128, 8, 128], F32)
        u1 = SB("u1", [128, 8, 128], F32); u2 = SB("u2", [128, 8, 128], F32)
        v1 = SB("v1", [128, 8, 128], F32); v2 = SB("v2", [128, 8, 128], F32)
        Cxr = SB("Cxr", [128, 8, 128], BF16); Cxi = SB("Cxi", [128, 8, 128], BF16)
        Pkr = [SB("Pkr%d" % i, [128, 8, 128], BF16) for i in range(2)]
        Pki = [SB("Pki%d" % i, [128, 8, 128], BF16) for i in range(2)]
        CLr = [SB("CLr%d" % i, [128, 8, 128], BF16) for i in range(2)]
        CLi = [SB("CLi%d" % i, [128, 8, 128], BF16) for i in range(2)]
        winst = [SB("winst%d" % i, [128, 16, 128], BF16) for i in range(2)]
        wlags = SB("wlags", [128, 31, 128], BF16)
        ddiag = SB("ddiag", [128, 128], F32)
        ptk = [PS("ptk%d" % i, [128, 512], F32) for i in range(2)]
        ptr = [PS("ptr%d" % i, [128, 8, 128], BF16) for i in range(2)]

        def bc(tile, off, pstep):
            return AP(tile, off, [[pstep, 128], [1, 8], [0, 128]])
        for T in range(4):
            P.dma('sp', bre[:], bA[0, T], 'c_bre', w=['bre']); P.dma('sp', bim[:], bA[1, T], 'c_bim', w=['bim'])
            P.dma('sp', cre[:], cA[0, T], 'c_cre', w=['cre']); P.dma('sp', cim[:], cA[1, T], 'c_cim', w=['cim'])
            crb = bc(T_['cr'], T * 8, 32); cib = bc(T_['ci'], T * 8, 32)
            P.op('dve', lambda e, crb=crb: e.tensor_tensor(out=u1[:], in0=bre[:], in1=crb, op=ALU.mult), r=['bre', 'cr'], w=['u1'])
            P.op('dve', lambda e, cib=cib: e.tensor_tensor(out=u2[:], in0=bim[:], in1=cib, op=ALU.mult), r=['bim', 'ci'], w=['u2'])
            P.op('dve', lambda e: e.tensor_tensor(out=Br[:], in0=u1[:], in1=u2[:], op=ALU.subtract), r=['u1', 'u2'], w=['Br'])
            P.op('dve', lambda e, crb=crb: e.tensor_tensor(out=u1[:], in0=bim[:], in1=crb, op=ALU.mult), r=['bim', 'cr'], w=['u1'])
            P.op('dve', lambda e, cib=cib: e.tensor_tensor(out=u2[:], in0=bre[:], in1=cib, op=ALU.mult), r=['bre', 'ci'], w=['u2'])
            P.op('dve', lambda e: e.tensor_tensor(out=Bi[:], in0=u1[:], in1=u2[:], op=ALU.add), r=['u1', 'u2'], w=['Bi'])
            P.op('pool', lambda e: e.tensor_copy(out=Cxr[:], in_=cre[:]), r=['cre'], w=['Cxr'])
            P.op('pool', lambda e: e.tensor_scalar(out=Cxi[:], in0=cim[:], scalar1=-1.0, scalar2=None, op0=ALU.mult), r=['cim'], w=['Cxi'])
            P.op('dve', lambda e, T=T: e.tensor_scalar(out=ddiag[:], in0=identf[:], scalar1=dDs[:, T:T + 1], scalar2=None, op0=ALU.mult),
                 r=['identf', 'dD'], w=['ddiag'])
            for k in range(17):
                i2 = k % 2
                pkr = bc(prt, k * 32 + T * 8, 17 * 32); pki = bc(pit, k * 32 + T * 8, 17 * 32)
                if k <= 15:
                    P.op('dve', lambda e, pkr=pkr: e.tensor_tensor(out=u1[:], in0=Br[:], in1=pkr, op=ALU.mult), r=['Br', 'prt'], w=['u1'])
                    P.op('dve', lambda e, pki=pki: e.tensor_tensor(out=u2[:], in0=Bi[:], in1=pki, op=ALU.mult), r=['Bi', 'pit'], w=['u2'])
                    P.op('dve', lambda e, i2=i2: e.tensor_tensor(out=Pkr[i2][:], in0=u1[:], in1=u2[:], op=ALU.subtract), r=['u1', 'u2'], w=['Pkr%d' % i2])
                    P.op('dve', lambda e, pki=pki: e.tensor_tensor(out=u1[:], in0=Br[:], in1=pki, op=ALU.mult), r=['Br', 'pit'], w=['u1'])
                    P.op('dve', lambda e, pkr=pkr: e.tensor_tensor(out=u2[:], in0=Bi[:], in1=pkr, op=ALU.mult), r=['Bi', 'prt'], w=['u2'])
                    P.op('dve', lambda e, i2=i2: e.tensor_tensor(out=Pki[i2][:], in0=u1[:], in1=u2[:], op=ALU.add), r=['u1', 'u2'], w=['Pki%d' % i2])
                    if k == 0:
                        def tk0(e, i2=i2):
                            n = 0
                            for dr in range(2):
                                for q in range(4):
                                    for (Pt, Ct) in ((Pkr[i2], Cxr), (Pki[i2], Cxi)):
                                        ins = e.matmul(ptk[0][:, 0:128], lhsT=Pt[:, q * 2 + dr, :], rhs=Ct[:, q * 2 + dr, :], start=(n == 0), stop=(n == 15))
                                        n += 1
                            return ins
                        P.op('pe', tk0, r=['Pkr%d' % i2, 'Pki%d' % i2, 'Cxr', 'Cxi'], w=['ptk0'])
                        P.op('dve', lambda e: e.tensor_tensor(out=wlags[:, 15, :], in0=ptk[0][:, 0:128], in1=ddiag[:], op=ALU.add),
                             r=['ptk0', 'ddiag'], w=['wlags'])
                    else:
                        for dr in range(2):
                            def tk(e, i2=i2, dr=dr):
                                n = 0
                                for q in range(4):
                                    for (Pt, Ct) in ((Pkr[i2], Cxr), (Pki[i2], Cxi)):
                                        ins = e.matmul(ptk[dr][:, 0:128], lhsT=Pt[:, q * 2 + dr, :], rhs=Ct[:, q * 2 + dr, :], start=(n == 0), stop=(n == 7))
                                        n += 1
                                return ins
                            P.op('pe', tk, r=['Pkr%d' % i2, 'Pki%d' % i2, 'Cxr', 'Cxi'], w=['ptk%d' % dr])
                            li_ = 15 + k if dr == 0 else 15 - k
                            P.op('act', lambda e, dr=dr, li_=li_: e.copy(out=wlags[:, li_, :], in_=ptk[dr][:, 0:128]), r=['ptk%d' % dr], w=['wlags'])
                    for h in range(2):
                        def trw(e, i2=i2, h=h):
                            for m in range(8):
                                idx = h * 8 + m
                                cb, ri = idx // 2, idx % 2
                                ins = e.transpose(out=ptr[h][:, m, :], in_=(Pkr[i2] if ri == 0 else Pki[i2])[:, cb, :], identity=identb[:])
                            return ins
                        P.op('pe', trw, r=['Pkr%d' % i2, 'Pki%d' % i2, 'identb'], w=['ptr%d' % h])
                        P.op('act', lambda e, i2=i2, h=h: e.copy(out=winst[i2][:, h * 8:(h + 1) * 8, :], in_=ptr[h][:]), r=['ptr%d' % h], w=['winst%d' % i2])
                    P.dma('sp', WIN_d[T, k], winst[i2][:], 'wino%d' % i2, r=['winst%d' % i2], w=['winst%d' % i2])
                if k >= 1:
                    P.op('dve', lambda e, pkr=pkr: e.tensor_tensor(out=u1[:], in0=cre[:], in1=pkr, op=ALU.mult), r=['cre', 'prt'], w=['u1'])
                    P.op('dve', lambda e, pki=pki: e.tensor_tensor(out=u2[:], in0=cim[:], in1=pki, op=ALU.mult), r=['cim', 'pit'], w=['u2'])
                    P.op('dve', lambda e, i2=i2: e.tensor_tensor(out=CLr[i2][:], in0=u1[:], in1=u2[:], op=ALU.subtract), r=['u1', 'u2'], w=['CLr%d' % i2])
                    P.op('pool', lambda e, pki=pki: e.tensor_tensor(out=v1[:], in0=cre[:], in1=pki, op=ALU.mult), r=['cre', 'pit'], w=['v1'])
                    P.op('pool', lambda e, pkr=pkr: e.tensor_tensor(out=v2[:], in0=cim[:], in1=pkr, op=ALU.mult), r=['cim', 'prt'], w=['v2'])
                    P.op('pool', lambda e: e.tensor_tensor(out=v1[:], in0=v1[:], in1=v2[:], op=ALU.add), r=['v1', 'v2'], w=['v1'])
                    P.op('pool', lambda e, i2=i2: e.tensor_scalar(out=CLi[i2][:], in0=v1[:], scalar1=-1.0, scalar2=None, op0=ALU.mult), r=['v1'], w=['CLi%d' % i2])
                    wd = WOUT_d[T, k - 1].rearrange("p (c r) m -> p c r m", r=2)
                    P.dma('sp', wd[:, :, 0, :], CLr[i2][:], 'wouto%d' % i2, r=['CLr%d' % i2], w=['CLr%d' % i2])
                    P.dma('sp', wd[:, :, 1, :], CLi[i2][:], 'woutp%d' % i2, r=['CLi%d' % i2], w=['CLi%d' % i2])
            P.dma('sp', WLAG_d[T], wlags[:], 'wlago', r=['wlags'], w=['wlags'])
        P.barrier()
    P.new_sems()
    if upto < 4:
        return nc, P, {}
    UPW = 7968
    for b in range(NB):
        for T in range(4):
            if quick and (b > 0 or T > 0):
                continue
            with ExitStack() as st:
                def SB(name, shape, dt, b=b, T=T):
                    return st.enter_context(nc.sbuf_tensor("%s_%d_%d" % (name, b, T), list(shape), dt))

                def PS(name, shape, dt=F32, b=b, T=T):
                    return st.enter_context(nc.psum_tensor("%s_%d_%d" % (name, b, T), list(shape), dt))
                zs = SB("zs", [128, S], BF16)
                upad = SB("upad", [128, UPW], BF16)
                wlag = SB("wlag", [128, 31, 128], BF16)
                Hs = [SB("Hs%d" % i, [128, 257, 16], F32) for i in range(2)]
                Xh = SB("Xh", [128, 4, 2, 2, 256], BF16)
                Ssb = SB("Ssb", [128, 2, 256, 8], F32)
                win = SB("win", [128, 16, 4, 128], BF16)
                wout = SB("wout_s", [128, 16, 16, 128], BF16)
                A12 = SB("A12", [128, 2, 16], F32)
                tP = [SB("tP%d" % i, [128, 16], F32) for i in range(2)]
                tQ = [SB("tQ%d" % i, [128, 8], F32) for i in range(2)]
                ygs = [SB("ygs%d" % i, [128, 512], BF16) for i in range(2)]
                ysum = [SB("ysum%d" % i, [128, 512], F32) for i in range(2)]
                crs = SB("crs", [128, 16, 128], F32)
                pss = [PS("pss%d" % i, [128, 512], F32) for i in range(4)]
                py = [PS("py%d" % i, [128, 512], F32) for i in range(2)]

                P.dma('sp', zs[:], zS_d[b, T], 'zs', w=['zs'])
                P.dma('sp', wlag[:], WLAG_d[T], 'wlag', w=['wlag'])
                P.dma('sp', wout[:], WOUT_d[T].rearrange("k p m c -> p k m c"), 'wout', w=['wout'])
                P.op('pool', lambda e: e.memset(upad[:], 0.0), w=['upad'])
                P.op('pool', lambda e: e.tensor_copy(out=AP(upad, 15, [[UPW, 128], [31, 256], [1, 16]]),
                                                     in_=AP(zs, 0, [[S, 128], [16, 256], [1, 16]])), r=['zs'], w=['upad'])
                for dr in range(2):
                    src_r = AP(prt, 16 * 32 + T * 8 + dr, [[17 * 32, 128], [2, 4]])
                    src_i = AP(pit, 16 * 32 + T * 8 + dr, [[17 * 32, 128], [2, 4]])
                    P.op('dve', lambda e, dr=dr, src_r=src_r: e.tensor_copy(out=A12[:, dr, 0:4], in_=src_r), r=['prt'], w=['A12'])
                    P.op('dve', lambda e, dr=dr, src_r=src_r: e.tensor_copy(out=A12[:, dr, 4:8], in_=src_r), r=['prt'], w=['A12'])
                    P.op('dve', lambda e, dr=dr, src_i=src_i: e.tensor_scalar(out=A12[:, dr, 8:12], in0=src_i, scalar1=-1.0, scalar2=None, op0=ALU.mult), r=['pit'], w=['A12'])
                    P.op('dve', lambda e, dr=dr, src_i=src_i: e.tensor_copy(out=A12[:, dr, 12:16], in_=src_i), r=['pit'], w=['A12'])
                P.op('dve', lambda e: e.memset(Hs[0][:], 0.0), w=['H0'])
                P.op('pool', lambda e: e.memset(Hs[1][:], 0.0), w=['H1'])
                for q in range(4):
                    P.dma('sp', win[:], WIN_d[T, :, :, q * 4:(q + 1) * 4, :].rearrange("k p m c -> p k m c"), 'win', w=['win'])
                    for dr in range(2):
                        for ri in range(2):
                            pi_ = dr * 2 + ri

                            def smm(e, dr=dr, ri=ri, pi_=pi_):
                                for jp in range(16):
                                    kk = 15 - jp if dr == 0 else jp
                                    ins = e.matmul(pss[pi_][:, 0:256], lhsT=win[:, kk, dr * 2 + ri, :],
                                                   rhs=AP(upad, 15 + jp, [[UPW, 128], [31, 256]]), start=(jp == 0), stop=(jp == 15))
                                return ins
                            P.op('pe', smm, r=['win', 'upad'], w=['pss%d' % pi_])
                            P.op('act', lambda e, dr=dr, ri=ri, q=q, pi_=pi_: e.copy(out=AP(Ssb, dr * 2048 + ri * 4 + q, [[4096, 128], [8, 256]]),
                                                                                     in_=pss[pi_][:, 0:256]), r=['pss%d' % pi_], w=['Ssb'])
                for step in range(256 if not quick else 256):
                    for dr, eng in ((0, 'dve'), (1, 'pool')):
                        H = Hs[dr]
                        if dr == 0:
                            src, dst, sc_ = step, step + 1, step
                        else:
                            src, dst, sc_ = 256 - step, 255 - step, 255 - step
                        hk = 'H%d' % dr
                        HW = 257 * 16
                        P.op(eng, lambda e, H=H, src=src, dr=dr, HW=HW: e.tensor_tensor(out=AP(tP[dr], 0, [[16, 128], [8, 2], [1, 8]]),
                                                                                 in0=AP(H, src * 16, [[HW, 128], [4, 2], [1, 8]]),
                                                                                 in1=AP(A12, dr * 16, [[32, 128], [8, 2], [1, 8]]), op=ALU.mult),
                             r=[hk, 'A12'], w=['tP%d' % dr])
                        P.op(eng, lambda e, dr=dr: e.tensor_tensor(out=tQ[dr][:], in0=tP[dr][:, 0:8], in1=tP[dr][:, 8:16], op=ALU.add),
                             r=['tP%d' % dr], w=['tQ%d' % dr])
                        P.op(eng, lambda e, H=H, dst=dst, dr=dr, sc_=sc_, HW=HW: e.tensor_tensor(out=AP(H, dst * 16, [[HW, 128], [8, 2], [1, 8]]),
                                                                                          in0=AP(tQ[dr], 0, [[8, 128], [0, 2], [1, 8]]),
                                                                                          in1=AP(Ssb, dr * 2048 + sc_ * 8, [[4096, 128], [0, 2], [1, 8]]), op=ALU.add),
                             r=['tQ%d' % dr, 'Ssb'], w=[hk])
                P.op('dve', lambda e: e.tensor_copy(out=AP(Xh, 0, [[4096, 128], [1024, 4], [256, 2], [1, 256]]),
                                                    in_=AP(Hs[0], 0, [[257 * 16, 128], [1, 4], [4, 2], [16, 256]])), r=['H0'], w=['Xh'])
                P.op('pool', lambda e: e.tensor_copy(out=AP(Xh, 512, [[4096, 128], [1024, 4], [256, 2], [1, 256]]),
                                                     in_=AP(Hs[1], 16, [[257 * 16, 128], [1, 4], [4, 2], [16, 256]])), r=['H1'], w=['Xh'])
                for half in range(2):
                    h0 = half * 128
                    for g4 in range(4):
                        def cmm(e, g4=g4, h0=h0):
                            for jj in range(4):
                                j = g4 * 4 + jj
                                n = 0
                                for q in range(4):
                                    for dr in range(2):
                                        kidx = j if dr == 0 else 15 - j
                                        for ri in range(2):
                                            ins = e.matmul(pss[g4][:, jj * 128:(jj + 1) * 128], lhsT=wout[:, kidx, q * 4 + dr * 2 + ri, :],
                                                           rhs=Xh[:, q, dr, ri, h0:h0 + 128], start=(n == 0), stop=(n == 15))
                                            n += 1
                            return ins
                        P.op('pe', cmm, r=['wout', 'Xh'], w=['pss%d' % g4])
                        P.op('act', lambda e, g4=g4: e.copy(out=crs[:, g4 * 4:(g4 + 1) * 4, :], in_=pss[g4][:, :]), r=['pss%d' % g4], w=['crs'])
                    for t4 in range(4):
                        tt_ = half * 4 + t4
                        c0 = tt_ * 32
                        ip = tt_ % 2

                        def ymm(e, c0=c0, ip=ip):
                            outv = AP(py[ip], 0, [[512, 128], [16, 32], [1, 16]])
                            for li_ in range(31):
                                dl = li_ - 15
                                ins = e.matmul(outv, lhsT=wlag[:, li_, :], rhs=AP(upad, 15 + c0 * 31 - dl, [[UPW, 128], [31, 32], [1, 16]]),
                                               start=(li_ == 0), stop=(li_ == 30))
                            return ins
                        P.op('pe', ymm, r=['wlag', 'upad'], w=['py%d' % ip])
                        P.op('dve', lambda e, ip=ip, t4=t4: e.tensor_tensor(out=AP(ysum[ip], 0, [[512, 128], [16, 32], [1, 16]]),
                                                                            in0=AP(py[ip], 0, [[512, 128], [16, 32], [1, 16]]),
                                                                            in1=AP(crs, t4 * 32, [[2048, 128], [1, 32], [128, 16]]), op=ALU.add),
                             r=['py%d' % ip, 'crs'], w=['ysum%d' % ip])
                        P.op('act', lambda e, ip=ip: e.activation(out=ygs[ip][:], in_=ysum[ip][:], func=AF.Gelu), r=['ysum%d' % ip], w=['ygs%d' % ip])
                        P.dma('sp', yG_d[b, T, :, tt_ * 512:(tt_ + 1) * 512], ygs[ip][:], 'ygo%d' % ip, r=['ygs%d' % ip], w=['ygs%d' % ip])
                P.barrier()
            P.new_sems()
    if upto < 5:
        return nc, P, {}
    with ExitStack() as st:
        def SB(name, shape, dt):
            return st.enter_context(nc.sbuf_tensor(name, list(shape), dt))

        def PS(name, shape, dt=F32):
            return st.enter_context(nc.psum_tensor(name, list(shape), dt))
        stg = SB("stg", [128, 8, D], F32)
        wglub = SB("wglub", [128, 4, 512], BF16)
        woutb = SB("woutb", [128, 8, D], BF16)
        P.dma('sp', AP(stg, 0, [[8 * D, 128], [1, 2048]]), wglu.rearrange("p t n -> p (t n)"), 'c_wglu', w=['stg'])
        P.op('dve', lambda e: e.tensor_copy(out=wglub[:], in_=AP(stg, 0, [[8 * D, 128], [512, 4], [1, 512]])), r=['stg'], w=['wglub'])
        P.dma('sp', stg[:], woutw[:, :, :], 'c_wout', r=['wglub'], w=['stg'])
        P.op('dve', lambda e: e.tensor_copy(out=woutb[:], in_=stg[:]), r=['stg'], w=['woutb'])
        ygb = [SB("ygb%d" % i, [128, 4, 128], BF16) for i in range(2)]
        yfb = [SB("yfb%d" % i, [128, 4, 128], BF16) for i in range(2)]
        xs = [SB("xsd%d" % i, [128, D], F32) for i in range(2)]
        sg = SB("sg", [128, 4, 128], BF16)
        ysg = [SB("ysg%d" % i, [128, 4, 128], BF16) for i in range(2)]
        x1s = [SB("x1s%d" % i, [128, D], F32) for i in range(2)]
        sq = SB("sqd", [128, D], F32)
        ss = [SB("ssd%d" % i, [128, 4], F32) for i in range(2)]
        xn2 = [SB("xn2%d" % i, [128, D], BF16) for i in range(2)]
        hT2 = [SB("hT2%d" % i, [128, 8, 128], BF16) for i in range(2)]
        pg = PS("pg", [128, 4, 128], F32)
        po = [PS("pod%d" % i, [128, 512], F32) for i in range(2)]
        pT2 = PS("pT2", [128, 8, 128], BF16)
        nblk = NB * (S // 128)
        for blk in range(nblk if not quick else 2):
            b, tb_ = divmod(blk, S // 128)
            i = blk % 2
            t0 = tb_ * 128
            P.dma('sp', ygb[i][:], yG_d[b, :, :, t0:t0 + 128].rearrange("t p s -> p t s"), 'ygb%d' % i, w=['ygb%d' % i])
            P.dma('sp', yfb[i][:], yF_d[b, :, :, t0:t0 + 128].rearrange("t p s -> p t s"), 'yfb%d' % i, w=['yfb%d' % i])
            P.dma('sp', xs[i][:], x[b, t0:t0 + 128, :], 'xsd%d' % i, w=['xsd%d' % i])

            def glu(e, i=i):
                for n in range(4):
                    for T in range(4):
                        ins = e.matmul(pg[:, n, :], lhsT=wglub[:, T, n * 128:(n + 1) * 128], rhs=ygb[i][:, T, :], start=(T == 0), stop=(T == 3))
                return ins
            P.op('pe', glu, r=['wglub', 'ygb%d' % i], w=['pg'])
            P.op('act', lambda e: e.activation(out=sg[:], in_=pg[:], func=AF.Sigmoid), r=['pg'], w=['sg'])
            P.op('dve', lambda e, i=i: e.tensor_tensor(out=ysg[i][:], in0=sg[:], in1=ygb[i][:], op=ALU.mult), r=['sg', 'ygb%d' % i], w=['ysg%d' % i])
            for hf in range(2):
                def omm(e, i=i, hf=hf):
                    for c8 in range(8):
                        lt = ysg[i][:, c8, :] if c8 < 4 else yfb[i][:, c8 - 4, :]
                        ins = e.matmul(po[hf][:, :], lhsT=lt, rhs=woutb[:, c8, hf * 512:(hf + 1) * 512], start=(c8 == 0), stop=(c8 == 7))
                    return ins
                P.op('pe', omm, r=['ysg%d' % i, 'yfb%d' % i, 'woutb'], w=['pod%d' % hf])
                P.op('dve', lambda e, i=i, hf=hf: e.tensor_tensor(out=x1s[i][:, hf * 512:(hf + 1) * 512], in0=po[hf][:, :], in1=xs[i][:, hf * 512:(hf + 1) * 512], op=ALU.add),
                     r=['pod%d' % hf, 'xsd%d' % i], w=['x1s%d_%d' % (i, hf)])
            P.dma('sp', x1_d[b, t0:t0 + 128, :], x1s[i][:], 'x1o%d' % i, r=['x1s%d_0' % i, 'x1s%d_1' % i], w=['x1o%d' % i])
            P.op('act', lambda e, i=i: e.activation(out=sq[:], in_=x1s[i][:], func=AF.Square, accum_out=ss[i][:, 0:1]),
                 r=['x1s%d_0' % i, 'x1s%d_1' % i], w=['sqd', 'ssa%d' % i])
            P.op('act', lambda e, i=i: e.activation(out=ss[i][:, 1:2], in_=ss[i][:, 0:1], func=AF.Sqrt, scale=1.0 / D, bias=epsc[:, 0:1]),
                 r=['ssa%d' % i, 'epsc'], w=['ssb%d' % i])
            P.op('dve', lambda e, i=i: e.reciprocal(out=ss[i][:, 2:3], in_=ss[i][:, 1:2]), r=['ssb%d' % i], w=['ssc%d' % i])
            P.op('dve', lambda e, i=i: e.tensor_scalar(out=xn2[i][:], in0=x1s[i][:], scalar1=ss[i][:, 2:3], scalar2=None, op0=ALU.mult),
                 r=['x1s%d_0' % i, 'x1s%d_1' % i, 'ssc%d' % i], w=['xn2%d' % i])

            def tr2(e, i=i):
                for kc in range(8):
                    ins = e.transpose(out=pT2[:, kc, :], in_=xn2[i][:, kc * 128:(kc + 1) * 128], identity=identb[:])
                return ins
            P.op('pe', tr2, r=['xn2%d' % i, 'identb'], w=['pT2'])
            P.op('act', lambda e, i=i: e.copy(out=hT2[i][:], in_=pT2[:]), r=['pT2'], w=['hT2%d' % i])
            P.dma('sp', h2T_d[b, :, :, t0:t0 + 128], hT2[i][:], 'h2o%d' % i, r=['hT2%d' % i], w=['hT2%d' % i])
        P.barrier()
    P.new_sems()
    if upto < 6:
        return nc, P, {}
    keysT_in = din("keysT_in", [128, 16, 128])
    iota128_in = din("iota128", [128, 128])
    iota16_in = din("iota16", [128, 16])
    G_d = dscr("G_d", [64, 128, 128, 128])
    wqb = SBp("wqb", [128, 8, 2048], BF16)
    keysb = SBp("keysb", [128, 16, 128], BF16)
    iota128 = SBp("iota128s", [128, 128], F32)
    iota16 = SBp("iota16s", [128, 16], F32)
    P.dma('sp', iota128[:], iota128_in[:, :], 'c_io128', w=['iota128'])
    P.dma('sp', iota16[:], iota16_in[:, :], 'c_io16', w=['iota16'])
    with ExitStack() as st:
        def SB(name, shape, dt):
            return st.enter_context(nc.sbuf_tensor(name, list(shape), dt))
        stq = SB("stq", [128, 4, 2048], F32)
        kst = SB("kst", [128, 16, 128], F32)
        P.dma('sp', kst[:], keysT_in[:, :, :], 'c_keys', w=['kst'])
        for hq in range(2):
            P.dma('sp', stq[:], wq[:, hq * 4:(hq + 1) * 4, :], 'c_wq', w=['stq'])
            for kk in range(4):
                kc = hq * 4 + kk
                P.op('dve', lambda e, kc=kc, kk=kk: e.tensor_scalar(out=wqb[:, kc, :], in0=stq[:, kk, :], scalar1=g2s[:, kc:kc + 1], scalar2=None, op0=ALU.mult),
                     r=['stq', 'g2s'], w=['wqb'])
        P.op('pool', lambda e: e.tensor_copy(out=keysb[:], in_=kst[:]), r=['kst'], w=['keysb'])
        P.barrier()
    P.new_sems()

    with ExitStack() as st:
        def SB(name, shape, dt):
            return st.enter_context(nc.sbuf_tensor(name, list(shape), dt))

        def PS(name, shape, dt=F32):
            return st.enter_context(nc.psum_tensor(name, list(shape), dt))
        NT = 128
        h2s = [SB("h2_%d" % i, [128, 8, NT], BF16) for i in range(2)]
        qTs = [SB("qT_%d" % i, [128, 16, NT], BF16) for i in range(2)]
        scss = [SB("scs_%d" % i, [128, 16, 128], F32) for i in range(2)]
        scr = SB("scr", [128, 256], F32)
        v16 = SB("v16", [128, 16, 16], F32)
        ix16 = SB("ix16", [128, 16, 16], U32)
        ixf = SB("ixf", [128, 16, 16], F32)
        cand = SB("cand", [128, 8, 256], F32)
        tv = SB("tv", [128, 8, 16], F32)
        tve = SB("tve", [128, 8, 16], F32)
        pos = SB("pos", [128, 8, 16], U32)
        posf = SB("posf", [128, 8, 16], F32)
        paf = SB("paf", [128, 8, 16], F32); pbf = SB("pbf", [128, 8, 16], F32)
        eq = SB("eq", [128, 8, 16, 16], F32)
        i16a = SB("i16a", [128, 16], F32); i16b = SB("i16b", [128, 16], F32)
        sel = SB("sel", [128, 3, 128], F32)
        selb = SB("selb", [128, 3, 128], BF16)
        selT = SB("selT", [128, 3, 128], BF16)
        iob = SB("iob", [128, 128], BF16)
        zsum = SB("zsum", [128, 8], F32)
        OJs = [SB("OJ%d" % i, [128, 32, 128], BF16) for i in range(2)]
        OIs = [SB("OI%d" % i, [128, 32, 128], BF16) for i in range(2)]
        Gsb = [SB("Gs%d" % i, [128, 128, NT], BF16) for i in range(2)]
        Bk = [PS("Bk%d" % i, [128, 512], F32) for i in range(7)]
        Bk7b = PS("Bk7b", [128, 1024], BF16)
        eq2 = cand

        def bk(i):
            return 'Bk%d' % i
        P.op('dve', lambda e: e.tensor_copy(out=iob[:], in_=iota128[:]), r=['iota128'], w=['iob'])
        P.op('dve', lambda e: e.tensor_scalar(out=i16a[:], in0=iota16[:], scalar1=16.0, scalar2=None, op0=ALU.mult), r=['iota16'], w=['i16a'])
        P.op('dve', lambda e: e.tensor_scalar(out=i16b[:], in0=iota16[:], scalar1=16.0, scalar2=16.0, op0=ALU.mult, op1=ALU.add), r=['iota16'], w=['i16b'])
        nblk = NB * S // NT
        def front(blk):
                b, tb_ = divmod(blk, S // NT)
                t0 = tb_ * NT
                par = blk % 2
                h2 = h2s[par]; qT = qTs[par]; scs = scss[par]
                Gs = Gsb[blk % 2]
                gsk = 'Gs%d' % (blk % 2)
                P.dma('sp', h2[:], h2T_d[b, :, :, t0:t0 + NT], 'h2_%d' % par, w=['h2_%d' % par])
                for m in range(16):
                    pb_ = 4 + (m % 2)

                    def qmm(e, m=m, pb_=pb_):
                        for kc in range(8):
                            ins = e.matmul(Bk[pb_][:, 0:NT], lhsT=wqb[:, kc, m * 128:(m + 1) * 128], rhs=h2[:, kc, :], start=(kc == 0), stop=(kc == 7))
                        return ins
                    P.op('pe', qmm, r=['wqb', 'h2_%d' % par], w=[bk(pb_)])
                    P.op('act', lambda e, m=m, pb_=pb_: e.copy(out=qT[:, m, :], in_=Bk[pb_][:, 0:NT]), r=[bk(pb_)], w=['qT_%d' % par])
                for m4 in range(4):
                    def smm2(e, m4=m4):
                        for mm in range(4):
                            m = m4 * 4 + mm
                            ins = e.matmul(Bk[m4][:, mm * 128:(mm + 1) * 128], lhsT=qT[:, m, :], rhs=keysb[:, m, :], start=True, stop=True)
                        return ins
                    P.op('pe', smm2, r=['qT_%d' % par, 'keysb'], w=[bk(m4)])
                    P.op('act', lambda e, m4=m4: e.copy(out=scs[:, m4 * 4:(m4 + 1) * 4, :], in_=Bk[m4][:, :]), r=[bk(m4)], w=['scs_%d' % par])

        def mid_a(blk):
                b, tb_ = divmod(blk, S // NT)
                t0 = tb_ * NT
                par = blk % 2
                h2 = h2s[par]; qT = qTs[par]; scs = scss[par]
                for m in range(16):
                    P.op('dve', lambda e, m=m: e.max(out=v16[:, m, 0:8], in_=scs[:, m, :]), r=['scs_%d' % par], w=['v16'])
                    P.op('dve', lambda e, m=m: e.max_index(out=ix16[:, m, 0:8], in_max=v16[:, m, 0:8], in_values=scs[:, m, :]), r=['scs_%d' % par, 'v16'], w=['ix16'])
                    P.op('dve', lambda e, m=m: e.match_replace(out=scr[:, 0:128], in_to_replace=v16[:, m, 0:8], in_values=scs[:, m, :], imm_value=-1e30),
                         r=['scs_%d' % par, 'v16'], w=['scr'])
                    P.op('dve', lambda e, m=m: e.max(out=v16[:, m, 8:16], in_=scr[:, 0:128]), r=['scr'], w=['v16'])
                    P.op('dve', lambda e, m=m: e.max_index(out=ix16[:, m, 8:16], in_max=v16[:, m, 8:16], in_values=scr[:, 0:128]), r=['scr', 'v16'], w=['ix16'])
                P.op('dve', lambda e: e.tensor_copy(out=ixf[:], in_=ix16[:]), r=['ix16'], w=['ixf'])
                P.op('dve', lambda e: e.tensor_tensor(out=AP(cand, 0, [[2048, 128], [256, 8], [16, 16], [1, 16]]),
                                                      in0=AP(v16, 0, [[256, 128], [32, 8], [1, 16], [0, 16]]),
                                                      in1=AP(v16, 16, [[256, 128], [32, 8], [0, 16], [1, 16]]), op=ALU.add), r=['v16'], w=['cand'])
                for h in range(8):
                    P.op('dve', lambda e, h=h: e.max(out=tv[:, h, 0:8], in_=cand[:, h, :]), r=['cand'], w=['tv'])
                    P.op('dve', lambda e, h=h: e.max_index(out=pos[:, h, 0:8], in_max=tv[:, h, 0:8], in_values=cand[:, h, :]), r=['cand', 'tv'], w=['pos'])
                    P.op('dve', lambda e, h=h: e.match_replace(out=scr[:, 0:256], in_to_replace=tv[:, h, 0:8], in_values=cand[:, h, :], imm_value=-1e30),
                         r=['cand', 'tv'], w=['scr'])
                    P.op('dve', lambda e, h=h: e.max(out=tv[:, h, 8:16], in_=scr[:, 0:256]), r=['scr'], w=['tv'])
                    P.op('dve', lambda e, h=h: e.max_index(out=pos[:, h, 8:16], in_max=tv[:, h, 8:16], in_values=scr[:, 0:256]), r=['scr', 'tv'], w=['pos'])

        def gate_(blk):
                b, tb_ = divmod(blk, S // NT)
                t0 = tb_ * NT
                par = blk % 2
                h2 = h2s[par]; qT = qTs[par]; scs = scss[par]
                P.op('dve', lambda e: e.tensor_tensor(out=tve[:], in0=tv[:], in1=AP(tv, 0, [[128, 128], [16, 8], [0, 16]]), op=ALU.subtract), r=['tv'], w=['tve'])
                P.op('act', lambda e: e.activation(out=tve[:], in_=tve[:], func=AF.Exp), r=['tve'], w=['tve'])

        def mid_b(blk):
                b, tb_ = divmod(blk, S // NT)
                t0 = tb_ * NT
                par = blk % 2
                h2 = h2s[par]; qT = qTs[par]; scs = scss[par]
                P.op('dve', lambda e: e.tensor_copy(out=posf[:], in_=pos[:]), r=['pos'], w=['posf'])
                posb = AP(posf, 0, [[128, 128], [16, 8], [1, 16], [0, 16]])
                P.op('dve', lambda e, posb=posb: e.tensor_tensor(out=eq[:], in0=posb, in1=AP(i16a, 0, [[16, 128], [0, 8], [0, 16], [1, 16]]), op=ALU.is_ge),
                     r=['posf', 'i16a'], w=['eq'])
                P.op('dve', lambda e, posb=posb: e.tensor_tensor(out=eq2[:].rearrange("p h (a b) -> p h a b", b=16) if False else AP(cand, 0, [[2048, 128], [256, 8], [16, 16], [1, 16]]),
                                                                in0=posb, in1=AP(i16b, 0, [[16, 128], [0, 8], [0, 16], [1, 16]]), op=ALU.is_ge),
                     r=['posf', 'i16b', 'pos'], w=['cand'])
                P.op('dve', lambda e: e.tensor_tensor(out=eq[:], in0=eq[:], in1=AP(cand, 0, [[2048, 128], [256, 8], [16, 16], [1, 16]]), op=ALU.subtract), r=['eq', 'cand'], w=['eq'])
                c4 = AP(cand, 0, [[2048, 128], [256, 8], [16, 16], [1, 16]])
                c3 = AP(cand, 0, [[2048, 128], [16, 128], [1, 16]])
                P.op('dve', lambda e, c4=c4: e.tensor_tensor(out=c4, in0=eq[:], in1=AP(ixf, 0, [[256, 128], [32, 8], [0, 16], [1, 16]]), op=ALU.mult), r=['eq', 'ixf'], w=['cand'])
                P.op('dve', lambda e, c3=c3: e.tensor_reduce(out=sel[:, 0, :], in_=c3, axis=AX.X, op=ALU.add), r=['cand'], w=['sel'])
                P.op('dve', lambda e, c4=c4: e.tensor_tensor(out=c4, in0=eq[:], in1=AP(iota16, 0, [[16, 128], [0, 8], [0, 16], [1, 16]]), op=ALU.mult), r=['eq', 'iota16'], w=['cand'])
                P.op('dve', lambda e, c3=c3: e.tensor_reduce(out=paf[:], in_=c3, axis=AX.X, op=ALU.add), r=['cand'], w=['paf'])
                P.op('dve', lambda e: e.scalar_tensor_tensor(out=pbf[:], in0=paf[:], scalar=-16.0, in1=posf[:], op0=ALU.mult, op1=ALU.add), r=['paf', 'posf'], w=['pbf'])
                P.op('dve', lambda e: e.tensor_tensor(out=eq[:], in0=AP(iota16, 0, [[16, 128], [0, 8], [0, 16], [1, 16]]),
                                                      in1=AP(pbf, 0, [[128, 128], [16, 8], [1, 16], [0, 16]]), op=ALU.is_equal), r=['iota16', 'pbf'], w=['eq'])
                P.op('dve', lambda e, c4=c4: e.tensor_tensor(out=c4, in0=eq[:], in1=AP(ixf, 16, [[256, 128], [32, 8], [0, 16], [1, 16]]), op=ALU.mult), r=['eq', 'ixf'], w=['cand'])
                P.op('dve', lambda e, c3=c3: e.tensor_reduce(out=sel[:, 1, :], in_=c3, axis=AX.X, op=ALU.add), r=['cand'], w=['sel'])
                P.op('dve', lambda e: e.tensor_reduce(out=zsum[:], in_=tve[:], axis=AX.X, op=ALU.add), r=['tve'], w=['zsum'])
                P.op('dve', lambda e: e.reciprocal(out=zsum[:], in_=zsum[:]), r=['zsum'], w=['zsum'])
                P.op('dve', lambda e: e.tensor_tensor(out=AP(sel, 256, [[384, 128], [16, 8], [1, 16]]), in0=tve[:], in1=AP(zsum, 0, [[8, 128], [1, 8], [0, 16]]), op=ALU.mult),
                     r=['tve', 'zsum'], w=['sel'])
                P.op('dve', lambda e: e.tensor_copy(out=selb[:], in_=sel[:]), r=['sel'], w=['selb'])

        def tail(blk):
                b, tb_ = divmod(blk, S // NT)
                t0 = tb_ * NT
                par = blk % 2
                h2 = h2s[par]; qT = qTs[par]; scs = scss[par]
                Gs = Gsb[blk % 2]
                gsk = 'Gs%d' % (blk % 2)

                def trs(e):
                    for c3_ in range(3):
                        ins = e.transpose(out=AP(Bk7b, c3_ * 128, [[1024, 128], [1, 128]]), in_=selb[:, c3_, :], identity=identb[:])
                    return ins
                P.op('pe', trs, r=['selb', 'identb'], w=['Bk7b'])
                P.op('act', lambda e: e.copy(out=selT[:], in_=AP(Bk7b, 0, [[1024, 128], [128, 3], [1, 128]])), r=['Bk7b'], w=['selT'])
                for tg in range(4):
                    io_b = AP(iob, 0, [[128, 128], [0, 32], [1, 128]])
                    OJ = OJs[tg % 2]; OI = OIs[tg % 2]; ojk = 'OJ%d' % (tg % 2); oik = 'OI%d' % (tg % 2)
                    P.op('dve', lambda e, io_b=io_b, tg=tg, OJ=OJ: e.tensor_tensor(out=OJ[:], in0=io_b, in1=AP(selT, 128 + tg * 32, [[384, 128], [1, 32], [0, 128]]), op=ALU.is_equal),
                         r=['iob', 'selT'], w=[ojk])
                    P.op('dve', lambda e, io_b=io_b, tg=tg, OI=OI: e.tensor_tensor(out=OI[:], in0=io_b, in1=AP(selT, tg * 32, [[384, 128], [1, 32], [0, 128]]), op=ALU.is_equal),
                         r=['iob', 'selT'], w=[oik])
                    P.op('pool', lambda e, tg=tg, OI=OI: e.tensor_tensor(out=OI[:], in0=OI[:], in1=AP(selT, 256 + tg * 32, [[384, 128], [1, 32], [0, 128]]), op=ALU.mult),
                         r=[oik, 'selT'], w=[oik])
                    for t4 in range(8):
                        pbk = (6, 4, 5, 0, 1, 2, 3)[(tg * 8 + t4) % 7]

                        def gmm(e, t4=t4, pbk=pbk, OJ=OJ, OI=OI):
                            for tq in range(4):
                                tl = t4 * 4 + tq
                                ins = e.matmul(Bk[pbk][:, tq * 128:(tq + 1) * 128], lhsT=OJ[:, tl, :], rhs=OI[:, tl, :], start=True, stop=True)
                            return ins
                        P.op('pe', gmm, r=[ojk, oik], w=[bk(pbk)])
                        P.op('act', lambda e, t4=t4, pbk=pbk, tg=tg, Gs=Gs: e.copy(out=AP(Gs, tg * 32 + t4 * 4, [[128 * NT, 128], [1, 4], [NT, 128]]),
                                                                           in_=AP(Bk[pbk], 0, [[512, 128], [128, 4], [1, 128]])), r=[bk(pbk)], w=[gsk])
                P.dma('sp', G_d[blk], Gs[:], 'gdo%d' % (blk % 2), r=[gsk], w=[gsk, 'Gd%d' % blk])

        nb1 = nblk if not quick else 1
        front(0)
        for blk in range(nb1):
            mid_a(blk)
            gate_(blk)
            if blk + 1 < nb1:
                front(blk + 1)
            mid_b(blk)
            tail(blk)
        P.barrier()

    with ExitStack() as st:
        def SB(name, shape, dt):
            return st.enter_context(nc.sbuf_tensor(name, list(shape), dt))

        def PS(name, shape, dt=F32):
            return st.enter_context(nc.psum_tensor(name, list(shape), dt))
        NTM = 384
        h2e = SB("h2e", [128, 8, NTM], BF16)
        utb = [SB("utb%d" % i, [128, 8, 128], BF16) for i in range(8)]
        vtb = [SB("vtb%d" % i, [128, 1024], BF16) for i in range(8)]
        gq = [SB("gq%d" % i, [128, 3, 4, 128], BF16) for i in range(2)]
        glb = [SB("glb%d" % i, [128, NTM], BF16) for i in range(2)]
        actb = [SB("actb%d" % i, [128, NTM], BF16) for i in range(2)]
        x1b = SB("x1b", [128, D], F32)
        o2 = SB("o2", [128, D], F32)
        sqe = SB("sqe", [128, D], F32)
        sse = SB("sse", [128, 4], F32)
        Bo = [PS("Bo%d" % i, [128, 512], F32) for i in range(6)]
        Bp = [PS("Bp%d" % i, [128, 512], F32) for i in range(2)]
        blocks = []
        for b in range(NB):
            t = 0
            for nt in [384] * 10 + [256]:
                blocks.append((b, t, nt))
                t += nt
        for (b, t0, nt) in (blocks if not quick else blocks[:1]):
            nsub = nt // 128
            tb0 = (b * S + t0) // 128
            P.dma('sp', h2e[:, :, 0:nt], h2T_d[b, :, :, t0:t0 + nt], 'h2e', w=['h2e'])
            def emit_pre(ci):
                sl = ci % 8
                P.dma('sp', utb[sl][:], UTb_d[ci], 'utb%d' % sl, w=['utb%d' % sl])
                P.dma('pool', vtb[sl][:], Vb_d[ci], 'vtb%d' % sl, w=['vtb%d' % sl])
                gsl = (ci // 4) % 2
                if ci % 4 == 0:
                    for sub in range(nsub):
                        P.dma('sp', gq[gsl][:, sub, :, :], G_d[tb0 + sub, :, ci:ci + 4, :], 'gq%d' % gsl, r=['Gd%d' % (tb0 + sub)], w=['gq%d' % gsl])
                pp = ci % 2

                def pmm(e, sl=sl, pp=pp, nt=nt):
                    for kc in range(8):
                        ins = e.matmul(Bp[pp][:, 0:nt], lhsT=utb[sl][:, kc, :], rhs=h2e[:, kc, 0:nt], start=(kc == 0), stop=(kc == 7))
                    return ins
                P.op('pe', pmm, r=['utb%d' % sl, 'h2e'], w=['Bp%d' % pp])
                P.op('act', lambda e, pp=pp, nt=nt: e.activation(out=glb[pp][:, 0:nt], in_=Bp[pp][:, 0:nt], func=AF.Gelu), r=['Bp%d' % pp], w=['glb%d' % pp])
                P.op('dve', lambda e, pp=pp, nt=nt, nsub=nsub, gsl=gsl, ci=ci: e.tensor_tensor(
                    out=AP(actb[pp], 0, [[NTM, 128], [128, nsub], [1, 128]]), in0=AP(glb[pp], 0, [[NTM, 128], [128, nsub], [1, 128]]),
                    in1=AP(gq[gsl], (ci % 4) * 128, [[1536, 128], [512, nsub], [1, 128]]), op=ALU.mult),
                    r=['glb%d' % pp, 'gq%d' % gsl], w=['actb%d' % pp])

            def emit_out(ci):
                sl = ci % 8
                pp = ci % 2
                def omm(e, sl=sl, pp=pp, nsub=nsub, ci=ci):
                    for sub in range(nsub):
                        for hf in range(2):
                            ins = e.matmul(Bo[sub * 2 + hf][:, :], lhsT=actb[pp][:, sub * 128:(sub + 1) * 128], rhs=vtb[sl][:, hf * 512:(hf + 1) * 512],
                                           start=(ci == 0), stop=(ci == 127))
                    return ins
                P.op('pe', omm, r=['actb%d' % pp, 'vtb%d' % sl], w=['Bo'])
            emit_pre(0)
            for ci in range(128):
                if ci + 1 < 128:
                    emit_pre(ci + 1)
                emit_out(ci)
            for sub in range(nsub):
                tt0 = t0 + sub * 128
                P.dma('sp', x1b[:], x1_d[b, tt0:tt0 + 128, :], 'x1b', w=['x1b'])
                for hf in range(2):
                    P.op('dve', lambda e, sub=sub, hf=hf: e.tensor_tensor(out=o2[:, hf * 512:(hf + 1) * 512], in0=Bo[sub * 2 + hf][:, :], in1=x1b[:, hf * 512:(hf + 1) * 512], op=ALU.add),
                         r=['Bo', 'x1b'], w=['o2_%d' % hf])
                P.op('act', lambda e: e.activation(out=sqe[:], in_=o2[:], func=AF.Square, accum_out=sse[:, 0:1]), r=['o2_0', 'o2_1'], w=['sqe', 'ssea'])
                P.op('act', lambda e: e.activation(out=sse[:, 1:2], in_=sse[:, 0:1], func=AF.Sqrt, scale=1.0 / D, bias=epsc[:, 0:1]), r=['ssea', 'epsc'], w=['sseb'])
                P.op('dve', lambda e: e.reciprocal(out=sse[:, 2:3], in_=sse[:, 1:2]), r=['sseb'], w=['ssec'])
                P.op('dve', lambda e: e.scalar_tensor_tensor(out=sqe[:], in0=o2[:], scalar=sse[:, 2:3], in1=gfin_s[:], op0=ALU.mult, op1=ALU.mult),
                     r=['o2_0', 'o2_1', 'ssec', 'gfin', 'sqe'], w=['sqe'])
                P.dma('sp', y[b, tt0:tt0 + 128, :], sqe[:], 'yo', r=['sqe'], w=['yo'])
        P.barrier()
    return nc, P, {}


def host_inputs(inp):
    f = np.float32

    def kmaj(w):
        K, N = w.shape
        return np.ascontiguousarray(w.reshape(K // 128, 128, N).transpose(1, 0, 2)).astype(f)
    com = {}
    com["w1"] = kmaj(inp["w_in"][0])
    com["g1"] = np.ascontiguousarray(inp["norm1_g"][0].reshape(8, 128).T).astype(f)
    com["wout"] = kmaj(inp["w_out"][0])
    com["wglu"] = kmaj(inp["w_glu"][0])
    com["wq"] = kmaj(inp["w_query"][0])
    com["g2"] = np.ascontiguousarray(inp["norm2_g"][0].reshape(8, 128).T).astype(f)
    com["gfin"] = np.ascontiguousarray(np.broadcast_to(inp["final_g"][None, :], (128, D))).astype(f)
    com["wf"] = np.ascontiguousarray(inp["w_fourier"][0].transpose(1, 0, 2)).astype(f)
    c = np.arange(128)
    ang = 2 * np.pi * np.outer(c, c) / 128.0
    sc = 1.0 / math.sqrt(S * 128)
    com["ccsc"] = np.stack([np.cos(ang) * sc, -np.sin(ang) * sc], axis=1).astype(f)
    com["ident"] = np.eye(128, dtype=f)
    s_idx = np.arange(S)
    ks = (np.outer(s_idx, s_idx) % S).astype(np.float64) * (2 * np.pi / S)
    ct = np.cos(ks).astype(f).astype(ml_dtypes.bfloat16)
    stt = np.sin(ks).astype(f).astype(ml_dtypes.bfloat16)
    t = np.stack([ct, stt], axis=0)
    t = t.reshape(2, 32, 128, 16, 256).transpose(3, 2, 0, 1, 4)
    com["tab"] = np.ascontiguousarray(t)
    com["iota128"] = np.ascontiguousarray(np.broadcast_to(np.arange(128, dtype=f)[None, :], (128, 128)))
    com["iota16"] = np.ascontiguousarray(np.broadcast_to(np.arange(16, dtype=f)[None, :], (128, 16)))

    def lamA(arr):
        return np.ascontiguousarray(arr.reshape(2, 4, 4, 2, 64).transpose(3, 4, 1, 2, 0).reshape(128, 32)).astype(f)
    com["lam_are"] = lamA(inp["ssm_a_re"][0])
    com["lam_aim"] = lamA(inp["ssm_a_im"][0])
    com["lam_lst"] = lamA(np.broadcast_to(inp["ssm_log_step"][0][:, :, None], (2, 32, 64)))
    bA = np.zeros((2, 4, 128, 8, 128), f)
    cA = np.zeros((2, 4, 128, 8, 128), f)
    for ri, (bsrc, csrc) in enumerate(((inp["ssm_b_re"][0], inp["ssm_c_re"][0]), (inp["ssm_b_im"][0], inp["ssm_c_im"][0]))):
        for T in range(4):
            for q in range(4):
                for gp in range(2):
                    g = 8 * T + 2 * q + gp
                    for dr in range(2):
                        col = (2 * q + gp) * 16
                        bA[ri, T, gp * 64:(gp + 1) * 64, q * 2 + dr, col:col + 16] = bsrc[dr, g]
                        cA[ri, T, gp * 64:(gp + 1) * 64, q * 2 + dr, col:col + 16] = csrc[dr, g].T
    com["bA"] = bA
    com["cA"] = cA
    com["dD"] = np.ascontiguousarray(inp["ssm_d"][0].reshape(4, 128).T).astype(f)
    eu = inp["expert_u"][0]
    com["uT_in"] = np.ascontiguousarray(eu.reshape(16384, 8, 128).transpose(2, 1, 0)).astype(f)
    com["v_in"] = np.ascontiguousarray(inp["expert_v"][0].reshape(128, 128, 1024)).astype(f)
    sk = inp["sub_keys"][0]
    com["keysT_in"] = np.ascontiguousarray(sk.reshape(16, 128, 128).transpose(2, 0, 1)).astype(f)
    return com


_CACHE = {}


def kernel(**inp):
    if "nc" not in _CACHE:
        _CACHE["nc"] = build()[0]
    nc = _CACHE["nc"]
    com = host_inputs(inp)
    xs = np.ascontiguousarray(inp["x"]).astype(np.float32)
    in_maps = []
    for c in range(8):
        m = dict(com)
        m["x"] = xs[2 * c:2 * c + 2]
        in_maps.append(m)
    res = run_bass_kernel_spmd(nc, in_maps, core_ids=list(range(8)))
    return np.concatenate([np.asarray(r["y"]) for r in res.results], axis=0).astype(np.float32)
```

```python
import math
from contextlib import ExitStack
import numpy as np
import ml_dtypes
import concourse.bass as bass
import concourse.mybir as mybir
from concourse.bass_utils import run_bass_kernel_spmd

F32 = mybir.dt.float32
BF16 = mybir.dt.bfloat16
U32 = mybir.dt.uint32
ALU = mybir.AluOpType
AF = mybir.ActivationFunctionType
AX = mybir.AxisListType

NB = 2
S = 4096
D = 1024
L = 16
NCH = S // L
PADW = 2 * L - 1
EPS = 1e-6
ENG = ['pe', 'act', 'dve', 'pool', 'sp']


class Prog:
    def __init__(s, nc):
        s.nc = nc
        s.e = dict(pe=nc.tensor, act=nc.scalar, dve=nc.vector, pool=nc.gpsimd, sp=nc.sync)
        s.nsem = 0
        s.new_sems()
        s.dsem = {}
        s.lastw = {}
        s.readers = {}
        s.items = {k: [] for k in ENG}

    def new_sems(s):
        if s.nsem > 0:
            return
        s.sem = {}
        for k in ENG:
            s.sem[k] = s.nc.alloc_semaphore("es%d_%s" % (s.nsem, k))
        s.nsem += 1
        s.cnt = {k: 0 for k in ENG}
        s.waited = {}

    def _wait(s, eng, tok):
        kind, name, val = tok
        h = s.sem[name] if kind == 'e' else s.dsem[name][0]
        key = (eng, h.num)
        if s.waited.get(key, 0) >= val:
            return
        s.waited[key] = val
        s.items[eng].append(('w', h, val))

    def _deps(s, eng, r, w):
        deps = []
        for k in r:
            if k in s.lastw:
                deps.append((s.lastw[k], True))
        for k in w:
            if k in s.lastw:
                deps.append((s.lastw[k], False))
            for t in s.readers.get(k, ()):
                deps.append((t, False))
        for tok, raw in deps:
            if tok[0] == 'e' and tok[1] == eng:
                if raw and s.cnt[eng] - tok[2] < 2:
                    s._wait(eng, tok)
                continue
            s._wait(eng, tok)

    def _upd(s, tok, r, w):
        for k in r:
            s.readers.setdefault(k, []).append(tok)
        for k in w:
            s.lastw[k] = tok
            s.readers[k] = []

    def op(s, eng, fn, r=(), w=()):
        s._deps(eng, r, w)
        s.cnt[eng] += 1
        s.items[eng].append(('o', fn, s.sem[eng]))
        s._upd(('e', eng, s.cnt[eng]), r, w)

    def dma(s, eng, out, in_, sem, r=(), w=()):
        s._deps(eng, r, w)
        if sem not in s.dsem:
            if getattr(s, 'free_d', None):
                s.free_d.sort(key=lambda hc: hc[1])
                s.dsem[sem] = s.free_d.pop(0)
            else:
                s.ndsem = getattr(s, 'ndsem', 0) + 1
                s.dsem[sem] = [s.nc.alloc_semaphore("ds%d" % s.ndsem), 0]
        d = s.dsem[sem]
        d[1] += 16
        s.items[eng].append(('d', out, in_, d[0]))
        s._upd(('d', sem, d[1]), r, w)

    def barrier(s, final=False):
        toks = [('e', k, s.cnt[k]) for k in ENG if s.cnt[k] > 0]
        toks += [('d', n, d[1]) for n, d in s.dsem.items() if d[1] > 0]
        for eng in (['sp'] if final else ENG):
            for tok in toks:
                if tok[0] == 'e' and tok[1] == eng:
                    continue
                s._wait(eng, tok)
        s.lastw.clear()
        s.readers.clear()
        s.flush()
        if not hasattr(s, 'free_d'):
            s.free_d = []
        s.free_d.extend(s.dsem.values())
        s.dsem = {}

    def flush(s):
        def replay(items, embed=False):
            def f(e):
                pend = []
                for it in items:
                    if it[0] == 'w':
                        if embed:
                            pend.append(it)
                        else:
                            e.wait_ge(it[1], it[2])
                    elif it[0] == 'o':
                        for p in pend[:-1]:
                            e.wait_ge(p[1], p[2])
                        ins = it[1](e)
                        if pend:
                            ins._wait_ge(pend[-1][1], pend[-1][2])
                        pend = []
                        ins.then_inc(it[2], 1)
                    else:
                        e.dma_start(out=it[1], in_=it[2]).then_inc(it[3], 16)
            return f
        with s.nc.Block() as block:
            for k, dec in (('pe', block.tensor), ('act', block.scalar), ('dve', block.vector), ('pool', block.gpsimd), ('sp', block.sync)):
                if s.items[k]:
                    dec(replay(s.items[k], embed=False))
        s.items = {k: [] for k in ENG}


def AP(t, off, dims):
    return bass.AP(t, off, [list(d) for d in dims])


def build(debug=(), nblk_lim=None, stage=99, upto=99, quick=False):
    nc = bass.Bass("TRN2", target_bir_lowering=False)
    P = Prog(nc)

    def din(name, shape, dt=F32):
        return nc.dram_tensor(name, list(shape), dt, kind="ExternalInput").ap()

    dbg = {}

    def dscr(name, shape, dt=BF16):
        kind = "ExternalOutput" if name in debug else "Internal"
        a = nc.dram_tensor(name, list(shape), dt, kind=kind).ap()
        return a

    x = din("x", [NB, S, D])
    w1 = din("w1", [128, 8, D])
    g1 = din("g1", [128, 8])
    woutw = din("wout", [128, 8, D])
    wglu = din("wglu", [128, 4, 512])
    wq = din("wq", [128, 8, 2048])
    g2 = din("g2", [128, 8])
    gfin = din("gfin", [128, D])
    wf = din("wf", [128, 4, 128])
    ccsc = din("ccsc", [128, 2, 128])
    ident_in = din("ident", [128, 128])
    tab = din("tab", [16, 128, 2, 32, 256], BF16)
    y = nc.dram_tensor("y", [NB, S, D], F32, kind="ExternalOutput").ap()

    zS_d = dscr("zS_d", [NB, 4, 128, S])
    A_d = dscr("A_d", [NB, 32, 128, 1024])
    yF_d = dscr("yF_d", [NB, 4, 128, S])
    yG_d = dscr("yG_d", [NB, 4, 128, S])
    x1_d = dscr("x1_d", [NB, S, D], F32)
    h2T_d = dscr("h2T_d", [NB, 128, 8, S])

    def dump(name, tile, shape, dt, key):
        if name in debug:
            d = nc.dram_tensor(name, list(shape), dt, kind="ExternalOutput").ap()
            P.dma('sp', d, tile, 'dbg_' + name, r=[key], w=['dbg_' + name])

    pst = ExitStack()

    def SBp(name, shape, dt):
        return pst.enter_context(nc.sbuf_tensor(name, list(shape), dt))

    identb = SBp("identb", [128, 128], BF16)
    identf = SBp("identf", [128, 128], F32)
    gfin_s = SBp("gfin_s", [128, D], F32)
    epsc = SBp("epsc", [128, 1], F32)

    P.dma('sp', identf[:], ident_in[:, :], 'c_identf', w=['identf'])
    P.dma('sp', gfin_s[:], gfin[:, :], 'c_gfin', w=['gfin'])
    P.op('dve', lambda e: e.tensor_copy(out=identb[:], in_=identf[:]), r=['identf'], w=['identb'])
    P.op('dve', lambda e: e.memset(epsc[:], EPS), w=['epsc'])

    uT_in = din("uT_in", [128, 8, 16384])
    v_in = din("v_in", [128, 128, 1024])
    UTb_d = dscr("UTb_d", [128, 128, 8, 128])
    Vb_d = dscr("Vb_d", [128, 128, 1024])
    g2s = SBp("g2s", [128, 8], F32)
    P.dma('sp', g2s[:], g2[:, :], 'c_g2s', w=['g2s'])
    e0st = ExitStack()
    uf0 = e0st.enter_context(nc.sbuf_tensor("uf0", [128, 8, 512], F32))
    ub0 = e0st.enter_context(nc.sbuf_tensor("ub0", [128, 8, 512], BF16))
    vf0 = e0st.enter_context(nc.sbuf_tensor("vf0", [128, 4, 1024], F32))
    vb0 = e0st.enter_context(nc.sbuf_tensor("vb0", [128, 4, 1024], BF16))

    def e0_iter(it):
        P.dma('pool', uf0[:], uT_in[:, :, it * 512:(it + 1) * 512], 'uf0', w=['uf0'])
        for kc in range(8):
            P.op('dve' if kc % 2 else 'pool', lambda e, kc=kc: e.tensor_scalar(out=ub0[:, kc, :], in0=uf0[:, kc, :], scalar1=g2s[:, kc:kc + 1], scalar2=None, op0=ALU.mult),
                 r=['uf0', 'g2s'], w=['ub0_%d' % kc] + ['ubo0_%d' % i4 for i4 in range(4)])
        for i4 in range(4):
            P.dma('pool', UTb_d[it * 4 + i4], AP(ub0, i4 * 128, [[4096, 128], [512, 8], [1, 128]]),
                  'ubo0', r=['ub0_%d' % kc for kc in range(8)], w=['ubo0_%d' % i4])
        P.dma('pool', vf0[:], v_in[it * 4:(it + 1) * 4].rearrange("i p d -> p i d"), 'vf0', w=['vf0'])
        P.op('pool', lambda e: e.tensor_copy(out=vb0[:], in_=vf0[:]), r=['vf0'], w=['vb0'])
        P.dma('pool', Vb_d[it * 4:(it + 1) * 4].rearrange("i p d -> p i d"), vb0[:], 'vbo0', r=['vb0'], w=['vb0'])

    with ExitStack() as st:
        def SB(name, shape, dt):
            return st.enter_context(nc.sbuf_tensor(name, list(shape), dt))

        def PS(name, shape, dt=F32):
            return st.enter_context(nc.psum_tensor(name, list(shape), dt))

        w1f = SB("w1f", [128, 8, D], F32)
        w1b = SB("w1b", [128, 8, D], BF16)
        g1s = SB("g1s", [128, 8], F32)
        wfs = SB("wfs", [128, 4, 128], F32)
        ccs = SB("ccs", [128, 2, 128], F32)
        csw = SB("csw", [128, 4, 256], BF16)
        wfb = SB("wfb", [128, 4, 128], BF16)
        ccb = SB("ccb", [128, 2, 128], BF16)
        xs = [SB("xs%d" % i, [128, D], F32) for i in range(2)]
        sq = SB("sq", [128, D], F32)
        ss = [SB("ss%d" % i, [128, 4], F32) for i in range(2)]
        xn = [SB("xn%d" % i, [128, D], BF16) for i in range(2)]
        hT = [SB("hT%d" % i, [128, 8, 128], BF16) for i in range(2)]
        zT = [SB("zT%d" % i, [128, 8, 128], BF16) for i in range(2)]
        Ab = [SB("Ab%d" % i, [128, 1024], BF16) for i in range(2)]
        pT = [PS("pT%d" % i, [128, 8, 128], BF16) for i in range(2)]
        pz = [PS("pz%d" % i, [128, 8, 128], F32) for i in range(1)]
        pA = [PS("pA%d" % i, [128, 1024], F32) for i in range(1)]
        pw = PS("pw", [128, 512], F32)

        P.dma('sp', w1f[:], w1[:, :, :], 'c_w1f', w=['w1f'])
        P.dma('sp', g1s[:], g1[:, :], 'c_g1s', w=['g1s'])
        P.dma('sp', wfs[:], wf[:, :, :], 'c_wfs', w=['wfs'])
        P.dma('sp', ccs[:], ccsc[:, :, :], 'c_ccs', w=['ccs'])
        for kc in range(8):
            P.op('dve', lambda e, kc=kc: e.tensor_scalar(out=w1b[:, kc, :], in0=w1f[:, kc, :], scalar1=g1s[:, kc:kc + 1],
                                                        scalar2=None, op0=ALU.mult), r=['w1f', 'g1s'], w=['w1b'])
        P.op('dve', lambda e: e.tensor_copy(out=wfb[:], in_=wfs[:]), r=['wfs'], w=['wfb'])
        P.op('dve', lambda e: e.tensor_copy(out=ccb[:], in_=ccs[:]), r=['ccs'], w=['ccb'])
        for g in range(4 if stage >= 0 else 0):
            for t in range(2):
                P.op('pe', lambda e, g=g, t=t: e.matmul(pw[:, (g % 2) * 256 + t * 128:(g % 2) * 256 + t * 128 + 128],
                                                        lhsT=ccb[:, t, :], rhs=wfb[:, g, :], start=True, stop=True),
                     r=['ccb', 'wfb'], w=['pw'])
            P.op('dve', lambda e, g=g: e.tensor_copy(out=csw[:, g, :], in_=pw[:, (g % 2) * 256:(g % 2) * 256 + 256]),
                 r=['pw'], w=['csw'])

        nblk = NB * (S // 128) if nblk_lim is None else nblk_lim
        if stage == -2:
            nblk = 0
        for blk in range(nblk):
            b, tb = divmod(blk, S // 128)
            i = blk % 2
            t0 = tb * 128
            if blk % 2 == 0:
                e0_iter(blk // 2)
            P.dma('sp', xs[i][:], x[b, t0:t0 + 128, :], 'xs%d' % i, w=['xs%d' % i])
            P.op('act', lambda e, i=i: e.activation(out=sq[:], in_=xs[i][:], func=AF.Square, accum_out=ss[i][:, 0:1]),
                 r=['xs%d' % i], w=['sq', 'ssa%d' % i])
            P.op('act', lambda e, i=i: e.activation(out=ss[i][:, 1:2], in_=ss[i][:, 0:1], func=AF.Sqrt, scale=1.0 / D, bias=epsc[:, 0:1]),
                 r=['ssa%d' % i, 'epsc'], w=['ssb%d' % i])
            P.op('dve', lambda e, i=i: e.reciprocal(out=ss[i][:, 2:3], in_=ss[i][:, 1:2]), r=['ssb%d' % i], w=['ssc%d' % i])
            P.op('dve', lambda e, i=i: e.tensor_scalar(out=xn[i][:], in0=xs[i][:], scalar1=ss[i][:, 2:3], scalar2=None, op0=ALU.mult),
                 r=['xs%d' % i, 'ssc%d' % i], w=['xn%d' % i])

            if stage < 1:
                continue
            def tr(e, i=i):
                for kc in range(8):
                    ins = e.transpose(out=pT[i][:, kc, :], in_=xn[i][:, kc * 128:(kc + 1) * 128], identity=identb[:])
                return ins
            P.op('pe', tr, r=['xn%d' % i, 'identb'], w=['pT%d' % i])
            P.op('act', lambda e, i=i: e.copy(out=hT[i][:], in_=pT[i][:]), r=['pT%d' % i], w=['hT%d' % i])

            if stage < 2:
                continue
            def zmm(e, i=i):
                for n in range(8):
                    for kc in range(8):
                        ins = e.matmul(pz[0][:, n, :], lhsT=w1b[:, kc, n * 128:(n + 1) * 128], rhs=hT[i][:, kc, :],
                                       start=(kc == 0), stop=(kc == 7))
                return ins
            P.op('pe', zmm, r=['hT%d' % i, 'w1b'], w=['pz'])
            P.op('dve', lambda e, i=i: e.tensor_copy(out=zT[i][:], in_=pz[0][:]), r=['pz'], w=['zT%d' % i])
            if blk == 0:
                dump('d_xn', xn[i][:], [128, D], BF16, 'xn%d' % i)
                dump('d_hT', hT[i][:], [128, 8, 128], BF16, 'hT%d' % i)
                dump('d_zT', zT[i][:], [128, 8, 128], BF16, 'zT%d' % i)
                dump('d_w1b', w1b[:], [128, 8, D], BF16, 'w1b')
            P.dma('sp', zS_d[b, :, :, t0:t0 + 128].rearrange("t p s -> p t s"), zT[i][:, 0:4, :], 'zso%d' % i, r=['zT%d' % i], w=['zso%d' % i])

            if stage < 3:
                continue
            def amm(e, i=i):
                for g in range(4):
                    ins = e.matmul(pA[0][:, g * 256:(g + 1) * 256], lhsT=zT[i][:, 4 + g, :], rhs=csw[:, g, :], start=True, stop=True)
                return ins
            P.op('pe', amm, r=['zT%d' % i, 'csw'], w=['pA'])
            P.op('act', lambda e, i=i: e.copy(out=Ab[i][:], in_=pA[0][:]), r=['pA'], w=['Ab%d' % i])
            P.dma('sp', A_d[b, tb, :, :], Ab[i][:], 'ao%d' % i, r=['Ab%d' % i], w=['ao%d' % i])
        P.barrier()
    e0st.close()
    P.new_sems()
    if upto < 2:
        return nc, P, {}

    with ExitStack() as st:
        def SB(name, shape, dt):
            return st.enter_context(nc.sbuf_tensor(name, list(shape), dt))

        def PS(name, shape, dt=F32):
            return st.enter_context(nc.psum_tensor(name, list(shape), dt))
        Asb = SB("Asb", [128, 32, 1024], BF16)
        tbs = [SB("tb%d" % i, [128, 2, 32, 256], BF16) for i in range(2)]
        yst = [SB("yst%d" % i, [128, 4, 256], BF16) for i in range(2)]
        pf = [PS("pf%d" % i, [128, 512], F32) for i in range(4)]
        for b in range(NB):
            P.dma('sp', Asb[:], A_d[b].rearrange("s p n -> p s n"), 'Asb', w=['Asb'])
            for kt in range(16 if not quick else 1):
                it = (b * 16 + kt) % 2
                P.dma('sp', tbs[it][:], tab[kt], 'tb%d' % it, w=['tb%d' % it])
                for g in range(4):
                    def fmm(e, g=g, it=it):
                        n = 0
                        for cs in range(2):
                            for sc in range(32):
                                ins = e.matmul(pf[g][:, 0:256], lhsT=Asb[:, sc, g * 256 + cs * 128:g * 256 + cs * 128 + 128],
                                               rhs=tbs[it][:, cs, sc, :], start=(n == 0), stop=(n == 63))
                                n += 1
                        return ins
                    P.op('pe', fmm, r=['Asb', 'tb%d' % it], w=['pf%d' % g])
                    if g % 2:
                        P.op('act', lambda e, g=g, it=it: e.copy(out=yst[it][:, g, :], in_=pf[g][:, 0:256]), r=['pf%d' % g], w=['yst%d_%d' % (it, g)])
                    else:
                        P.op('dve', lambda e, g=g, it=it: e.tensor_copy(out=yst[it][:, g, :], in_=pf[g][:, 0:256]), r=['pf%d' % g], w=['yst%d_%d' % (it, g)])
                P.dma('sp', yF_d[b, :, :, kt * 256:(kt + 1) * 256].rearrange("g p s -> p g s"), yst[it][:], 'yfo%d' % it,
                      r=['yst%d_%d' % (it, g) for g in range(4)], w=['yst%d_%d' % (it, g) for g in range(4)])
        P.barrier()
    P.new_sems()
    if upto < 3:
        return nc, P, {}

    lam_are = din("lam_are", [128, 32]); lam_aim = din("lam_aim", [128, 32]); lam_lst = din("lam_lst", [128, 32])
    bA = din("bA", [2, 4, 128, 8, 128]); cA = din("cA", [2, 4, 128, 8, 128]); dD = din("dD", [128, 4])
    WLAG_d = dscr("WLAG_d", [4, 128, 31, 128])
    WIN_d = dscr("WIN_d", [4, 16, 128, 16, 128])
    WOUT_d = dscr("WOUT_d", [4, 16, 128, 16, 128])
    prt = SBp("prt", [128, 17, 32], F32)
    pit = SBp("pit", [128, 17, 32], F32)
    with ExitStack() as st:
        def SB(name, shape, dt):
            return st.enter_context(nc.sbuf_tensor(name, list(shape), dt))

        def PS(name, shape, dt=F32):
            return st.enter_context(nc.psum_tensor(name, list(shape), dt))
        names = ['are', 'aim', 'lst', 'stp', 'ar', 'ai', 'mag', 's16', 'cc', 'sn', 't1', 't2', 't3', 'lr', 'li', 'den', 'nr', 'cr', 'ci']
        T_ = {n: SB("l_" + n, [128, 32], F32) for n in names}
        P.dma('sp', T_['are'][:], lam_are[:, :], 'c_are', w=['are'])
        P.dma('sp', T_['aim'][:], lam_aim[:, :], 'c_aim', w=['aim'])
        P.dma('sp', T_['lst'][:], lam_lst[:, :], 'c_lst', w=['lst'])
        dDs = SB("dDs", [128, 4], F32)
        P.dma('sp', dDs[:], dD[:, :], 'c_dD', w=['dD'])

        def tt(o, a, b_, op, eng='dve'):
            P.op(eng, lambda e: e.tensor_tensor(out=T_[o][:], in0=T_[a][:], in1=T_[b_][:], op=op), r=[a, b_], w=[o])

        def ts(o, a, s1, s2, op0, op1=None):
            if op1 is None:
                P.op('dve', lambda e: e.tensor_scalar(out=T_[o][:], in0=T_[a][:], scalar1=s1, scalar2=None, op0=op0), r=[a], w=[o])
            else:
                P.op('dve', lambda e: e.tensor_scalar(out=T_[o][:], in0=T_[a][:], scalar1=s1, scalar2=s2, op0=op0, op1=op1), r=[a], w=[o])

        def act(o, a, func, scale=1.0):
            P.op('act', lambda e: e.activation(out=T_[o][:], in_=T_[a][:], func=func, scale=scale), r=[a], w=[o])
        act('stp', 'lst', AF.Exp)
        tt('ar', 'are', 'stp', ALU.mult)
        tt('ai', 'aim', 'stp', ALU.mult)
        act('mag', 'ar', AF.Exp)
        act('s16', 'ai', AF.Sin, 1.0 / 16)
        act('sn', 'ai', AF.Sin, 1.0 / 8)
        tt('t1', 's16', 's16', ALU.mult)
        ts('cc', 't1', -2.0, 1.0, ALU.mult, ALU.add)
        for _ in range(3):
            tt('t1', 'cc', 'cc', ALU.mult)
            tt('t2', 'sn', 'sn', ALU.mult)
            tt('t3', 'cc', 'sn', ALU.mult)
            tt('cc', 't1', 't2', ALU.subtract)
            ts('sn', 't3', 2.0, None, ALU.mult)
        tt('lr', 'mag', 'cc', ALU.mult)
        tt('li', 'mag', 'sn', ALU.mult)
        tt('t1', 'are', 'are', ALU.mult)
        tt('t2', 'aim', 'aim', ALU.mult)
        tt('den', 't1', 't2', ALU.add)
        P.op('dve', lambda e: e.reciprocal(out=T_['den'][:], in_=T_['den'][:]), r=['den'], w=['den'])
        ts('nr', 'lr', -1.0, None, ALU.add)
        tt('t1', 'nr', 'are', ALU.mult)
        tt('t2', 'li', 'aim', ALU.mult)
        tt('t3', 't1', 't2', ALU.add)
        tt('cr', 't3', 'den', ALU.mult)
        tt('t1', 'li', 'are', ALU.mult)
        tt('t2', 'nr', 'aim', ALU.mult)
        tt('t3', 't1', 't2', ALU.subtract)
        tt('ci', 't3', 'den', ALU.mult)
        P.op('dve', lambda e: e.memset(prt[:, 0, :], 1.0), w=['prt'])
        P.op('dve', lambda e: e.memset(pit[:, 0, :], 0.0), w=['pit'])
        for k in range(16):
            P.op('dve', lambda e, k=k: e.tensor_tensor(out=T_['t1'][:], in0=prt[:, k, :], in1=T_['lr'][:], op=ALU.mult), r=['prt', 'lr'], w=['t1'])
            P.op('dve', lambda e, k=k: e.tensor_tensor(out=T_['t2'][:], in0=pit[:, k, :], in1=T_['li'][:], op=ALU.mult), r=['pit', 'li'], w=['t2'])
            P.op('dve', lambda e, k=k: e.tensor_tensor(out=T_['t3'][:], in0=prt[:, k, :], in1=T_['li'][:], op=ALU.mult), r=['prt', 'li'], w=['t3'])
            P.op('dve', lambda e, k=k: e.tensor_tensor(out=T_['nr'][:], in0=pit[:, k, :], in1=T_['lr'][:], op=ALU.mult), r=['pit', 'lr'], w=['nr'])
            P.op('dve', lambda e, k=k: e.tensor_tensor(out=prt[:, k + 1, :], in0=T_['t1'][:], in1=T_['t2'][:], op=ALU.subtract), r=['t1', 't2'], w=['prt'])
            P.op('dve', lambda e, k=k: e.tensor_tensor(out=pit[:, k + 1, :], in0=T_['t3'][:], in1=T_['nr'][:], op=ALU.add), r=['t3', 'nr'], w=['pit'])

        bre = SB("bre", [128, 8, 128], F32); bim = SB("bim", [128, 8, 128], F32)
        cre = SB("cre", [128, 8, 128], F32); cim = SB("cim", [128, 8, 128], F32)
        Br = SB("Br", [128, 8, 128], F32); Bi = SB("Bi", [128, 8, 128], F32)
        u1 = SB("u1", [128, 8, 128], F32); u2 = SB("u2", [128, 8, 128], F32)
        v1 = SB("v1", [128, 8, 128], F32); v2 = SB("v2", [128, 8, 128], F32)
        Cxr = SB("Cxr", [128, 8, 128], BF16); Cxi = SB("Cxi", [128, 8, 128], BF16)
        Pkr = [SB("Pkr%d" % i, [128, 8, 128], BF16) for i in range(2)]
        Pki = [SB("Pki%d" % i, [128, 8, 128], BF16) for i in range(2)]
        CLr = [SB("CLr%d" % i, [128, 8, 128], BF16) for i in range(2)]
        CLi = [SB("CLi%d" % i, [128, 8, 128], BF16) for i in range(2)]
        winst = [SB("winst%d" % i, [128, 16, 128], BF16) for i in range(2)]
        wlags = SB("wlags", [128, 31, 128], BF16)
        ddiag = SB("ddiag", [128, 128], F32)
        ptk = [PS("ptk%d" % i, [128, 512], F32) for i in range(2)]
        ptr = [PS("ptr%d" % i, [128, 8, 128], BF16) for i in range(2)]

        def bc(tile, off, pstep):
            return AP(tile, off, [[pstep, 128], [1, 8], [0, 128]])
        for T in range(4):
            P.dma('sp', bre[:], bA[0, T], 'c_bre', w=['bre']); P.dma('sp', bim[:], bA[1, T], 'c_bim', w=['bim'])
            P.dma('sp', cre[:], cA[0, T], 'c_cre', w=['cre']); P.dma('sp', cim[:], cA[1, T], 'c_cim', w=['cim'])
            crb = bc(T_['cr'], T * 8, 32); cib = bc(T_['ci'], T * 8, 32)
            P.op('dve', lambda e, crb=crb: e.tensor_tensor(out=u1[:], in0=bre[:], in1=crb, op=ALU.mult), r=['bre', 'cr'], w=['u1'])
            P.op('dve', lambda e, cib=cib: e.tensor_tensor(out=u2[:], in0=bim[:], in1=cib, op=ALU.mult), r=['bim', 'ci'], w=['u2'])
            P.op('dve', lambda e: e.tensor_tensor(out=Br[:], in0=u1[:], in1=u2[:], op=ALU.subtract), r=['u1', 'u2'], w=['Br'])
            P.op('dve', lambda e, crb=crb: e.tensor_tensor(out=u1[:], in0=bim[:], in1=crb, op=ALU.mult), r=['bim', 'cr'], w=['u1'])
            P.op('dve', lambda e, cib=cib: e.tensor_tensor(out=u2[:], in0=bre[:], in1=cib, op=ALU.mult), r=['bre', 'ci'], w=['u2'])
            P.op('dve', lambda e: e.tensor_tensor(out=Bi[:], in0=u1[:], in1=u2[:], op=ALU.add), r=['u1', 'u2'], w=['Bi'])
            P.op('pool', lambda e: e.tensor_copy(out=Cxr[:], in_=cre[:]), r=['cre'], w=['Cxr'])
            P.op('pool', lambda e: e.tensor_scalar(out=Cxi[:], in0=cim[:], scalar1=-1.0, scalar2=None, op0=ALU.mult), r=['cim'], w=['Cxi'])
            P.op('dve', lambda e, T=T: e.tensor_scalar(out=ddiag[:], in0=identf[:], scalar1=dDs[:, T:T + 1], scalar2=None, op0=ALU.mult),
                 r=['identf', 'dD'], w=['ddiag'])
            for k in range(17):
                i2 = k % 2
                pkr = bc(prt, k * 32 + T * 8, 17 * 32); pki = bc(pit, k * 32 + T * 8, 17 * 32)
                if k <= 15:
                    P.op('dve', lambda e, pkr=pkr: e.tensor_tensor(out=u1[:], in0=Br[:], in1=pkr, op=ALU.mult), r=['Br', 'prt'], w=['u1'])
                    P.op('dve', lambda e, pki=pki: e.tensor_tensor(out=u2[:], in0=Bi[:], in1=pki, op=ALU.mult), r=['Bi', 'pit'], w=['u2'])
                    P.op('dve', lambda e, i2=i2: e.tensor_tensor(out=Pkr[i2][:], in0=u1[:], in1=u2[:], op=ALU.subtract), r=['u1', 'u2'], w=['Pkr%d' % i2])
                    P.op('dve', lambda e, pki=pki: e.tensor_tensor(out=u1[:], in0=Br[:], in1=pki, op=ALU.mult), r=['Br', 'pit'], w=['u1'])
                    P.op('dve', lambda e, pkr=pkr: e.tensor_tensor(out=u2[:], in0=Bi[:], in1=pkr, op=ALU.mult), r=['Bi', 'prt'], w=['u2'])
                    P.op('dve', lambda e, i2=i2: e.tensor_tensor(out=Pki[i2][:], in0=u1[:], in1=u2[:], op=ALU.add), r=['u1', 'u2'], w=['Pki%d' % i2])
                    if k == 0:
                        def tk0(e, i2=i2):
                            n = 0
                            for dr in range(2):
                                for q in range(4):
                                    for (Pt, Ct) in ((Pkr[i2], Cxr), (Pki[i2], Cxi)):
                                        ins = e.matmul(ptk[0][:, 0:128], lhsT=Pt[:, q * 2 + dr, :], rhs=Ct[:, q * 2 + dr, :], start=(n == 0), stop=(n == 15))
                                        n += 1
                            return ins
                        P.op('pe', tk0, r=['Pkr%d' % i2, 'Pki%d' % i2, 'Cxr', 'Cxi'], w=['ptk0'])
                        P.op('dve', lambda e: e.tensor_tensor(out=wlags[:, 15, :], in0=ptk[0][:, 0:128], in1=ddiag[:], op=ALU.add),
                             r=['ptk0', 'ddiag'], w=['wlags'])
                    else:
                        for dr in range(2):
                            def tk(e, i2=i2, dr=dr):
                                n = 0
                                for q in range(4):
                                    for (Pt, Ct) in ((Pkr[i2], Cxr), (Pki[i2], Cxi)):
                                        ins = e.matmul(ptk[dr][:, 0:128], lhsT=Pt[:, q * 2 + dr, :], rhs=Ct[:, q * 2 + dr, :], start=(n == 0), stop=(n == 7))
                                        n += 1
                                return ins
                            P.op('pe', tk, r=['Pkr%d' % i2, 'Pki%d' % i2, 'Cxr', 'Cxi'], w=['ptk%d' % dr])
                            li_ = 15 + k if dr == 0 else 15 - k
                            P.op('act', lambda e, dr=dr, li_=li_: e.copy(out=wlags[:, li_, :], in_=ptk[dr][:, 0:128]), r=['ptk%d' % dr], w=['wlags'])
                    for h in range(2):
                        def trw(e, i2=i2, h=h):
                            for m in range(8):
                                idx = h * 8 + m
                                cb, ri = idx // 2, idx % 2
                                ins = e.transpose(out=ptr[h][:, m, :], in_=(Pkr[i2] if ri == 0 else Pki[i2])[:, cb, :], identity=identb[:])
                            return ins
                        P.op('pe', trw, r=['Pkr%d' % i2, 'Pki%d' % i2, 'identb'], w=['ptr%d' % h])
                        P.op('act', lambda e, i2=i2, h=h: e.copy(out=winst[i2][:, h * 8:(h + 1) * 8, :], in_=ptr[h][:]), r=['ptr%d' % h], w=['winst%d' % i2])
                    P.dma('sp', WIN_d[T, k], winst[i2][:], 'wino%d' % i2, r=['winst%d' % i2], w=['winst%d' % i2])
                if k >= 1:
                    P.op('dve', lambda e, pkr=pkr: e.tensor_tensor(out=u1[:], in0=cre[:], in1=pkr, op=ALU.mult), r=['cre', 'prt'], w=['u1'])
                    P.op('dve', lambda e, pki=pki: e.tensor_tensor(out=u2[:], in0=cim[:], in1=pki, op=ALU.mult), r=['cim', 'pit'], w=['u2'])
                    P.op('dve', lambda e, i2=i2: e.tensor_tensor(out=CLr[i2][:], in0=u1[:], in1=u2[:], op=ALU.subtract), r=['u1', 'u2'], w=['CLr%d' % i2])
                    P.op('pool', lambda e, pki=pki: e.tensor_tensor(out=v1[:], in0=cre[:], in1=pki, op=ALU.mult), r=['cre', 'pit'], w=['v1'])
                    P.op('pool', lambda e, pkr=pkr: e.tensor_tensor(out=v2[:], in0=cim[:], in1=pkr, op=ALU.mult), r=['cim', 'prt'], w=['v2'])
                    P.op('pool', lambda e: e.tensor_tensor(out=v1[:], in0=v1[:], in1=v2[:], op=ALU.add), r=['v1', 'v2'], w=['v1'])
                    P.op('pool', lambda e, i2=i2: e.tensor_scalar(out=CLi[i2][:], in0=v1[:], scalar1=-1.0, scalar2=None, op0=ALU.mult), r=['v1'], w=['CLi%d' % i2])
                    wd = WOUT_d[T, k - 1].rearrange("p (c r) m -> p c r m", r=2)
                    P.dma('sp', wd[:, :, 0, :], CLr[i2][:], 'wouto%d' % i2, r=['CLr%d' % i2], w=['CLr%d' % i2])
                    P.dma('sp', wd[:, :, 1, :], CLi[i2][:], 'woutp%d' % i2, r=['CLi%d' % i2], w=['CLi%d' % i2])
            P.dma('sp', WLAG_d[T], wlags[:], 'wlago', r=['wlags'], w=['wlags'])
        P.barrier()
    P.new_sems()
    if upto < 4:
        return nc, P, {}
    UPW = 7968
    for b in range(NB):
        for T in range(4):
            if quick and (b > 0 or T > 0):
                continue
            with ExitStack() as st:
                def SB(name, shape, dt, b=b, T=T):
                    return st.enter_context(nc.sbuf_tensor("%s_%d_%d" % (name, b, T), list(shape), dt))

                def PS(name, shape, dt=F32, b=b, T=T):
                    return st.enter_context(nc.psum_tensor("%s_%d_%d" % (name, b, T), list(shape), dt))
                zs = SB("zs", [128, S], BF16)
                upad = SB("upad", [128, UPW], BF16)
                wlag = SB("wlag", [128, 31, 128], BF16)
                Hs = [SB("Hs%d" % i, [128, 257, 16], F32) for i in range(2)]
                Xh = SB("Xh", [128, 4, 2, 2, 256], BF16)
                Ssb = SB("Ssb", [128, 2, 256, 8], F32)
                win = SB("win", [128, 16, 4, 128], BF16)
                wout = SB("wout_s", [128, 16, 16, 128], BF16)
                A12 = SB("A12", [128, 2, 16], F32)
                tP = [SB("tP%d" % i, [128, 16], F32) for i in range(2)]
                tQ = [SB("tQ%d" % i, [128, 8], F32) for i in range(2)]
                ygs = [SB("ygs%d" % i, [128, 512], BF16) for i in range(2)]
                ysum = [SB("ysum%d" % i, [128, 512], F32) for i in range(2)]
                crs = SB("crs", [128, 16, 128], F32)
                pss = [PS("pss%d" % i, [128, 512], F32) for i in range(4)]
                py = [PS("py%d" % i, [128, 512], F32) for i in range(2)]

                P.dma('sp', zs[:], zS_d[b, T], 'zs', w=['zs'])
                P.dma('sp', wlag[:], WLAG_d[T], 'wlag', w=['wlag'])
                P.dma('sp', wout[:], WOUT_d[T].rearrange("k p m c -> p k m c"), 'wout', w=['wout'])
                P.op('pool', lambda e: e.memset(upad[:], 0.0), w=['upad'])
                P.op('pool', lambda e: e.tensor_copy(out=AP(upad, 15, [[UPW, 128], [31, 256], [1, 16]]),
                                                     in_=AP(zs, 0, [[S, 128], [16, 256], [1, 16]])), r=['zs'], w=['upad'])
                for dr in range(2):
                    src_r = AP(prt, 16 * 32 + T * 8 + dr, [[17 * 32, 128], [2, 4]])
                    src_i = AP(pit, 16 * 32 + T * 8 + dr, [[17 * 32, 128], [2, 4]])
                    P.op('dve', lambda e, dr=dr, src_r=src_r: e.tensor_copy(out=A12[:, dr, 0:4], in_=src_r), r=['prt'], w=['A12'])
                    P.op('dve', lambda e, dr=dr, src_r=src_r: e.tensor_copy(out=A12[:, dr, 4:8], in_=src_r), r=['prt'], w=['A12'])
                    P.op('dve', lambda e, dr=dr, src_i=src_i: e.tensor_scalar(out=A12[:, dr, 8:12], in0=src_i, scalar1=-1.0, scalar2=None, op0=ALU.mult), r=['pit'], w=['A12'])
                    P.op('dve', lambda e, dr=dr, src_i=src_i: e.tensor_copy(out=A12[:, dr, 12:16], in_=src_i), r=['pit'], w=['A12'])
                P.op('dve', lambda e: e.memset(Hs[0][:], 0.0), w=['H0'])
                P.op('pool', lambda e: e.memset(Hs[1][:], 0.0), w=['H1'])
                for q in range(4):
                    P.dma('sp', win[:], WIN_d[T, :, :, q * 4:(q + 1) * 4, :].rearrange("k p m c -> p k m c"), 'win', w=['win'])
                    for dr in range(2):
                        for ri in range(2):
                            pi_ = dr * 2 + ri

                            def smm(e, dr=dr, ri=ri, pi_=pi_):
                                for jp in range(16):
                                    kk = 15 - jp if dr == 0 else jp
                                    ins = e.matmul(pss[pi_][:, 0:256], lhsT=win[:, kk, dr * 2 + ri, :],
                                                   rhs=AP(upad, 15 + jp, [[UPW, 128], [31, 256]]), start=(jp == 0), stop=(jp == 15))
                                return ins
                            P.op('pe', smm, r=['win', 'upad'], w=['pss%d' % pi_])
                            P.op('act', lambda e, dr=dr, ri=ri, q=q, pi_=pi_: e.copy(out=AP(Ssb, dr * 2048 + ri * 4 + q, [[4096, 128], [8, 256]]),
                                                                                     in_=pss[pi_][:, 0:256]), r=['pss%d' % pi_], w=['Ssb'])
                for step in range(256 if not quick else 256):
                    for dr, eng in ((0, 'dve'), (1, 'pool')):
                        H = Hs[dr]
                        if dr == 0:
                            src, dst, sc_ = step, step + 1, step
                        else:
                            src, dst, sc_ = 256 - step, 255 - step, 255 - step
                        hk = 'H%d' % dr
                        HW = 257 * 16
                        P.op(eng, lambda e, H=H, src=src, dr=dr, HW=HW: e.tensor_tensor(out=AP(tP[dr], 0, [[16, 128], [8, 2], [1, 8]]),
                                                                                 in0=AP(H, src * 16, [[HW, 128], [4, 2], [1, 8]]),
                                                                                 in1=AP(A12, dr * 16, [[32, 128], [8, 2], [1, 8]]), op=ALU.mult),
                             r=[hk, 'A12'], w=['tP%d' % dr])
                        P.op(eng, lambda e, dr=dr: e.tensor_tensor(out=tQ[dr][:], in0=tP[dr][:, 0:8], in1=tP[dr][:, 8:16], op=ALU.add),
                             r=['tP%d' % dr], w=['tQ%d' % dr])
                        P.op(eng, lambda e, H=H, dst=dst, dr=dr, sc_=sc_, HW=HW: e.tensor_tensor(out=AP(H, dst * 16, [[HW, 128], [8, 2], [1, 8]]),
                                                                                          in0=AP(tQ[dr], 0, [[8, 128], [0, 2], [1, 8]]),
                                                                                          in1=AP(Ssb, dr * 2048 + sc_ * 8, [[4096, 128], [0, 2], [1, 8]]), op=ALU.add),
                             r=['tQ%d' % dr, 'Ssb'], w=[hk])
                P.op('dve', lambda e: e.tensor_copy(out=AP(Xh, 0, [[4096, 128], [1024, 4], [256, 2], [1, 256]]),
                                                    in_=AP(Hs[0], 0, [[257 * 16, 128], [1, 4], [4, 2], [16, 256]])), r=['H0'], w=['Xh'])
                P.op('pool', lambda e: e.tensor_copy(out=AP(Xh, 512, [[4096, 128], [1024, 4], [256, 2], [1, 256]]),
                                                     in_=AP(Hs[1], 16, [[257 * 16, 128], [1, 4], [4, 2], [16, 256]])), r=['H1'], w=['Xh'])
                for half in range(2):
                    h0 = half * 128
                    for g4 in range(4):
                        def cmm(e, g4=g4, h0=h0):
                            for jj in range(4):
                                j = g4 * 4 + jj
                                n = 0
                                for q in range(4):
                                    for dr in range(2):
                                        kidx = j if dr == 0 else 15 - j
                                        for ri in range(2):
                                            ins = e.matmul(pss[g4][:, jj * 128:(jj + 1) * 128], lhsT=wout[:, kidx, q * 4 + dr * 2 + ri, :],
                                                           rhs=Xh[:, q, dr, ri, h0:h0 + 128], start=(n == 0), stop=(n == 15))
                                            n += 1
                            return ins
                        P.op('pe', cmm, r=['wout', 'Xh'], w=['pss%d' % g4])
                        P.op('act', lambda e, g4=g4: e.copy(out=crs[:, g4 * 4:(g4 + 1) * 4, :], in_=pss[g4][:, :]), r=['pss%d' % g4], w=['crs'])
                    for t4 in range(4):
                        tt_ = half * 4 + t4
                        c0 = tt_ * 32
                        ip = tt_ % 2

                        def ymm(e, c0=c0, ip=ip):
                            outv = AP(py[ip], 0, [[512, 128], [16, 32], [1, 16]])
                            for li_ in range(31):
                                dl = li_ - 15
                                ins = e.matmul(outv, lhsT=wlag[:, li_, :], rhs=AP(upad, 15 + c0 * 31 - dl, [[UPW, 128], [31, 32], [1, 16]]),
                                               start=(li_ == 0), stop=(li_ == 30))
                            return ins
                        P.op('pe', ymm, r=['wlag', 'upad'], w=['py%d' % ip])
                        P.op('dve', lambda e, ip=ip, t4=t4: e.tensor_tensor(out=AP(ysum[ip], 0, [[512, 128], [16, 32], [1, 16]]),
                                                                            in0=AP(py[ip], 0, [[512, 128], [16, 32], [1, 16]]),
                                                                            in1=AP(crs, t4 * 32, [[2048, 128], [1, 32], [128, 16]]), op=ALU.add),
                             r=['py%d' % ip, 'crs'], w=['ysum%d' % ip])
                        P.op('act', lambda e, ip=ip: e.activation(out=ygs[ip][:], in_=ysum[ip][:], func=AF.Gelu), r=['ysum%d' % ip], w=['ygs%d' % ip])
                        P.dma('sp', yG_d[b, T, :, tt_ * 512:(tt_ + 1) * 512], ygs[ip][:], 'ygo%d' % ip, r=['ygs%d' % ip], w=['ygs%d' % ip])
                P.barrier()
            P.new_sems()
    if upto < 5:
        return nc, P, {}
    with ExitStack() as st:
        def SB(name, shape, dt):
            return st.enter_context(nc.sbuf_tensor(name, list(shape), dt))

        def PS(name, shape, dt=F32):
            return st.enter_context(nc.psum_tensor(name, list(shape), dt))
        stg = SB("stg", [128, 8, D], F32)
        wglub = SB("wglub", [128, 4, 512], BF16)
        woutb = SB("woutb", [128, 8, D], BF16)
        P.dma('sp', AP(stg, 0, [[8 * D, 128], [1, 2048]]), wglu.rearrange("p t n -> p (t n)"), 'c_wglu', w=['stg'])
        P.op('dve', lambda e: e.tensor_copy(out=wglub[:], in_=AP(stg, 0, [[8 * D, 128], [512, 4], [1, 512]])), r=['stg'], w=['wglub'])
        P.dma('sp', stg[:], woutw[:, :, :], 'c_wout', r=['wglub'], w=['stg'])
        P.op('dve', lambda e: e.tensor_copy(out=woutb[:], in_=stg[:]), r=['stg'], w=['woutb'])
        ygb = [SB("ygb%d" % i, [128, 4, 128], BF16) for i in range(2)]
        yfb = [SB("yfb%d" % i, [128, 4, 128], BF16) for i in range(2)]
        xs = [SB("xsd%d" % i, [128, D], F32) for i in range(2)]
        sg = SB("sg", [128, 4, 128], BF16)
        ysg = [SB("ysg%d" % i, [128, 4, 128], BF16) for i in range(2)]
        x1s = [SB("x1s%d" % i, [128, D], F32) for i in range(2)]
        sq = SB("sqd", [128, D], F32)
        ss = [SB("ssd%d" % i, [128, 4], F32) for i in range(2)]
        xn2 = [SB("xn2%d" % i, [128, D], BF16) for i in range(2)]
        hT2 = [SB("hT2%d" % i, [128, 8, 128], BF16) for i in range(2)]
        pg = PS("pg", [128, 4, 128], F32)
        po = [PS("pod%d" % i, [128, 512], F32) for i in range(2)]
        pT2 = PS("pT2", [128, 8, 128], BF16)
        nblk = NB * (S // 128)
        def d_s1(blk):
                b, tb_ = divmod(blk, S // 128)
                i = blk % 2
                t0 = tb_ * 128
                P.dma('sp', ygb[i][:], yG_d[b, :, :, t0:t0 + 128].rearrange("t p s -> p t s"), 'ygb%d' % i, w=['ygb%d' % i])
                P.dma('sp', yfb[i][:], yF_d[b, :, :, t0:t0 + 128].rearrange("t p s -> p t s"), 'yfb%d' % i, w=['yfb%d' % i])
                P.dma('sp', xs[i][:], x[b, t0:t0 + 128, :], 'xsd%d' % i, w=['xsd%d' % i])

                def glu(e, i=i):
                    for n in range(4):
                        for T in range(4):
                            ins = e.matmul(pg[:, n, :], lhsT=wglub[:, T, n * 128:(n + 1) * 128], rhs=ygb[i][:, T, :], start=(T == 0), stop=(T == 3))
                    return ins
                P.op('pe', glu, r=['wglub', 'ygb%d' % i], w=['pg'])
                P.op('act', lambda e: e.activation(out=sg[:], in_=pg[:], func=AF.Sigmoid), r=['pg'], w=['sg'])
                P.op('dve', lambda e, i=i: e.tensor_tensor(out=ysg[i][:], in0=sg[:], in1=ygb[i][:], op=ALU.mult), r=['sg', 'ygb%d' % i], w=['ysg%d' % i])

        def d_s2(blk):
                b, tb_ = divmod(blk, S // 128)
                i = blk % 2
                t0 = tb_ * 128
                for hf in range(2):
                    def omm(e, i=i, hf=hf):
                        for c8 in range(8):
                            lt = ysg[i][:, c8, :] if c8 < 4 else yfb[i][:, c8 - 4, :]
                            ins = e.matmul(po[hf][:, :], lhsT=lt, rhs=woutb[:, c8, hf * 512:(hf + 1) * 512], start=(c8 == 0), stop=(c8 == 7))
                        return ins
                    P.op('pe', omm, r=['ysg%d' % i, 'yfb%d' % i, 'woutb'], w=['pod%d' % hf])
                    P.op('dve', lambda e, i=i, hf=hf: e.tensor_tensor(out=x1s[i][:, hf * 512:(hf + 1) * 512], in0=po[hf][:, :], in1=xs[i][:, hf * 512:(hf + 1) * 512], op=ALU.add),
                         r=['pod%d' % hf, 'xsd%d' % i], w=['x1s%d_%d' % (i, hf)])
                P.dma('sp', x1_d[b, t0:t0 + 128, :], x1s[i][:], 'x1o%d' % i, r=['x1s%d_0' % i, 'x1s%d_1' % i], w=['x1o%d' % i])
                P.op('act', lambda e, i=i: e.activation(out=sq[:], in_=x1s[i][:], func=AF.Square, accum_out=ss[i][:, 0:1]),
                     r=['x1s%d_0' % i, 'x1s%d_1' % i], w=['sqd', 'ssa%d' % i])
                P.op('act', lambda e, i=i: e.activation(out=ss[i][:, 1:2], in_=ss[i][:, 0:1], func=AF.Sqrt, scale=1.0 / D, bias=epsc[:, 0:1]),
                     r=['ssa%d' % i, 'epsc'], w=['ssb%d' % i])
                P.op('dve', lambda e, i=i: e.reciprocal(out=ss[i][:, 2:3], in_=ss[i][:, 1:2]), r=['ssb%d' % i], w=['ssc%d' % i])
                P.op('dve', lambda e, i=i: e.tensor_scalar(out=xn2[i][:], in0=x1s[i][:], scalar1=ss[i][:, 2:3], scalar2=None, op0=ALU.mult),
                     r=['x1s%d_0' % i, 'x1s%d_1' % i, 'ssc%d' % i], w=['xn2%d' % i])


        def d_s3(blk):
                b, tb_ = divmod(blk, S // 128)
                i = blk % 2
                t0 = tb_ * 128
                def tr2(e, i=i):
                    for kc in range(8):
                        ins = e.transpose(out=pT2[:, kc, :], in_=xn2[i][:, kc * 128:(kc + 1) * 128], identity=identb[:])
                    return ins
                P.op('pe', tr2, r=['xn2%d' % i, 'identb'], w=['pT2'])
                P.op('act', lambda e, i=i: e.copy(out=hT2[i][:], in_=pT2[:]), r=['pT2'], w=['hT2%d' % i])
                P.dma('sp', h2T_d[b, :, :, t0:t0 + 128], hT2[i][:], 'h2o%d' % i, r=['hT2%d' % i], w=['hT2%d' % i])


        nbd = nblk if not quick else 2
        for step in range(nbd + 2):
            if step < nbd:
                d_s1(step)
            if 0 <= step - 1 < nbd:
                d_s2(step - 1)
            if 0 <= step - 2 < nbd:
                d_s3(step - 2)
        P.barrier()
    P.new_sems()
    if upto < 6:
        return nc, P, {}
    keysT_in = din("keysT_in", [128, 16, 128])
    iota128_in = din("iota128", [128, 128])
    iota16_in = din("iota16", [128, 16])
    G_d = dscr("G_d", [64, 128, 128, 128])
    wqb = SBp("wqb", [128, 8, 2048], BF16)
    keysb = SBp("keysb", [128, 16, 128], BF16)
    iota128 = SBp("iota128s", [128, 128], F32)
    iota16 = SBp("iota16s", [128, 16], F32)
    P.dma('sp', iota128[:], iota128_in[:, :], 'c_io128', w=['iota128'])
    P.dma('sp', iota16[:], iota16_in[:, :], 'c_io16', w=['iota16'])
    with ExitStack() as st:
        def SB(name, shape, dt):
            return st.enter_context(nc.sbuf_tensor(name, list(shape), dt))
        stq = SB("stq", [128, 4, 2048], F32)
        kst = SB("kst", [128, 16, 128], F32)
        P.dma('sp', kst[:], keysT_in[:, :, :], 'c_keys', w=['kst'])
        for hq in range(2):
            P.dma('sp', stq[:], wq[:, hq * 4:(hq + 1) * 4, :], 'c_wq', w=['stq'])
            for kk in range(4):
                kc = hq * 4 + kk
                P.op('dve', lambda e, kc=kc, kk=kk: e.tensor_scalar(out=wqb[:, kc, :], in0=stq[:, kk, :], scalar1=g2s[:, kc:kc + 1], scalar2=None, op0=ALU.mult),
                     r=['stq', 'g2s'], w=['wqb'])
        P.op('pool', lambda e: e.tensor_copy(out=keysb[:], in_=kst[:]), r=['kst'], w=['keysb'])
        P.barrier()
    P.new_sems()

    with ExitStack() as st:
        def SB(name, shape, dt):
            return st.enter_context(nc.sbuf_tensor(name, list(shape), dt))

        def PS(name, shape, dt=F32):
            return st.enter_context(nc.psum_tensor(name, list(shape), dt))
        NT = 128
        h2s = [SB("h2_%d" % i, [128, 8, NT], BF16) for i in range(2)]
        qTs = [SB("qT_%d" % i, [128, 16, NT], BF16) for i in range(2)]
        scss = [SB("scs_%d" % i, [128, 16, 128], F32) for i in range(2)]
        scr = SB("scr", [128, 256], F32)
        v16 = SB("v16", [128, 16, 16], F32)
        ix16 = SB("ix16", [128, 16, 16], U32)
        ixf = SB("ixf", [128, 16, 16], F32)
        cand = SB("cand", [128, 8, 256], F32)
        tv = SB("tv", [128, 8, 16], F32)
        tve = SB("tve", [128, 8, 16], F32)
        pos = SB("pos", [128, 8, 16], U32)
        posf = SB("posf", [128, 8, 16], F32)
        paf = SB("paf", [128, 8, 16], F32); pbf = SB("pbf", [128, 8, 16], F32)
        eq = SB("eq", [128, 8, 16, 16], F32)
        i16a = SB("i16a", [128, 16], F32); i16b = SB("i16b", [128, 16], F32)
        sel = SB("sel", [128, 3, 128], F32)
        selb = SB("selb", [128, 3, 128], BF16)
        selT = SB("selT", [128, 3, 128], BF16)
        iob = SB("iob", [128, 128], BF16)
        zsum = SB("zsum", [128, 8], F32)
        OJs = [SB("OJ%d" % i, [128, 32, 128], BF16) for i in range(2)]
        OIs = [SB("OI%d" % i, [128, 32, 128], BF16) for i in range(2)]
        Gsb = [SB("Gs%d" % i, [128, 128, NT], BF16) for i in range(2)]
        Bk = [PS("Bk%d" % i, [128, 512], F32) for i in range(7)]
        Bk7b = PS("Bk7b", [128, 1024], BF16)
        eq2 = cand

        def bk(i):
            return 'Bk%d' % i
        P.op('dve', lambda e: e.tensor_copy(out=iob[:], in_=iota128[:]), r=['iota128'], w=['iob'])
        P.op('dve', lambda e: e.tensor_scalar(out=i16a[:], in0=iota16[:], scalar1=16.0, scalar2=None, op0=ALU.mult), r=['iota16'], w=['i16a'])
        P.op('dve', lambda e: e.tensor_scalar(out=i16b[:], in0=iota16[:], scalar1=16.0, scalar2=16.0, op0=ALU.mult, op1=ALU.add), r=['iota16'], w=['i16b'])
        nblk = NB * S // NT
        def front(blk):
                b, tb_ = divmod(blk, S // NT)
                t0 = tb_ * NT
                par = blk % 2
                h2 = h2s[par]; qT = qTs[par]; scs = scss[par]
                Gs = Gsb[blk % 2]
                gsk = 'Gs%d' % (blk % 2)
                P.dma('sp', h2[:], h2T_d[b, :, :, t0:t0 + NT], 'h2_%d' % par, w=['h2_%d' % par])
                for m in range(16):
                    pb_ = 4 + (m % 2)

                    def qmm(e, m=m, pb_=pb_):
                        for kc in range(8):
                            ins = e.matmul(Bk[pb_][:, 0:NT], lhsT=wqb[:, kc, m * 128:(m + 1) * 128], rhs=h2[:, kc, :], start=(kc == 0), stop=(kc == 7))
                        return ins
                    P.op('pe', qmm, r=['wqb', 'h2_%d' % par], w=[bk(pb_)])
                    P.op('act', lambda e, m=m, pb_=pb_: e.copy(out=qT[:, m, :], in_=Bk[pb_][:, 0:NT]), r=[bk(pb_)], w=['qT_%d' % par])
                for m4 in range(4):
                    def smm2(e, m4=m4):
                        for mm in range(4):
                            m = m4 * 4 + mm
                            ins = e.matmul(Bk[m4][:, mm * 128:(mm + 1) * 128], lhsT=qT[:, m, :], rhs=keysb[:, m, :], start=True, stop=True)
                        return ins
                    P.op('pe', smm2, r=['qT_%d' % par, 'keysb'], w=[bk(m4)])
                    P.op('act', lambda e, m4=m4: e.copy(out=scs[:, m4 * 4:(m4 + 1) * 4, :], in_=Bk[m4][:, :]), r=[bk(m4)], w=['scs_%d' % par])

        def mid_a(blk):
                b, tb_ = divmod(blk, S // NT)
                t0 = tb_ * NT
                par = blk % 2
                h2 = h2s[par]; qT = qTs[par]; scs = scss[par]
                for m in range(16):
                    P.op('dve', lambda e, m=m: e.max(out=v16[:, m, 0:8], in_=scs[:, m, :]), r=['scs_%d' % par], w=['v16'])
                    P.op('dve', lambda e, m=m: e.max_index(out=ix16[:, m, 0:8], in_max=v16[:, m, 0:8], in_values=scs[:, m, :]), r=['scs_%d' % par, 'v16'], w=['ix16'])
                    P.op('dve', lambda e, m=m: e.match_replace(out=scr[:, 0:128], in_to_replace=v16[:, m, 0:8], in_values=scs[:, m, :], imm_value=-1e30),
                         r=['scs_%d' % par, 'v16'], w=['scr'])
                    P.op('dve', lambda e, m=m: e.max(out=v16[:, m, 8:16], in_=scr[:, 0:128]), r=['scr'], w=['v16'])
                    P.op('dve', lambda e, m=m: e.max_index(out=ix16[:, m, 8:16], in_max=v16[:, m, 8:16], in_values=scr[:, 0:128]), r=['scr', 'v16'], w=['ix16'])
                P.op('dve', lambda e: e.tensor_copy(out=ixf[:], in_=ix16[:]), r=['ix16'], w=['ixf'])
                P.op('dve', lambda e: e.tensor_tensor(out=AP(cand, 0, [[2048, 128], [256, 8], [16, 16], [1, 16]]),
                                                      in0=AP(v16, 0, [[256, 128], [32, 8], [1, 16], [0, 16]]),
                                                      in1=AP(v16, 16, [[256, 128], [32, 8], [0, 16], [1, 16]]), op=ALU.add), r=['v16'], w=['cand'])
                for h in range(8):
                    P.op('dve', lambda e, h=h: e.max(out=tv[:, h, 0:8], in_=cand[:, h, :]), r=['cand'], w=['tv'])
                    P.op('dve', lambda e, h=h: e.max_index(out=pos[:, h, 0:8], in_max=tv[:, h, 0:8], in_values=cand[:, h, :]), r=['cand', 'tv'], w=['pos'])
                    P.op('dve', lambda e, h=h: e.match_replace(out=scr[:, 0:256], in_to_replace=tv[:, h, 0:8], in_values=cand[:, h, :], imm_value=-1e30),
                         r=['cand', 'tv'], w=['scr'])
                    P.op('dve', lambda e, h=h: e.max(out=tv[:, h, 8:16], in_=scr[:, 0:256]), r=['scr'], w=['tv'])
                    P.op('dve', lambda e, h=h: e.max_index(out=pos[:, h, 8:16], in_max=tv[:, h, 8:16], in_values=scr[:, 0:256]), r=['scr', 'tv'], w=['pos'])

        def gate_(blk):
                b, tb_ = divmod(blk, S // NT)
                t0 = tb_ * NT
                par = blk % 2
                h2 = h2s[par]; qT = qTs[par]; scs = scss[par]
                P.op('dve', lambda e: e.tensor_tensor(out=tve[:], in0=tv[:], in1=AP(tv, 0, [[128, 128], [16, 8], [0, 16]]), op=ALU.subtract), r=['tv'], w=['tve'])
                P.op('act', lambda e: e.activation(out=tve[:], in_=tve[:], func=AF.Exp), r=['tve'], w=['tve'])

        def mid_b(blk):
                b, tb_ = divmod(blk, S // NT)
                t0 = tb_ * NT
                par = blk % 2
                h2 = h2s[par]; qT = qTs[par]; scs = scss[par]
                P.op('dve', lambda e: e.tensor_copy(out=posf[:], in_=pos[:]), r=['pos'], w=['posf'])
                posb = AP(posf, 0, [[128, 128], [16, 8], [1, 16], [0, 16]])
                P.op('dve', lambda e, posb=posb: e.tensor_tensor(out=eq[:], in0=posb, in1=AP(i16a, 0, [[16, 128], [0, 8], [0, 16], [1, 16]]), op=ALU.is_ge),
                     r=['posf', 'i16a'], w=['eq'])
                P.op('dve', lambda e, posb=posb: e.tensor_tensor(out=eq2[:].rearrange("p h (a b) -> p h a b", b=16) if False else AP(cand, 0, [[2048, 128], [256, 8], [16, 16], [1, 16]]),
                                                                in0=posb, in1=AP(i16b, 0, [[16, 128], [0, 8], [0, 16], [1, 16]]), op=ALU.is_ge),
                     r=['posf', 'i16b', 'pos'], w=['cand'])
                P.op('dve', lambda e: e.tensor_tensor(out=eq[:], in0=eq[:], in1=AP(cand, 0, [[2048, 128], [256, 8], [16, 16], [1, 16]]), op=ALU.subtract), r=['eq', 'cand'], w=['eq'])
                c4 = AP(cand, 0, [[2048, 128], [256, 8], [16, 16], [1, 16]])
                c3 = AP(cand, 0, [[2048, 128], [16, 128], [1, 16]])
                P.op('dve', lambda e, c4=c4: e.tensor_tensor(out=c4, in0=eq[:], in1=AP(ixf, 0, [[256, 128], [32, 8], [0, 16], [1, 16]]), op=ALU.mult), r=['eq', 'ixf'], w=['cand'])
                P.op('dve', lambda e, c3=c3: e.tensor_reduce(out=sel[:, 0, :], in_=c3, axis=AX.X, op=ALU.add), r=['cand'], w=['sel'])
                P.op('dve', lambda e, c4=c4: e.tensor_tensor(out=c4, in0=eq[:], in1=AP(iota16, 0, [[16, 128], [0, 8], [0, 16], [1, 16]]), op=ALU.mult), r=['eq', 'iota16'], w=['cand'])
                P.op('dve', lambda e, c3=c3: e.tensor_reduce(out=paf[:], in_=c3, axis=AX.X, op=ALU.add), r=['cand'], w=['paf'])
                P.op('dve', lambda e: e.scalar_tensor_tensor(out=pbf[:], in0=paf[:], scalar=-16.0, in1=posf[:], op0=ALU.mult, op1=ALU.add), r=['paf', 'posf'], w=['pbf'])
                P.op('dve', lambda e: e.tensor_tensor(out=eq[:], in0=AP(iota16, 0, [[16, 128], [0, 8], [0, 16], [1, 16]]),
                                                      in1=AP(pbf, 0, [[128, 128], [16, 8], [1, 16], [0, 16]]), op=ALU.is_equal), r=['iota16', 'pbf'], w=['eq'])
                P.op('dve', lambda e, c4=c4: e.tensor_tensor(out=c4, in0=eq[:], in1=AP(ixf, 16, [[256, 128], [32, 8], [0, 16], [1, 16]]), op=ALU.mult), r=['eq', 'ixf'], w=['cand'])
                P.op('dve', lambda e, c3=c3: e.tensor_reduce(out=sel[:, 1, :], in_=c3, axis=AX.X, op=ALU.add), r=['cand'], w=['sel'])
                P.op('dve', lambda e: e.tensor_reduce(out=zsum[:], in_=tve[:], axis=AX.X, op=ALU.add), r=['tve'], w=['zsum'])
                P.op('dve', lambda e: e.reciprocal(out=zsum[:], in_=zsum[:]), r=['zsum'], w=['zsum'])
                P.op('dve', lambda e: e.tensor_tensor(out=AP(sel, 256, [[384, 128], [16, 8], [1, 16]]), in0=tve[:], in1=AP(zsum, 0, [[8, 128], [1, 8], [0, 16]]), op=ALU.mult),
                     r=['tve', 'zsum'], w=['sel'])
                P.op('dve', lambda e: e.tensor_copy(out=selb[:], in_=sel[:]), r=['sel'], w=['selb'])

        def tail(blk):
                b, tb_ = divmod(blk, S // NT)
                t0 = tb_ * NT
                par = blk % 2
                h2 = h2s[par]; qT = qTs[par]; scs = scss[par]
                Gs = Gsb[blk % 2]
                gsk = 'Gs%d' % (blk % 2)

                def trs(e):
                    for c3_ in range(3):
                        ins = e.transpose(out=AP(Bk7b, c3_ * 128, [[1024, 128], [1, 128]]), in_=selb[:, c3_, :], identity=identb[:])
                    return ins
                P.op('pe', trs, r=['selb', 'identb'], w=['Bk7b'])
                P.op('act', lambda e: e.copy(out=selT[:], in_=AP(Bk7b, 0, [[1024, 128], [128, 3], [1, 128]])), r=['Bk7b'], w=['selT'])
                for tg in range(4):
                    io_b = AP(iob, 0, [[128, 128], [0, 32], [1, 128]])
                    OJ = OJs[tg % 2]; OI = OIs[tg % 2]; ojk = 'OJ%d' % (tg % 2); oik = 'OI%d' % (tg % 2)
                    P.op('dve', lambda e, io_b=io_b, tg=tg, OJ=OJ: e.tensor_tensor(out=OJ[:], in0=io_b, in1=AP(selT, 128 + tg * 32, [[384, 128], [1, 32], [0, 128]]), op=ALU.is_equal),
                         r=['iob', 'selT'], w=[ojk])
                    P.op('dve', lambda e, io_b=io_b, tg=tg, OI=OI: e.tensor_tensor(out=OI[:], in0=io_b, in1=AP(selT, tg * 32, [[384, 128], [1, 32], [0, 128]]), op=ALU.is_equal),
                         r=['iob', 'selT'], w=[oik])
                    P.op('pool', lambda e, tg=tg, OI=OI: e.tensor_tensor(out=OI[:], in0=OI[:], in1=AP(selT, 256 + tg * 32, [[384, 128], [1, 32], [0, 128]]), op=ALU.mult),
                         r=[oik, 'selT'], w=[oik])
                    for t4 in range(8):
                        pbk = (6, 4, 5, 0, 1, 2, 3)[(tg * 8 + t4) % 7]

                        def gmm(e, t4=t4, pbk=pbk, OJ=OJ, OI=OI):
                            for tq in range(4):
                                tl = t4 * 4 + tq
                                ins = e.matmul(Bk[pbk][:, tq * 128:(tq + 1) * 128], lhsT=OJ[:, tl, :], rhs=OI[:, tl, :], start=True, stop=True)
                            return ins
                        P.op('pe', gmm, r=[ojk, oik], w=[bk(pbk)])
                        P.op('act', lambda e, t4=t4, pbk=pbk, tg=tg, Gs=Gs: e.copy(out=AP(Gs, tg * 32 + t4 * 4, [[128 * NT, 128], [1, 4], [NT, 128]]),
                                                                           in_=AP(Bk[pbk], 0, [[512, 128], [128, 4], [1, 128]])), r=[bk(pbk)], w=[gsk])
                P.dma('sp', G_d[blk], Gs[:], 'gdo%d' % (blk % 2), r=[gsk], w=[gsk, 'Gd%d' % blk])

        nb1 = nblk if not quick else 1
        front(0)
        for blk in range(nb1):
            mid_a(blk)
            gate_(blk)
            if blk + 1 < nb1:
                front(blk + 1)
            mid_b(blk)
            tail(blk)
        P.barrier()

    with ExitStack() as st:
        def SB(name, shape, dt):
            return st.enter_context(nc.sbuf_tensor(name, list(shape), dt))

        def PS(name, shape, dt=F32):
            return st.enter_context(nc.psum_tensor(name, list(shape), dt))
        NTM = 384
        h2e = SB("h2e", [128, 8, NTM], BF16)
        utb = [SB("utb%d" % i, [128, 8, 128], BF16) for i in range(8)]
        vtb = [SB("vtb%d" % i, [128, 1024], BF16) for i in range(8)]
        gq = [SB("gq%d" % i, [128, 3, 4, 128], BF16) for i in range(2)]
        glb = [SB("glb%d" % i, [128, NTM], BF16) for i in range(2)]
        actb = [SB("actb%d" % i, [128, NTM], BF16) for i in range(2)]
        x1b = SB("x1b", [128, D], F32)
        o2 = SB("o2", [128, D], F32)
        sqe = SB("sqe", [128, D], F32)
        sse = SB("sse", [128, 4], F32)
        Bo = [PS("Bo%d" % i, [128, 512], F32) for i in range(6)]
        Bp = [PS("Bp%d" % i, [128, 512], F32) for i in range(2)]
        blocks = []
        for b in range(NB):
            t = 0
            for nt in [384] * 10 + [256]:
                blocks.append((b, t, nt))
                t += nt
        for (b, t0, nt) in (blocks if not quick else blocks[:1]):
            nsub = nt // 128
            tb0 = (b * S + t0) // 128
            P.dma('sp', h2e[:, :, 0:nt], h2T_d[b, :, :, t0:t0 + nt], 'h2e', w=['h2e'])
            def emit_pre(ci):
                sl = ci % 8
                P.dma('sp', utb[sl][:], UTb_d[ci], 'utb%d' % sl, w=['utb%d' % sl])
                P.dma('pool', vtb[sl][:], Vb_d[ci], 'vtb%d' % sl, w=['vtb%d' % sl])
                gsl = (ci // 4) % 2
                if ci % 4 == 0:
                    for sub in range(nsub):
                        P.dma('sp', gq[gsl][:, sub, :, :], G_d[tb0 + sub, :, ci:ci + 4, :], 'gq%d' % gsl, r=['Gd%d' % (tb0 + sub)], w=['gq%d' % gsl])
                pp = ci % 2

                def pmm(e, sl=sl, pp=pp, nt=nt):
                    for kc in range(8):
                        ins = e.matmul(Bp[pp][:, 0:nt], lhsT=utb[sl][:, kc, :], rhs=h2e[:, kc, 0:nt], start=(kc == 0), stop=(kc == 7))
                    return ins
                P.op('pe', pmm, r=['utb%d' % sl, 'h2e'], w=['Bp%d' % pp])
                P.op('act', lambda e, pp=pp, nt=nt: e.activation(out=glb[pp][:, 0:nt], in_=Bp[pp][:, 0:nt], func=AF.Gelu), r=['Bp%d' % pp], w=['glb%d' % pp])
                P.op('dve', lambda e, pp=pp, nt=nt, nsub=nsub, gsl=gsl, ci=ci: e.tensor_tensor(
                    out=AP(actb[pp], 0, [[NTM, 128], [128, nsub], [1, 128]]), in0=AP(glb[pp], 0, [[NTM, 128], [128, nsub], [1, 128]]),
                    in1=AP(gq[gsl], (ci % 4) * 128, [[1536, 128], [512, nsub], [1, 128]]), op=ALU.mult),
                    r=['glb%d' % pp, 'gq%d' % gsl], w=['actb%d' % pp])

            def emit_out(ci):
                sl = ci % 8
                pp = ci % 2
                def omm(e, sl=sl, pp=pp, nsub=nsub, ci=ci):
                    for sub in range(nsub):
                        for hf in range(2):
                            ins = e.matmul(Bo[sub * 2 + hf][:, :], lhsT=actb[pp][:, sub * 128:(sub + 1) * 128], rhs=vtb[sl][:, hf * 512:(hf + 1) * 512],
                                           start=(ci == 0), stop=(ci == 127))
                    return ins
                P.op('pe', omm, r=['actb%d' % pp, 'vtb%d' % sl], w=['Bo'])
            emit_pre(0)
            for ci in range(128):
                if ci + 1 < 128:
                    emit_pre(ci + 1)
                emit_out(ci)
            for sub in range(nsub):
                tt0 = t0 + sub * 128
                P.dma('sp', x1b[:], x1_d[b, tt0:tt0 + 128, :], 'x1b', w=['x1b'])
                for hf in range(2):
                    P.op('dve', lambda e, sub=sub, hf=hf: e.tensor_tensor(out=o2[:, hf * 512:(hf + 1) * 512], in0=Bo[sub * 2 + hf][:, :], in1=x1b[:, hf * 512:(hf + 1) * 512], op=ALU.add),
                         r=['Bo', 'x1b'], w=['o2_%d' % hf])
                P.op('act', lambda e: e.activation(out=sqe[:], in_=o2[:], func=AF.Square, accum_out=sse[:, 0:1]), r=['o2_0', 'o2_1'], w=['sqe', 'ssea'])
                P.op('act', lambda e: e.activation(out=sse[:, 1:2], in_=sse[:, 0:1], func=AF.Sqrt, scale=1.0 / D, bias=epsc[:, 0:1]), r=['ssea', 'epsc'], w=['sseb'])
                P.op('dve', lambda e: e.reciprocal(out=sse[:, 2:3], in_=sse[:, 1:2]), r=['sseb'], w=['ssec'])
                P.op('dve', lambda e: e.scalar_tensor_tensor(out=sqe[:], in0=o2[:], scalar=sse[:, 2:3], in1=gfin_s[:], op0=ALU.mult, op1=ALU.mult),
                     r=['o2_0', 'o2_1', 'ssec', 'gfin', 'sqe'], w=['sqe'])
                P.dma('sp', y[b, tt0:tt0 + 128, :], sqe[:], 'yo', r=['sqe'], w=['yo'])
        P.barrier()
    return nc, P, {}


def host_inputs(inp):
    f = np.float32

    def kmaj(w):
        K, N = w.shape
        return np.ascontiguousarray(w.reshape(K // 128, 128, N).transpose(1, 0, 2)).astype(f)
    com = {}
    com["w1"] = kmaj(inp["w_in"][0])
    com["g1"] = np.ascontiguousarray(inp["norm1_g"][0].reshape(8, 128).T).astype(f)
    com["wout"] = kmaj(inp["w_out"][0])
    com["wglu"] = kmaj(inp["w_glu"][0])
    com["wq"] = kmaj(inp["w_query"][0])
    com["g2"] = np.ascontiguousarray(inp["norm2_g"][0].reshape(8, 128).T).astype(f)
    com["gfin"] = np.ascontiguousarray(np.broadcast_to(inp["final_g"][None, :], (128, D))).astype(f)
    com["wf"] = np.ascontiguousarray(inp["w_fourier"][0].transpose(1, 0, 2)).astype(f)
    c = np.arange(128)
    ang = 2 * np.pi * np.outer(c, c) / 128.0
    sc = 1.0 / math.sqrt(S * 128)
    com["ccsc"] = np.stack([np.cos(ang) * sc, -np.sin(ang) * sc], axis=1).astype(f)
    com["ident"] = np.eye(128, dtype=f)
    s_idx = np.arange(S)
    ks = (np.outer(s_idx, s_idx) % S).astype(np.float64) * (2 * np.pi / S)
    ct = np.cos(ks).astype(f).astype(ml_dtypes.bfloat16)
    stt = np.sin(ks).astype(f).astype(ml_dtypes.bfloat16)
    t = np.stack([ct, stt], axis=0)
    t = t.reshape(2, 32, 128, 16, 256).transpose(3, 2, 0, 1, 4)
    com["tab"] = np.ascontiguousarray(t)
    com["iota128"] = np.ascontiguousarray(np.broadcast_to(np.arange(128, dtype=f)[None, :], (128, 128)))
    com["iota16"] = np.ascontiguousarray(np.broadcast_to(np.arange(16, dtype=f)[None, :], (128, 16)))

    def lamA(arr):
        return np.ascontiguousarray(arr.reshape(2, 4, 4, 2, 64).transpose(3, 4, 1, 2, 0).reshape(128, 32)).astype(f)
    com["lam_are"] = lamA(inp["ssm_a_re"][0])
    com["lam_aim"] = lamA(inp["ssm_a_im"][0])
    com["lam_lst"] = lamA(np.broadcast_to(inp["ssm_log_step"][0][:, :, None], (2, 32, 64)))
    bA = np.zeros((2, 4, 128, 8, 128), f)
    cA = np.zeros((2, 4, 128, 8, 128), f)
    for ri, (bsrc, csrc) in enumerate(((inp["ssm_b_re"][0], inp["ssm_c_re"][0]), (inp["ssm_b_im"][0], inp["ssm_c_im"][0]))):
        for T in range(4):
            for q in range(4):
                for gp in range(2):
                    g = 8 * T + 2 * q + gp
                    for dr in range(2):
                        col = (2 * q + gp) * 16
                        bA[ri, T, gp * 64:(gp + 1) * 64, q * 2 + dr, col:col + 16] = bsrc[dr, g]
                        cA[ri, T, gp * 64:(gp + 1) * 64, q * 2 + dr, col:col + 16] = csrc[dr, g].T
    com["bA"] = bA
    com["cA"] = cA
    com["dD"] = np.ascontiguousarray(inp["ssm_d"][0].reshape(4, 128).T).astype(f)
    eu = inp["expert_u"][0]
    com["uT_in"] = np.ascontiguousarray(eu.reshape(16384, 8, 128).transpose(2, 1, 0)).astype(f)
    com["v_in"] = np.ascontiguousarray(inp["expert_v"][0].reshape(128, 128, 1024)).astype(f)
    sk = inp["sub_keys"][0]
    com["keysT_in"] = np.ascontiguousarray(sk.reshape(16, 128, 128).transpose(2, 0, 1)).astype(f)
    return com


_CACHE = {}


def kernel(**inp):
    if "nc" not in _CACHE:
        _CACHE["nc"] = build()[0]
    nc = _CACHE["nc"]
    com = host_inputs(inp)
    xs = np.ascontiguousarray(inp["x"]).astype(np.float32)
    in_maps = []
    for c in range(8):
        m = dict(com)
        m["x"] = xs[2 * c:2 * c + 2]
        in_maps.append(m)
    res = run_bass_kernel_spmd(nc, in_maps, core_ids=list(range(8)))
    return np.concatenate([np.asarray(r["y"]) for r in res.results], axis=0).astype(np.float32)
```

```python
import math
from contextlib import ExitStack
import numpy as np
import ml_dtypes
import concourse.bass as bass
import concourse.mybir as mybir
from concourse.bass_utils import run_bass_kernel_spmd

F32 = mybir.dt.float32
BF16 = mybir.dt.bfloat16
U32 = mybir.dt.uint32
ALU = mybir.AluOpType
AF = mybir.ActivationFunctionType
AX = mybir.AxisListType

NB = 2
S = 4096
D = 1024
L = 16
NCH = S // L
PADW = 2 * L - 1
EPS = 1e-6
ENG = ['pe', 'act', 'dve', 'pool', 'sp']


class Prog:
    def __init__(s, nc):
        s.nc = nc
        s.e = dict(pe=nc.tensor, act=nc.scalar, dve=nc.vector, pool=nc.gpsimd, sp=nc.sync)
        s.nsem = 0
        s.new_sems()
        s.dsem = {}
        s.lastw = {}
        s.readers = {}
        s.items = {k: [] for k in ENG}

    def new_sems(s):
        if s.nsem > 0:
            return
        s.sem = {}
        for k in ENG:
            s.sem[k] = s.nc.alloc_semaphore("es%d_%s" % (s.nsem, k))
        s.nsem += 1
        s.cnt = {k: 0 for k in ENG}
        s.waited = {}

    def _wait(s, eng, tok):
        kind, name, val = tok
        h = s.sem[name] if kind == 'e' else s.dsem[name][0]
        key = (eng, h.num)
        if s.waited.get(key, 0) >= val:
            return
        s.waited[key] = val
        s.items[eng].append(('w', h, val))

    def _deps(s, eng, r, w):
        deps = []
        for k in r:
            if k in s.lastw:
                deps.append((s.lastw[k], True))
        for k in w:
            if k in s.lastw:
                deps.append((s.lastw[k], False))
            for t in s.readers.get(k, ()):
                deps.append((t, False))
        for tok, raw in deps:
            if tok[0] == 'e' and tok[1] == eng:
                if raw and s.cnt[eng] - tok[2] < 2:
                    s._wait(eng, tok)
                continue
            s._wait(eng, tok)

    def _upd(s, tok, r, w):
        for k in r:
            s.readers.setdefault(k, []).append(tok)
        for k in w:
            s.lastw[k] = tok
            s.readers[k] = []

    def op(s, eng, fn, r=(), w=()):
        s._deps(eng, r, w)
        s.cnt[eng] += 1
        s.items[eng].append(('o', fn, s.sem[eng]))
        s._upd(('e', eng, s.cnt[eng]), r, w)

    def dma(s, eng, out, in_, sem, r=(), w=()):
        s._deps(eng, r, w)
        if sem not in s.dsem:
            if getattr(s, 'free_d', None):
                s.free_d.sort(key=lambda hc: hc[1])
                s.dsem[sem] = s.free_d.pop(0)
            else:
                s.ndsem = getattr(s, 'ndsem', 0) + 1
                s.dsem[sem] = [s.nc.alloc_semaphore("ds%d" % s.ndsem), 0]
        d = s.dsem[sem]
        d[1] += 16
        s.items[eng].append(('d', out, in_, d[0]))
        s._upd(('d', sem, d[1]), r, w)

    def barrier(s, final=False):
        toks = [('e', k, s.cnt[k]) for k in ENG if s.cnt[k] > 0]
        toks += [('d', n, d[1]) for n, d in s.dsem.items() if d[1] > 0]
        for eng in (['sp'] if final else ENG):
            for tok in toks:
                if tok[0] == 'e' and tok[1] == eng:
                    continue
                s._wait(eng, tok)
        s.lastw.clear()
        s.readers.clear()
        s.flush()
        if not hasattr(s, 'free_d'):
            s.free_d = []
        s.free_d.extend(s.dsem.values())
        s.dsem = {}

    def flush(s):
        def replay(items, embed=False):
            def f(e):
                pend = []
                for it in items:
                    if it[0] == 'w':
                        if embed:
                            pend.append(it)
                        else:
                            e.wait_ge(it[1], it[2])
                    elif it[0] == 'o':
                        for p in pend[:-1]:
                            e.wait_ge(p[1], p[2])
                        ins = it[1](e)
                        if pend:
                            ins._wait_ge(pend[-1][1], pend[-1][2])
                        pend = []
                        ins.then_inc(it[2], 1)
                    else:
                        e.dma_start(out=it[1], in_=it[2]).then_inc(it[3], 16)
            return f
        with s.nc.Block() as block:
            for k, dec in (('pe', block.tensor), ('act', block.scalar), ('dve', block.vector), ('pool', block.gpsimd), ('sp', block.sync)):
                if s.items[k]:
                    dec(replay(s.items[k], embed=False))
        s.items = {k: [] for k in ENG}


def AP(t, off, dims):
    return bass.AP(t, off, [list(d) for d in dims])


def build(debug=(), nblk_lim=None, stage=99, upto=99, quick=False):
    nc = bass.Bass("TRN2", target_bir_lowering=False)
    P = Prog(nc)

    def din(name, shape, dt=F32):
        return nc.dram_tensor(name, list(shape), dt, kind="ExternalInput").ap()

    dbg = {}

    def dscr(name, shape, dt=BF16):
        kind = "ExternalOutput" if name in debug else "Internal"
        a = nc.dram_tensor(name, list(shape), dt, kind=kind).ap()
        return a

    x = din("x", [NB, S, D])
    w1 = din("w1", [128, 8, D])
    g1 = din("g1", [128, 8])
    woutw = din("wout", [128, 8, D])
    wglu = din("wglu", [128, 4, 512])
    wq = din("wq", [128, 8, 2048])
    g2 = din("g2", [128, 8])
    gfin = din("gfin", [128, D])
    wf = din("wf", [128, 4, 128])
    ccsc = din("ccsc", [128, 2, 128])
    ident_in = din("ident", [128, 128])
    tab = din("tab", [16, 128, 2, 32, 256], BF16)
    y = nc.dram_tensor("y", [NB, S, D], F32, kind="ExternalOutput").ap()

    zS_d = dscr("zS_d", [NB, 4, 128, S])
    A_d = dscr("A_d", [NB, 32, 128, 1024])
    yF_d = dscr("yF_d", [NB, 4, 128, S])
    yG_d = dscr("yG_d", [NB, 4, 128, S])
    x1_d = dscr("x1_d", [NB, S, D], F32)
    h2T_d = dscr("h2T_d", [NB, 128, 8, S])

    def dump(name, tile, shape, dt, key):
        if name in debug:
            d = nc.dram_tensor(name, list(shape), dt, kind="ExternalOutput").ap()
            P.dma('sp', d, tile, 'dbg_' + name, r=[key], w=['dbg_' + name])

    pst = ExitStack()

    def SBp(name, shape, dt):
        return pst.enter_context(nc.sbuf_tensor(name, list(shape), dt))

    identb = SBp("identb", [128, 128], BF16)
    identf = SBp("identf", [128, 128], F32)
    gfin_s = SBp("gfin_s", [128, D], F32)
    epsc = SBp("epsc", [128, 1], F32)

    P.dma('sp', identf[:], ident_in[:, :], 'c_identf', w=['identf'])
    P.dma('sp', gfin_s[:], gfin[:, :], 'c_gfin', w=['gfin'])
    P.op('dve', lambda e: e.tensor_copy(out=identb[:], in_=identf[:]), r=['identf'], w=['identb'])
    P.op('dve', lambda e: e.memset(epsc[:], EPS), w=['epsc'])

    uT_in = din("uT_in", [128, 8, 16384])
    v_in = din("v_in", [128, 128, 1024])
    UTb_d = dscr("UTb_d", [128, 128, 8, 128])
    Vb_d = dscr("Vb_d", [128, 128, 1024])
    g2s = SBp("g2s", [128, 8], F32)
    P.dma('sp', g2s[:], g2[:, :], 'c_g2s', w=['g2s'])
    e0st = ExitStack()
    uf0 = e0st.enter_context(nc.sbuf_tensor("uf0", [128, 8, 512], F32))
    ub0 = e0st.enter_context(nc.sbuf_tensor("ub0", [128, 8, 512], BF16))
    vf0 = e0st.enter_context(nc.sbuf_tensor("vf0", [128, 4, 1024], F32))
    vb0 = e0st.enter_context(nc.sbuf_tensor("vb0", [128, 4, 1024], BF16))

    def e0_iter(it):
        P.dma('pool', uf0[:], uT_in[:, :, it * 512:(it + 1) * 512], 'uf0', w=['uf0'])
        for kc in range(8):
            P.op('dve' if kc % 2 else 'pool', lambda e, kc=kc: e.tensor_scalar(out=ub0[:, kc, :], in0=uf0[:, kc, :], scalar1=g2s[:, kc:kc + 1], scalar2=None, op0=ALU.mult),
                 r=['uf0', 'g2s'], w=['ub0_%d' % kc] + ['ubo0_%d' % i4 for i4 in range(4)])
        for i4 in range(4):
            P.dma('pool', UTb_d[it * 4 + i4], AP(ub0, i4 * 128, [[4096, 128], [512, 8], [1, 128]]),
                  'ubo0', r=['ub0_%d' % kc for kc in range(8)], w=['ubo0_%d' % i4])
        P.dma('pool', vf0[:], v_in[it * 4:(it + 1) * 4].rearrange("i p d -> p i d"), 'vf0', w=['vf0'])
        P.op('pool', lambda e: e.tensor_copy(out=vb0[:], in_=vf0[:]), r=['vf0'], w=['vb0'])
        P.dma('pool', Vb_d[it * 4:(it + 1) * 4].rearrange("i p d -> p i d"), vb0[:], 'vbo0', r=['vb0'], w=['vb0'])

    with ExitStack() as st:
        def SB(name, shape, dt):
            return st.enter_context(nc.sbuf_tensor(name, list(shape), dt))

        def PS(name, shape, dt=F32):
            return st.enter_context(nc.psum_tensor(name, list(shape), dt))

        w1f = SB("w1f", [128, 8, D], F32)
        w1b = SB("w1b", [128, 8, D], BF16)
        g1s = SB("g1s", [128, 8], F32)
        wfs = SB("wfs", [128, 4, 128], F32)
        ccs = SB("ccs", [128, 2, 128], F32)
        csw = SB("csw", [128, 4, 256], BF16)
        wfb = SB("wfb", [128, 4, 128], BF16)
        ccb = SB("ccb", [128, 2, 128], BF16)
        xs = [SB("xs%d" % i, [128, D], F32) for i in range(2)]
        sq = SB("sq", [128, D], F32)
        ss = [SB("ss%d" % i, [128, 4], F32) for i in range(2)]
        xn = [SB("xn%d" % i, [128, D], BF16) for i in range(2)]
        hT = [SB("hT%d" % i, [128, 8, 128], BF16) for i in range(2)]
        zT = [SB("zT%d" % i, [128, 8, 128], BF16) for i in range(2)]
        Ab = [SB("Ab%d" % i, [128, 1024], BF16) for i in range(2)]
        pT = [PS("pT%d" % i, [128, 8, 128], BF16) for i in range(2)]
        pz = [PS("pz%d" % i, [128, 8, 128], F32) for i in range(1)]
        pA = [PS("pA%d" % i, [128, 1024], F32) for i in range(1)]
        pw = PS("pw", [128, 512], F32)

        P.dma('sp', w1f[:], w1[:, :, :], 'c_w1f', w=['w1f'])
        P.dma('sp', g1s[:], g1[:, :], 'c_g1s', w=['g1s'])
        P.dma('sp', wfs[:], wf[:, :, :], 'c_wfs', w=['wfs'])
        P.dma('sp', ccs[:], ccsc[:, :, :], 'c_ccs', w=['ccs'])
        for kc in range(8):
            P.op('dve', lambda e, kc=kc: e.tensor_scalar(out=w1b[:, kc, :], in0=w1f[:, kc, :], scalar1=g1s[:, kc:kc + 1],
                                                        scalar2=None, op0=ALU.mult), r=['w1f', 'g1s'], w=['w1b'])
        P.op('dve', lambda e: e.tensor_copy(out=wfb[:], in_=wfs[:]), r=['wfs'], w=['wfb'])
        P.op('dve', lambda e: e.tensor_copy(out=ccb[:], in_=ccs[:]), r=['ccs'], w=['ccb'])
        for g in range(4 if stage >= 0 else 0):
            for t in range(2):
                P.op('pe', lambda e, g=g, t=t: e.matmul(pw[:, (g % 2) * 256 + t * 128:(g % 2) * 256 + t * 128 + 128],
                                                        lhsT=ccb[:, t, :], rhs=wfb[:, g, :], start=True, stop=True),
                     r=['ccb', 'wfb'], w=['pw'])
            P.op('dve', lambda e, g=g: e.tensor_copy(out=csw[:, g, :], in_=pw[:, (g % 2) * 256:(g % 2) * 256 + 256]),
                 r=['pw'], w=['csw'])

        nblk = NB * (S // 128) if nblk_lim is None else nblk_lim
        if stage == -2:
            nblk = 0
        for blk in range(nblk):
            b, tb = divmod(blk, S // 128)
            i = blk % 2
            t0 = tb * 128
            if blk % 2 == 0:
                e0_iter(blk // 2)
            P.dma('sp', xs[i][:], x[b, t0:t0 + 128, :], 'xs%d' % i, w=['xs%d' % i])
            P.op('act', lambda e, i=i: e.activation(out=sq[:], in_=xs[i][:], func=AF.Square, accum_out=ss[i][:, 0:1]),
                 r=['xs%d' % i], w=['sq', 'ssa%d' % i])
            P.op('act', lambda e, i=i: e.activation(out=ss[i][:, 1:2], in_=ss[i][:, 0:1], func=AF.Sqrt, scale=1.0 / D, bias=epsc[:, 0:1]),
                 r=['ssa%d' % i, 'epsc'], w=['ssb%d' % i])
            P.op('dve', lambda e, i=i: e.reciprocal(out=ss[i][:, 2:3], in_=ss[i][:, 1:2]), r=['ssb%d' % i], w=['ssc%d' % i])
            P.op('dve', lambda e, i=i: e.tensor_scalar(out=xn[i][:], in0=xs[i][:], scalar1=ss[i][:, 2:3], scalar2=None, op0=ALU.mult),
                 r=['xs%d' % i, 'ssc%d' % i], w=['xn%d' % i])

            if stage < 1:
                continue
            def tr(e, i=i):
                for kc in range(8):
                    ins = e.transpose(out=pT[i][:, kc, :], in_=xn[i][:, kc * 128:(kc + 1) * 128], identity=identb[:])
                return ins
            P.op('pe', tr, r=['xn%d' % i, 'identb'], w=['pT%d' % i])
            P.op('act', lambda e, i=i: e.copy(out=hT[i][:], in_=pT[i][:]), r=['pT%d' % i], w=['hT%d' % i])

            if stage < 2:
                continue
            def zmm(e, i=i):
                for n in range(8):
                    for kc in range(8):
                        ins = e.matmul(pz[0][:, n, :], lhsT=w1b[:, kc, n * 128:(n + 1) * 128], rhs=hT[i][:, kc, :],
                                       start=(kc == 0), stop=(kc == 7))
                return ins
            P.op('pe', zmm, r=['hT%d' % i, 'w1b'], w=['pz'])
            P.op('dve', lambda e, i=i: e.tensor_copy(out=zT[i][:], in_=pz[0][:]), r=['pz'], w=['zT%d' % i])
            if blk == 0:
                dump('d_xn', xn[i][:], [128, D], BF16, 'xn%d' % i)
                dump('d_hT', hT[i][:], [128, 8, 128], BF16, 'hT%d' % i)
                dump('d_zT', zT[i][:], [128, 8, 128], BF16, 'zT%d' % i)
                dump('d_w1b', w1b[:], [128, 8, D], BF16, 'w1b')
            P.dma('sp', zS_d[b, :, :, t0:t0 + 128].rearrange("t p s -> p t s"), zT[i][:, 0:4, :], 'zso%d' % i, r=['zT%d' % i], w=['zso%d' % i])

            if stage < 3:
                continue
            def amm(e, i=i):
                for g in range(4):
                    ins = e.matmul(pA[0][:, g * 256:(g + 1) * 256], lhsT=zT[i][:, 4 + g, :], rhs=csw[:, g, :], start=True, stop=True)
                return ins
            P.op('pe', amm, r=['zT%d' % i, 'csw'], w=['pA'])
            P.op('act', lambda e, i=i: e.copy(out=Ab[i][:], in_=pA[0][:]), r=['pA'], w=['Ab%d' % i])
            P.dma('sp', A_d[b, tb, :, :], Ab[i][:], 'ao%d' % i, r=['Ab%d' % i], w=['ao%d' % i])
        P.barrier()
    e0st.close()
    P.new_sems()
    if upto < 2:
        return nc, P, {}

    with ExitStack() as st:
        def SB(name, shape, dt):
            return st.enter_context(nc.sbuf_tensor(name, list(shape), dt))

        def PS(name, shape, dt=F32):
            return st.enter_context(nc.psum_tensor(name, list(shape), dt))
        Asb = SB("Asb", [128, 32, 1024], BF16)
        tbs = [SB("tb%d" % i, [128, 2, 32, 256], BF16) for i in range(2)]
        yst = [SB("yst%d" % i, [128, 4, 256], BF16) for i in range(2)]
        pf = [PS("pf%d" % i, [128, 512], F32) for i in range(4)]
        for b in range(NB):
            P.dma('sp', Asb[:], A_d[b].rearrange("s p n -> p s n"), 'Asb', w=['Asb'])
            for kt in range(16 if not quick else 1):
                it = (b * 16 + kt) % 2
                P.dma('sp', tbs[it][:], tab[kt], 'tb%d' % it, w=['tb%d' % it])
                for g in range(4):
                    def fmm(e, g=g, it=it):
                        n = 0
                        for cs in range(2):
                            for sc in range(32):
                                ins = e.matmul(pf[g][:, 0:256], lhsT=Asb[:, sc, g * 256 + cs * 128:g * 256 + cs * 128 + 128],
                                               rhs=tbs[it][:, cs, sc, :], start=(n == 0), stop=(n == 63))
                                n += 1
                        return ins
                    P.op('pe', fmm, r=['Asb', 'tb%d' % it], w=['pf%d' % g])
                    if g % 2:
                        P.op('act', lambda e, g=g, it=it: e.copy(out=yst[it][:, g, :], in_=pf[g][:, 0:256]), r=['pf%d' % g], w=['yst%d_%d' % (it, g)])
                    else:
                        P.op('dve', lambda e, g=g, it=it: e.tensor_copy(out=yst[it][:, g, :], in_=pf[g][:, 0:256]), r=['pf%d' % g], w=['yst%d_%d' % (it, g)])
                P.dma('sp', yF_d[b, :, :, kt * 256:(kt + 1) * 256].rearrange("g p s -> p g s"), yst[it][:], 'yfo%d' % it,
                      r=['yst%d_%d' % (it, g) for g in range(4)], w=['yst%d_%d' % (it, g) for g in range(4)])
        P.barrier()
    P.new_sems()
    if upto < 3:
        return nc, P, {}

    lam_are = din("lam_are", [128, 32]); lam_aim = din("lam_aim", [128, 32]); lam_lst = din("lam_lst", [128, 32])
    bA = din("bA", [2, 4, 128, 8, 128]); cA = din("cA", [2, 4, 128, 8, 128]); dD = din("dD", [128, 4])
    WLAG_d = dscr("WLAG_d", [4, 128, 31, 128])
    WIN_d = dscr("WIN_d", [4, 16, 128, 16, 128])
    WOUT_d = dscr("WOUT_d", [4, 16, 128, 16, 128])
    prt = SBp("prt", [128, 17, 32], F32)
    pit = SBp("pit", [128, 17, 32], F32)
    with ExitStack() as st:
        def SB(name, shape, dt):
            return st.enter_context(nc.sbuf_tensor(name, list(shape), dt))

        def PS(name, shape, dt=F32):
            return st.enter_context(nc.psum_tensor(name, list(shape), dt))
        names = ['are', 'aim', 'lst', 'stp', 'ar', 'ai', 'mag', 's16', 'cc', 'sn', 't1', 't2', 't3', 'lr', 'li', 'den', 'nr', 'cr', 'ci']
        T_ = {n: SB("l_" + n, [128, 32], F32) for n in names}
        P.dma('sp', T_['are'][:], lam_are[:, :], 'c_are', w=['are'])
        P.dma('sp', T_['aim'][:], lam_aim[:, :], 'c_aim', w=['aim'])
        P.dma('sp', T_['lst'][:], lam_lst[:, :], 'c_lst', w=['lst'])
        dDs = SB("dDs", [128, 4], F32)
        P.dma('sp', dDs[:], dD[:, :], 'c_dD', w=['dD'])

        def tt(o, a, b_, op, eng='dve'):
            P.op(eng, lambda e: e.tensor_tensor(out=T_[o][:], in0=T_[a][:], in1=T_[b_][:], op=op), r=[a, b_], w=[o])

        def ts(o, a, s1, s2, op0, op1=None):
            if op1 is None:
                P.op('dve', lambda e: e.tensor_scalar(out=T_[o][:], in0=T_[a][:], scalar1=s1, scalar2=None, op0=op0), r=[a], w=[o])
            else:
                P.op('dve', lambda e: e.tensor_scalar(out=T_[o][:], in0=T_[a][:], scalar1=s1, scalar2=s2, op0=op0, op1=op1), r=[a], w=[o])

        def act(o, a, func, scale=1.0):
            P.op('act', lambda e: e.activation(out=T_[o][:], in_=T_[a][:], func=func, scale=scale), r=[a], w=[o])
        act('stp', 'lst', AF.Exp)
        tt('ar', 'are', 'stp', ALU.mult)
        tt('ai', 'aim', 'stp', ALU.mult)
        act('mag', 'ar', AF.Exp)
        act('s16', 'ai', AF.Sin, 1.0 / 16)
        act('sn', 'ai', AF.Sin, 1.0 / 8)
        tt('t1', 's16', 's16', ALU.mult)
        ts('cc', 't1', -2.0, 1.0, ALU.mult, ALU.add)
        for _ in range(3):
            tt('t1', 'cc', 'cc', ALU.mult)
            tt('t2', 'sn', 'sn', ALU.mult)
            tt('t3', 'cc', 'sn', ALU.mult)
            tt('cc', 't1', 't2', ALU.subtract)
            ts('sn', 't3', 2.0, None, ALU.mult)
        tt('lr', 'mag', 'cc', ALU.mult)
        tt('li', 'mag', 'sn', ALU.mult)
        tt('t1', 'are', 'are', ALU.mult)
        tt('t2', 'aim', 'aim', ALU.mult)
        tt('den', 't1', 't2', ALU.add)
        P.op('dve', lambda e: e.reciprocal(out=T_['den'][:], in_=T_['den'][:]), r=['den'], w=['den'])
        ts('nr', 'lr', -1.0, None, ALU.add)
        tt('t1', 'nr', 'are', ALU.mult)
        tt('t2', 'li', 'aim', ALU.mult)
        tt('t3', 't1', 't2', ALU.add)
        tt('cr', 't3', 'den', ALU.mult)
        tt('t1', 'li', 'are', ALU.mult)
        tt('t2', 'nr', 'aim', ALU.mult)
        tt('t3', 't1', 't2', ALU.subtract)
        tt('ci', 't3', 'den', ALU.mult)
        P.op('dve', lambda e: e.memset(prt[:, 0, :], 1.0), w=['prt'])
        P.op('dve', lambda e: e.memset(pit[:, 0, :], 0.0), w=['pit'])
        for k in range(16):
            P.op('dve', lambda e, k=k: e.tensor_tensor(out=T_['t1'][:], in0=prt[:, k, :], in1=T_['lr'][:], op=ALU.mult), r=['prt', 'lr'], w=['t1'])
            P.op('dve', lambda e, k=k: e.tensor_tensor(out=T_['t2'][:], in0=pit[:, k, :], in1=T_['li'][:], op=ALU.mult), r=['pit', 'li'], w=['t2'])
            P.op('dve', lambda e, k=k: e.tensor_tensor(out=T_['t3'][:], in0=prt[:, k, :], in1=T_['li'][:], op=ALU.mult), r=['prt', 'li'], w=['t3'])
            P.op('dve', lambda e, k=k: e.tensor_tensor(out=T_['nr'][:], in0=pit[:, k, :], in1=T_['lr'][:], op=ALU.mult), r=['pit', 'lr'], w=['nr'])
            P.op('dve', lambda e, k=k: e.tensor_tensor(out=prt[:, k + 1, :], in0=T_['t1'][:], in1=T_['t2'][:], op=ALU.subtract), r=['t1', 't2'], w=['prt'])
            P.op('dve', lambda e, k=k: e.tensor_tensor(out=pit[:, k + 1, :], in0=T_['t3'][:], in1=T_['nr'][:], op=ALU.add), r=['t3', 'nr'], w=['pit'])

        bre = SB("bre", [128, 8, 128], F32); bim = SB("bim", [128, 8, 128], F32)
        cre = SB("cre", [128, 8, 128], F32); cim = SB("cim", [128, 8, 128], F32)
        Br = SB("Br", [128, 8, 128], F32); Bi = SB("Bi", [128, 8, 128], F32)
        u1 = SB("u1", [128, 8, 128], F32); u2 = SB("u2", [128, 8, 128], F32)
        v1 = SB("v1", [128, 8, 128], F32); v2 = SB("v2", [128, 8, 128], F32)
        Cxr = SB("Cxr", [128, 8, 128], BF16); Cxi = SB("Cxi", [128, 8, 128], BF16)
        Pkr = [SB("Pkr%d" % i, [128, 8, 128], BF16) for i in range(2)]
        Pki = [SB("Pki%d" % i, [128, 8, 128], BF16) for i in range(2)]
        CLr = [SB("CLr%d" % i, [128, 8, 128], BF16) for i in range(2)]
        CLi = [SB("CLi%d" % i, [128, 8, 128], BF16) for i in range(2)]
        winst = [SB("winst%d" % i, [128, 16, 128], BF16) for i in range(2)]
        wlags = SB("wlags", [128, 31, 128], BF16)
        ddiag = SB("ddiag", [128, 128], F32)
        ptk = [PS("ptk%d" % i, [128, 512], F32) for i in range(2)]
        ptr = [PS("ptr%d" % i, [128, 8, 128], BF16) for i in range(2)]

        def bc(tile, off, pstep):
            return AP(tile, off, [[pstep, 128], [1, 8], [0, 128]])
        for T in range(4):
            P.dma('sp', bre[:], bA[0, T], 'c_bre', w=['bre']); P.dma('sp', bim[:], bA[1, T], 'c_bim', w=['bim'])
            P.dma('sp', cre[:], cA[0, T], 'c_cre', w=['cre']); P.dma('sp', cim[:], cA[1, T], 'c_cim', w=['cim'])
            crb = bc(T_['cr'], T * 8, 32); cib = bc(T_['ci'], T * 8, 32)
            P.op('dve', lambda e, crb=crb: e.tensor_tensor(out=u1[:], in0=bre[:], in1=crb, op=ALU.mult), r=['bre', 'cr'], w=['u1'])
            P.op('dve', lambda e, cib=cib: e.tensor_tensor(out=u2[:], in0=bim[:], in1=cib, op=ALU.mult), r=['bim', 'ci'], w=['u2'])
            P.op('dve', lambda e: e.tensor_tensor(out=Br[:], in0=u1[:], in1=u2[:], op=ALU.subtract), r=['u1', 'u2'], w=['Br'])
            P.op('dve', lambda e, crb=crb: e.tensor_tensor(out=u1[:], in0=bim[:], in1=crb, op=ALU.mult), r=['bim', 'cr'], w=['u1'])
            P.op('dve', lambda e, cib=cib: e.tensor_tensor(out=u2[:], in0=bre[:], in1=cib, op=ALU.mult), r=['bre', 'ci'], w=['u2'])
            P.op('dve', lambda e: e.tensor_tensor(out=Bi[:], in0=u1[:], in1=u2[:], op=ALU.add), r=['u1', 'u2'], w=['Bi'])
            P.op('pool', lambda e: e.tensor_copy(out=Cxr[:], in_=cre[:]), r=['cre'], w=['Cxr'])
            P.op('pool', lambda e: e.tensor_scalar(out=Cxi[:], in0=cim[:], scalar1=-1.0, scalar2=None, op0=ALU.mult), r=['cim'], w=['Cxi'])
            P.op('dve', lambda e, T=T: e.tensor_scalar(out=ddiag[:], in0=identf[:], scalar1=dDs[:, T:T + 1], scalar2=None, op0=ALU.mult),
                 r=['identf', 'dD'], w=['ddiag'])
            for k in range(17):
                i2 = k % 2
                pkr = bc(prt, k * 32 + T * 8, 17 * 32); pki = bc(pit, k * 32 + T * 8, 17 * 32)
                if k <= 15:
                    P.op('dve', lambda e, pkr=pkr: e.tensor_tensor(out=u1[:], in0=Br[:], in1=pkr, op=ALU.mult), r=['Br', 'prt'], w=['u1'])
                    P.op('dve', lambda e, pki=pki: e.tensor_tensor(out=u2[:], in0=Bi[:], in1=pki, op=ALU.mult), r=['Bi', 'pit'], w=['u2'])
                    P.op('dve', lambda e, i2=i2: e.tensor_tensor(out=Pkr[i2][:], in0=u1[:], in1=u2[:], op=ALU.subtract), r=['u1', 'u2'], w=['Pkr%d' % i2])
                    P.op('dve', lambda e, pki=pki: e.tensor_tensor(out=u1[:], in0=Br[:], in1=pki, op=ALU.mult), r=['Br', 'pit'], w=['u1'])
                    P.op('dve', lambda e, pkr=pkr: e.tensor_tensor(out=u2[:], in0=Bi[:], in1=pkr, op=ALU.mult), r=['Bi', 'prt'], w=['u2'])
                    P.op('dve', lambda e, i2=i2: e.tensor_tensor(out=Pki[i2][:], in0=u1[:], in1=u2[:], op=ALU.add), r=['u1', 'u2'], w=['Pki%d' % i2])
                    if k == 0:
                        def tk0(e, i2=i2):
                            n = 0
                            for dr in range(2):
                                for q in range(4):
                                    for (Pt, Ct) in ((Pkr[i2], Cxr), (Pki[i2], Cxi)):
                                        ins = e.matmul(ptk[0][:, 0:128], lhsT=Pt[:, q * 2 + dr, :], rhs=Ct[:, q * 2 + dr, :], start=(n == 0), stop=(n == 15))
                                        n += 1
                            return ins
                        P.op('pe', tk0, r=['Pkr%d' % i2, 'Pki%d' % i2, 'Cxr', 'Cxi'], w=['ptk0'])
                        P.op('dve', lambda e: e.tensor_tensor(out=wlags[:, 15, :], in0=ptk[0][:, 0:128], in1=ddiag[:], op=ALU.add),
                             r=['ptk0', 'ddiag'], w=['wlags'])
                    else:
                        for dr in range(2):
                            def tk(e, i2=i2, dr=dr):
                                n = 0
                                for q in range(4):
                                    for (Pt, Ct) in ((Pkr[i2], Cxr), (Pki[i2], Cxi)):
                                        ins = e.matmul(ptk[dr][:, 0:128], lhsT=Pt[:, q * 2 + dr, :], rhs=Ct[:, q * 2 + dr, :], start=(n == 0), stop=(n == 7))
                                        n += 1
                                return ins
                            P.op('pe', tk, r=['Pkr%d' % i2, 'Pki%d' % i2, 'Cxr', 'Cxi'], w=['ptk%d' % dr])
                            li_ = 15 + k if dr == 0 else 15 - k
                            P.op('act', lambda e, dr=dr, li_=li_: e.copy(out=wlags[:, li_, :], in_=ptk[dr][:, 0:128]), r=['ptk%d' % dr], w=['wlags'])
                    for h in range(2):
                        def trw(e, i2=i2, h=h):
                            for m in range(8):
                                idx = h * 8 + m
                                cb, ri = idx // 2, idx % 2
                                ins = e.transpose(out=ptr[h][:, m, :], in_=(Pkr[i2] if ri == 0 else Pki[i2])[:, cb, :], identity=identb[:])
                            return ins
                        P.op('pe', trw, r=['Pkr%d' % i2, 'Pki%d' % i2, 'identb'], w=['ptr%d' % h])
                        P.op('act', lambda e, i2=i2, h=h: e.copy(out=winst[i2][:, h * 8:(h + 1) * 8, :], in_=ptr[h][:]), r=['ptr%d' % h], w=['winst%d' % i2])
                    P.dma('sp', WIN_d[T, k], winst[i2][:], 'wino%d' % i2, r=['winst%d' % i2], w=['winst%d' % i2])
                if k >= 1:
                    P.op('dve', lambda e, pkr=pkr: e.tensor_tensor(out=u1[:], in0=cre[:], in1=pkr, op=ALU.mult), r=['cre', 'prt'], w=['u1'])
                    P.op('dve', lambda e, pki=pki: e.tensor_tensor(out=u2[:], in0=cim[:], in1=pki, op=ALU.mult), r=['cim', 'pit'], w=['u2'])
                    P.op('dve', lambda e, i2=i2: e.tensor_tensor(out=CLr[i2][:], in0=u1[:], in1=u2[:], op=ALU.subtract), r=['u1', 'u2'], w=['CLr%d' % i2])
                    P.op('pool', lambda e, pki=pki: e.tensor_tensor(out=v1[:], in0=cre[:], in1=pki, op=ALU.mult), r=['cre', 'pit'], w=['v1'])
                    P.op('pool', lambda e, pkr=pkr: e.tensor_tensor(out=v2[:], in0=cim[:], in1=pkr, op=ALU.mult), r=['cim', 'prt'], w=['v2'])
                    P.op('pool', lambda e: e.tensor_tensor(out=v1[:], in0=v1[:], in1=v2[:], op=ALU.add), r=['v1', 'v2'], w=['v1'])
                    P.op('pool', lambda e, i2=i2: e.tensor_scalar(out=CLi[i2][:], in0=v1[:], scalar1=-1.0, scalar2=None, op0=ALU.mult), r=['v1'], w=['CLi%d' % i2])
                    wd = WOUT_d[T, k - 1].rearrange("p (c r) m -> p c r m", r=2)
                    P.dma('sp', wd[:, :, 0, :], CLr[i2][:], 'wouto%d' % i2, r=['CLr%d' % i2], w=['CLr%d' % i2])
                    P.dma('sp', wd[:, :, 1, :], CLi[i2][:], 'woutp%d' % i2, r=['CLi%d' % i2], w=['CLi%d' % i2])
            P.dma('sp', WLAG_d[T], wlags[:], 'wlago', r=['wlags'], w=['wlags'])
        P.barrier()
    P.new_sems()
    if upto < 4:
        return nc, P, {}
    UPW = 7968
    for b in range(NB):
        for T in range(4):
            if quick and (b > 0 or T > 0):
                continue
            with ExitStack() as st:
                def SB(name, shape, dt, b=b, T=T):
                    return st.enter_context(nc.sbuf_tensor("%s_%d_%d" % (name, b, T), list(shape), dt))

                def PS(name, shape, dt=F32, b=b, T=T):
                    return st.enter_context(nc.psum_tensor("%s_%d_%d" % (name, b, T), list(shape), dt))
                zs = SB("zs", [128, S], BF16)
                upad = SB("upad", [128, UPW], BF16)
                wlag = SB("wlag", [128, 31, 128], BF16)
                Hs = [SB("Hs%d" % i, [128, 257, 16], F32) for i in range(2)]
                Xh = SB("Xh", [128, 4, 2, 2, 256], BF16)
                Ssb = SB("Ssb", [128, 2, 256, 8], F32)
                wins = [SB("win%d" % i, [128, 16, 4, 128], BF16) for i in range(2)]
                wout = SB("wout_s", [128, 16, 16, 128], BF16)
                A12 = SB("A12", [128, 2, 16], F32)
                tP = [SB("tP%d" % i, [128, 16], F32) for i in range(2)]
                tQ = [SB("tQ%d" % i, [128, 8], F32) for i in range(2)]
                ygs = [SB("ygs%d" % i, [128, 512], BF16) for i in range(2)]
                ysum = [SB("ysum%d" % i, [128, 512], F32) for i in range(2)]
                crs = SB("crs", [128, 16, 128], F32)
                pss = [PS("pss%d" % i, [128, 512], F32) for i in range(4)]
                py = [PS("py%d" % i, [128, 512], F32) for i in range(2)]

                P.dma('sp', zs[:], zS_d[b, T], 'zs', w=['zs'])
                P.dma('sp', wlag[:], WLAG_d[T], 'wlag', w=['wlag'])
                P.dma('sp', wout[:], WOUT_d[T].rearrange("k p m c -> p k m c"), 'wout', w=['wout'])
                P.op('pool', lambda e: e.memset(upad[:], 0.0), w=['upad'])
                P.op('pool', lambda e: e.tensor_copy(out=AP(upad, 15, [[UPW, 128], [31, 256], [1, 16]]),
                                                     in_=AP(zs, 0, [[S, 128], [16, 256], [1, 16]])), r=['zs'], w=['upad'])
                for dr in range(2):
                    src_r = AP(prt, 16 * 32 + T * 8 + dr, [[17 * 32, 128], [2, 4]])
                    src_i = AP(pit, 16 * 32 + T * 8 + dr, [[17 * 32, 128], [2, 4]])
                    P.op('dve', lambda e, dr=dr, src_r=src_r: e.tensor_copy(out=A12[:, dr, 0:4], in_=src_r), r=['prt'], w=['A12'])
                    P.op('dve', lambda e, dr=dr, src_r=src_r: e.tensor_copy(out=A12[:, dr, 4:8], in_=src_r), r=['prt'], w=['A12'])
                    P.op('dve', lambda e, dr=dr, src_i=src_i: e.tensor_scalar(out=A12[:, dr, 8:12], in0=src_i, scalar1=-1.0, scalar2=None, op0=ALU.mult), r=['pit'], w=['A12'])
                    P.op('dve', lambda e, dr=dr, src_i=src_i: e.tensor_copy(out=A12[:, dr, 12:16], in_=src_i), r=['pit'], w=['A12'])
                P.op('dve', lambda e: e.memset(Hs[0][:], 0.0), w=['H0'])
                P.op('pool', lambda e: e.memset(Hs[1][:], 0.0), w=['H1'])
                for q in range(4):
                    win = wins[q % 2]
                    wkey = 'win%d' % (q % 2)
                    P.dma('sp', win[:], WIN_d[T, :, :, q * 4:(q + 1) * 4, :].rearrange("k p m c -> p k m c"), wkey, w=[wkey])
                    for dr in range(2):
                        for ri in range(2):
                            pi_ = dr * 2 + ri

                            def smm(e, dr=dr, ri=ri, pi_=pi_, win=win):
                                for jp in range(16):
                                    kk = 15 - jp if dr == 0 else jp
                                    ins = e.matmul(pss[pi_][:, 0:256], lhsT=win[:, kk, dr * 2 + ri, :],
                                                   rhs=AP(upad, 15 + jp, [[UPW, 128], [31, 256]]), start=(jp == 0), stop=(jp == 15))
                                return ins
                            P.op('pe', smm, r=[wkey, 'upad'], w=['pss%d' % pi_])
                            P.op('act', lambda e, dr=dr, ri=ri, q=q, pi_=pi_: e.copy(out=AP(Ssb, dr * 2048 + ri * 4 + q, [[4096, 128], [8, 256]]),
                                                                                     in_=pss[pi_][:, 0:256]), r=['pss%d' % pi_], w=['Ssb'])
                for step in range(256 if not quick else 256):
                    for dr, eng in ((0, 'dve'), (1, 'pool')):
                        H = Hs[dr]
                        if dr == 0:
                            src, dst, sc_ = step, step + 1, step
                        else:
                            src, dst, sc_ = 256 - step, 255 - step, 255 - step
                        hk = 'H%d' % dr
                        HW = 257 * 16
                        P.op(eng, lambda e, H=H, src=src, dr=dr, HW=HW: e.tensor_tensor(out=AP(tP[dr], 0, [[16, 128], [8, 2], [1, 8]]),
                                                                                 in0=AP(H, src * 16, [[HW, 128], [4, 2], [1, 8]]),
                                                                                 in1=AP(A12, dr * 16, [[32, 128], [8, 2], [1, 8]]), op=ALU.mult),
                             r=[hk, 'A12'], w=['tP%d' % dr])
                        P.op(eng, lambda e, dr=dr: e.tensor_tensor(out=tQ[dr][:], in0=tP[dr][:, 0:8], in1=tP[dr][:, 8:16], op=ALU.add),
                             r=['tP%d' % dr], w=['tQ%d' % dr])
                        P.op(eng, lambda e, H=H, dst=dst, dr=dr, sc_=sc_, HW=HW: e.tensor_tensor(out=AP(H, dst * 16, [[HW, 128], [8, 2], [1, 8]]),
                                                                                          in0=AP(tQ[dr], 0, [[8, 128], [0, 2], [1, 8]]),
                                                                                          in1=AP(Ssb, dr * 2048 + sc_ * 8, [[4096, 128], [0, 2], [1, 8]]), op=ALU.add),
                             r=['tQ%d' % dr, 'Ssb'], w=[hk])
                P.op('dve', lambda e: e.tensor_copy(out=AP(Xh, 0, [[4096, 128], [1024, 4], [256, 2], [1, 256]]),
                                                    in_=AP(Hs[0], 0, [[257 * 16, 128], [1, 4], [4, 2], [16, 256]])), r=['H0'], w=['Xh'])
                P.op('pool', lambda e: e.tensor_copy(out=AP(Xh, 512, [[4096, 128], [1024, 4], [256, 2], [1, 256]]),
                                                     in_=AP(Hs[1], 16, [[257 * 16, 128], [1, 4], [4, 2], [16, 256]])), r=['H1'], w=['Xh'])
                for half in range(2):
                    h0 = half * 128
                    for g4 in range(4):
                        def cmm(e, g4=g4, h0=h0):
                            for jj in range(4):
                                j = g4 * 4 + jj
                                n = 0
                                for q in range(4):
                                    for dr in range(2):
                                        kidx = j if dr == 0 else 15 - j
                                        for ri in range(2):
                                            ins = e.matmul(pss[g4][:, jj * 128:(jj + 1) * 128], lhsT=wout[:, kidx, q * 4 + dr * 2 + ri, :],
                                                           rhs=Xh[:, q, dr, ri, h0:h0 + 128], start=(n == 0), stop=(n == 15))
                                            n += 1
                            return ins
                        P.op('pe', cmm, r=['wout', 'Xh'], w=['pss%d' % g4])
                        P.op('act', lambda e, g4=g4: e.copy(out=crs[:, g4 * 4:(g4 + 1) * 4, :], in_=pss[g4][:, :]), r=['pss%d' % g4], w=['crs'])
                    for t4 in range(4):
                        tt_ = half * 4 + t4
                        c0 = tt_ * 32
                        ip = tt_ % 2

                        def ymm(e, c0=c0, ip=ip):
                            outv = AP(py[ip], 0, [[512, 128], [16, 32], [1, 16]])
                            for li_ in range(31):
                                dl = li_ - 15
                                ins = e.matmul(outv, lhsT=wlag[:, li_, :], rhs=AP(upad, 15 + c0 * 31 - dl, [[UPW, 128], [31, 32], [1, 16]]),
                                               start=(li_ == 0), stop=(li_ == 30))
                            return ins
                        P.op('pe', ymm, r=['wlag', 'upad'], w=['py%d' % ip])
                        P.op('dve', lambda e, ip=ip, t4=t4: e.tensor_tensor(out=AP(ysum[ip], 0, [[512, 128], [16, 32], [1, 16]]),
                                                                            in0=AP(py[ip], 0, [[512, 128], [16, 32], [1, 16]]),
                                                                            in1=AP(crs, t4 * 32, [[2048, 128], [1, 32], [128, 16]]), op=ALU.add),
                             r=['py%d' % ip, 'crs'], w=['ysum%d' % ip])
                        P.op('act', lambda e, ip=ip: e.activation(out=ygs[ip][:], in_=ysum[ip][:], func=AF.Gelu), r=['ysum%d' % ip], w=['ygs%d' % ip])
                        P.dma('sp', yG_d[b, T, :, tt_ * 512:(tt_ + 1) * 512], ygs[ip][:], 'ygo%d' % ip, r=['ygs%d' % ip], w=['ygs%d' % ip])
                P.barrier()
            P.new_sems()
    if upto < 5:
        return nc, P, {}
    with ExitStack() as st:
        def SB(name, shape, dt):
            return st.enter_context(nc.sbuf_tensor(name, list(shape), dt))

        def PS(name, shape, dt=F32):
            return st.enter_context(nc.psum_tensor(name, list(shape), dt))
        stg = SB("stg", [128, 8, D], F32)
        wglub = SB("wglub", [128, 4, 512], BF16)
        woutb = SB("woutb", [128, 8, D], BF16)
        P.dma('sp', AP(stg, 0, [[8 * D, 128], [1, 2048]]), wglu.rearrange("p t n -> p (t n)"), 'c_wglu', w=['stg'])
        P.op('dve', lambda e: e.tensor_copy(out=wglub[:], in_=AP(stg, 0, [[8 * D, 128], [512, 4], [1, 512]])), r=['stg'], w=['wglub'])
        P.dma('sp', stg[:], woutw[:, :, :], 'c_wout', r=['wglub'], w=['stg'])
        P.op('dve', lambda e: e.tensor_copy(out=woutb[:], in_=stg[:]), r=['stg'], w=['woutb'])
        ygb = [SB("ygb%d" % i, [128, 4, 128], BF16) for i in range(2)]
        yfb = [SB("yfb%d" % i, [128, 4, 128], BF16) for i in range(2)]
        xs = [SB("xsd%d" % i, [128, D], F32) for i in range(2)]
        sg = SB("sg", [128, 4, 128], BF16)
        ysg = [SB("ysg%d" % i, [128, 4, 128], BF16) for i in range(2)]
        x1s = [SB("x1s%d" % i, [128, D], F32) for i in range(2)]
        sq = SB("sqd", [128, D], F32)
        ss = [SB("ssd%d" % i, [128, 4], F32) for i in range(2)]
        xn2 = [SB("xn2%d" % i, [128, D], BF16) for i in range(2)]
        hT2 = [SB("hT2%d" % i, [128, 8, 128], BF16) for i in range(2)]
        pg = PS("pg", [128, 4, 128], F32)
        po = [PS("pod%d" % i, [128, 512], F32) for i in range(2)]
        pT2 = PS("pT2", [128, 8, 128], BF16)
        nblk = NB * (S // 128)
        def d_s1(blk):
                b, tb_ = divmod(blk, S // 128)
                i = blk % 2
                t0 = tb_ * 128
                P.dma('sp', ygb[i][:], yG_d[b, :, :, t0:t0 + 128].rearrange("t p s -> p t s"), 'ygb%d' % i, w=['ygb%d' % i])
                P.dma('sp', yfb[i][:], yF_d[b, :, :, t0:t0 + 128].rearrange("t p s -> p t s"), 'yfb%d' % i, w=['yfb%d' % i])
                P.dma('sp', xs[i][:], x[b, t0:t0 + 128, :], 'xsd%d' % i, w=['xsd%d' % i])

                def glu(e, i=i):
                    for n in range(4):
                        for T in range(4):
                            ins = e.matmul(pg[:, n, :], lhsT=wglub[:, T, n * 128:(n + 1) * 128], rhs=ygb[i][:, T, :], start=(T == 0), stop=(T == 3))
                    return ins
                P.op('pe', glu, r=['wglub', 'ygb%d' % i], w=['pg'])
                P.op('act', lambda e: e.activation(out=sg[:], in_=pg[:], func=AF.Sigmoid), r=['pg'], w=['sg'])
                P.op('dve', lambda e, i=i: e.tensor_tensor(out=ysg[i][:], in0=sg[:], in1=ygb[i][:], op=ALU.mult), r=['sg', 'ygb%d' % i], w=['ysg%d' % i])

        def d_s2(blk):
                b, tb_ = divmod(blk, S // 128)
                i = blk % 2
                t0 = tb_ * 128
                for hf in range(2):
                    def omm(e, i=i, hf=hf):
                        for c8 in range(8):
                            lt = ysg[i][:, c8, :] if c8 < 4 else yfb[i][:, c8 - 4, :]
                            ins = e.matmul(po[hf][:, :], lhsT=lt, rhs=woutb[:, c8, hf * 512:(hf + 1) * 512], start=(c8 == 0), stop=(c8 == 7))
                        return ins
                    P.op('pe', omm, r=['ysg%d' % i, 'yfb%d' % i, 'woutb'], w=['pod%d' % hf])
                    P.op('dve', lambda e, i=i, hf=hf: e.tensor_tensor(out=x1s[i][:, hf * 512:(hf + 1) * 512], in0=po[hf][:, :], in1=xs[i][:, hf * 512:(hf + 1) * 512], op=ALU.add),
                         r=['pod%d' % hf, 'xsd%d' % i], w=['x1s%d_%d' % (i, hf)])
                P.dma('sp', x1_d[b, t0:t0 + 128, :], x1s[i][:], 'x1o%d' % i, r=['x1s%d_0' % i, 'x1s%d_1' % i], w=['x1o%d' % i])
                P.op('act', lambda e, i=i: e.activation(out=sq[:], in_=x1s[i][:], func=AF.Square, accum_out=ss[i][:, 0:1]),
                     r=['x1s%d_0' % i, 'x1s%d_1' % i], w=['sqd', 'ssa%d' % i])
                P.op('act', lambda e, i=i: e.activation(out=ss[i][:, 1:2], in_=ss[i][:, 0:1], func=AF.Sqrt, scale=1.0 / D, bias=epsc[:, 0:1]),
                     r=['ssa%d' % i, 'epsc'], w=['ssb%d' % i])
                P.op('dve', lambda e, i=i: e.reciprocal(out=ss[i][:, 2:3], in_=ss[i][:, 1:2]), r=['ssb%d' % i], w=['ssc%d' % i])
                P.op('dve', lambda e, i=i: e.tensor_scalar(out=xn2[i][:], in0=x1s[i][:], scalar1=ss[i][:, 2:3], scalar2=None, op0=ALU.mult),
                     r=['x1s%d_0' % i, 'x1s%d_1' % i, 'ssc%d' % i], w=['xn2%d' % i])


        def d_s3(blk):
                b, tb_ = divmod(blk, S // 128)
                i = blk % 2
                t0 = tb_ * 128
                def tr2(e, i=i):
                    for kc in range(8):
                        ins = e.transpose(out=pT2[:, kc, :], in_=xn2[i][:, kc * 128:(kc + 1) * 128], identity=identb[:])
                    return ins
                P.op('pe', tr2, r=['xn2%d' % i, 'identb'], w=['pT2'])
                P.op('act', lambda e, i=i: e.copy(out=hT2[i][:], in_=pT2[:]), r=['pT2'], w=['hT2%d' % i])
                P.dma('sp', h2T_d[b, :, :, t0:t0 + 128], hT2[i][:], 'h2o%d' % i, r=['hT2%d' % i], w=['hT2%d' % i])


        nbd = nblk if not quick else 2
        for step in range(nbd + 2):
            if step < nbd:
                d_s1(step)
            if 0 <= step - 1 < nbd:
                d_s2(step - 1)
            if 0 <= step - 2 < nbd:
                d_s3(step - 2)
        P.barrier()
    P.new_sems()
    if upto < 6:
        return nc, P, {}
    keysT_in = din("keysT_in", [128, 16, 128])
    iota128_in = din("iota128", [128, 128])
    iota16_in = din("iota16", [128, 16])
    G_d = dscr("G_d", [64, 128, 128, 128])
    wqb = SBp("wqb", [128, 8, 2048], BF16)
    keysb = SBp("keysb", [128, 16, 128], BF16)
    iota128 = SBp("iota128s", [128, 128], F32)
    iota16 = SBp("iota16s", [128, 16], F32)
    P.dma('sp', iota128[:], iota128_in[:, :], 'c_io128', w=['iota128'])
    P.dma('sp', iota16[:], iota16_in[:, :], 'c_io16', w=['iota16'])
    with ExitStack() as st:
        def SB(name, shape, dt):
            return st.enter_context(nc.sbuf_tensor(name, list(shape), dt))
        stq = SB("stq", [128, 4, 2048], F32)
        kst = SB("kst", [128, 16, 128], F32)
        P.dma('sp', kst[:], keysT_in[:, :, :], 'c_keys', w=['kst'])
        for hq in range(2):
            P.dma('sp', stq[:], wq[:, hq * 4:(hq + 1) * 4, :], 'c_wq', w=['stq'])
            for kk in range(4):
                kc = hq * 4 + kk
                P.op('dve', lambda e, kc=kc, kk=kk: e.tensor_scalar(out=wqb[:, kc, :], in0=stq[:, kk, :], scalar1=g2s[:, kc:kc + 1], scalar2=None, op0=ALU.mult),
                     r=['stq', 'g2s'], w=['wqb'])
        P.op('pool', lambda e: e.tensor_copy(out=keysb[:], in_=kst[:]), r=['kst'], w=['keysb'])
        P.barrier()
    P.new_sems()

    with ExitStack() as st:
        def SB(name, shape, dt):
            return st.enter_context(nc.sbuf_tensor(name, list(shape), dt))

        def PS(name, shape, dt=F32):
            return st.enter_context(nc.psum_tensor(name, list(shape), dt))
        NT = 128
        h2s = [SB("h2_%d" % i, [128, 8, NT], BF16) for i in range(2)]
        qTs = [SB("qT_%d" % i, [128, 16, NT], BF16) for i in range(2)]
        scss = [SB("scs_%d" % i, [128, 16, 128], F32) for i in range(2)]
        scr = SB("scr", [128, 256], F32)
        v16 = SB("v16", [128, 16, 16], F32)
        ix16 = SB("ix16", [128, 16, 16], U32)
        ixf = SB("ixf", [128, 16, 16], F32)
        cand = SB("cand", [128, 8, 256], F32)
        tv = SB("tv", [128, 8, 16], F32)
        tve = SB("tve", [128, 8, 16], F32)
        pos = SB("pos", [128, 8, 16], U32)
        posf = SB("posf", [128, 8, 16], F32)
        paf = SB("paf", [128, 8, 16], F32); pbf = SB("pbf", [128, 8, 16], F32)
        eq = SB("eq", [128, 8, 16, 16], F32)
        i16a = SB("i16a", [128, 16], F32); i16b = SB("i16b", [128, 16], F32)
        sel = SB("sel", [128, 3, 128], F32)
        selb = SB("selb", [128, 3, 128], BF16)
        selT = SB("selT", [128, 3, 128], BF16)
        iob = SB("iob", [128, 128], BF16)
        zsum = SB("zsum", [128, 8], F32)
        OJs = [SB("OJ%d" % i, [128, 32, 128], BF16) for i in range(2)]
        OIs = [SB("OI%d" % i, [128, 32, 128], BF16) for i in range(2)]
        Gsb = [SB("Gs%d" % i, [128, 128, NT], BF16) for i in range(2)]
        Bk = [PS("Bk%d" % i, [128, 512], F32) for i in range(7)]
        Bk7b = PS("Bk7b", [128, 1024], BF16)
        eq2 = cand

        def bk(i):
            return 'Bk%d' % i
        P.op('dve', lambda e: e.tensor_copy(out=iob[:], in_=iota128[:]), r=['iota128'], w=['iob'])
        P.op('dve', lambda e: e.tensor_scalar(out=i16a[:], in0=iota16[:], scalar1=16.0, scalar2=None, op0=ALU.mult), r=['iota16'], w=['i16a'])
        P.op('dve', lambda e: e.tensor_scalar(out=i16b[:], in0=iota16[:], scalar1=16.0, scalar2=16.0, op0=ALU.mult, op1=ALU.add), r=['iota16'], w=['i16b'])
        nblk = NB * S // NT
        def front(blk):
                b, tb_ = divmod(blk, S // NT)
                t0 = tb_ * NT
                par = blk % 2
                h2 = h2s[par]; qT = qTs[par]; scs = scss[par]
                Gs = Gsb[blk % 2]
                gsk = 'Gs%d' % (blk % 2)
                P.dma('sp', h2[:], h2T_d[b, :, :, t0:t0 + NT], 'h2_%d' % par, w=['h2_%d' % par])
                for m in range(16):
                    pb_ = 4 + (m % 2)

                    def qmm(e, m=m, pb_=pb_):
                        for kc in range(8):
                            ins = e.matmul(Bk[pb_][:, 0:NT], lhsT=wqb[:, kc, m * 128:(m + 1) * 128], rhs=h2[:, kc, :], start=(kc == 0), stop=(kc == 7))
                        return ins
                    P.op('pe', qmm, r=['wqb', 'h2_%d' % par], w=[bk(pb_)])
                    P.op('act', lambda e, m=m, pb_=pb_: e.copy(out=qT[:, m, :], in_=Bk[pb_][:, 0:NT]), r=[bk(pb_)], w=['qT_%d' % par])
                for m4 in range(4):
                    def smm2(e, m4=m4):
                        for mm in range(4):
                            m = m4 * 4 + mm
                            ins = e.matmul(Bk[m4][:, mm * 128:(mm + 1) * 128], lhsT=qT[:, m, :], rhs=keysb[:, m, :], start=True, stop=True)
                        return ins
                    P.op('pe', smm2, r=['qT_%d' % par, 'keysb'], w=[bk(m4)])
                    P.op('act', lambda e, m4=m4: e.copy(out=scs[:, m4 * 4:(m4 + 1) * 4, :], in_=Bk[m4][:, :]), r=[bk(m4)], w=['scs_%d' % par])

        def mid_a(blk):
                b, tb_ = divmod(blk, S // NT)
                t0 = tb_ * NT
                par = blk % 2
                h2 = h2s[par]; qT = qTs[par]; scs = scss[par]
                for m in range(16):
                    P.op('dve', lambda e, m=m: e.max(out=v16[:, m, 0:8], in_=scs[:, m, :]), r=['scs_%d' % par], w=['v16'])
                    P.op('dve', lambda e, m=m: e.max_index(out=ix16[:, m, 0:8], in_max=v16[:, m, 0:8], in_values=scs[:, m, :]), r=['scs_%d' % par, 'v16'], w=['ix16'])
                    P.op('dve', lambda e, m=m: e.match_replace(out=scr[:, 0:128], in_to_replace=v16[:, m, 0:8], in_values=scs[:, m, :], imm_value=-1e30),
                         r=['scs_%d' % par, 'v16'], w=['scr'])
                    P.op('dve', lambda e, m=m: e.max(out=v16[:, m, 8:16], in_=scr[:, 0:128]), r=['scr'], w=['v16'])
                    P.op('dve', lambda e, m=m: e.max_index(out=ix16[:, m, 8:16], in_max=v16[:, m, 8:16], in_values=scr[:, 0:128]), r=['scr', 'v16'], w=['ix16'])
                P.op('dve', lambda e: e.tensor_copy(out=ixf[:], in_=ix16[:]), r=['ix16'], w=['ixf'])
                P.op('dve', lambda e: e.tensor_tensor(out=AP(cand, 0, [[2048, 128], [256, 8], [16, 16], [1, 16]]),
                                                      in0=AP(v16, 0, [[256, 128], [32, 8], [1, 16], [0, 16]]),
                                                      in1=AP(v16, 16, [[256, 128], [32, 8], [0, 16], [1, 16]]), op=ALU.add), r=['v16'], w=['cand'])
                for h in range(8):
                    P.op('dve', lambda e, h=h: e.max(out=tv[:, h, 0:8], in_=cand[:, h, :]), r=['cand'], w=['tv'])
                    P.op('dve', lambda e, h=h: e.max_index(out=pos[:, h, 0:8], in_max=tv[:, h, 0:8], in_values=cand[:, h, :]), r=['cand', 'tv'], w=['pos'])
                    P.op('dve', lambda e, h=h: e.match_replace(out=scr[:, 0:256], in_to_replace=tv[:, h, 0:8], in_values=cand[:, h, :], imm_value=-1e30),
                         r=['cand', 'tv'], w=['scr'])
                    P.op('dve', lambda e, h=h: e.max(out=tv[:, h, 8:16], in_=scr[:, 0:256]), r=['scr'], w=['tv'])
                    P.op('dve', lambda e, h=h: e.max_index(out=pos[:, h, 8:16], in_max=tv[:, h, 8:16], in_values=scr[:, 0:256]), r=['scr', 'tv'], w=['pos'])

        def gate_(blk):
                b, tb_ = divmod(blk, S // NT)
                t0 = tb_ * NT
                par = blk % 2
                h2 = h2s[par]; qT = qTs[par]; scs = scss[par]
                P.op('dve', lambda e: e.tensor_tensor(out=tve[:], in0=tv[:], in1=AP(tv, 0, [[128, 128], [16, 8], [0, 16]]), op=ALU.subtract), r=['tv'], w=['tve'])
                P.op('act', lambda e: e.activation(out=tve[:], in_=tve[:], func=AF.Exp), r=['tve'], w=['tve'])

        def mid_b(blk):
                b, tb_ = divmod(blk, S // NT)
                t0 = tb_ * NT
                par = blk % 2
                h2 = h2s[par]; qT = qTs[par]; scs = scss[par]
                P.op('dve', lambda e: e.tensor_copy(out=posf[:], in_=pos[:]), r=['pos'], w=['posf'])
                posb = AP(posf, 0, [[128, 128], [16, 8], [1, 16], [0, 16]])
                P.op('dve', lambda e, posb=posb: e.tensor_tensor(out=eq[:], in0=posb, in1=AP(i16a, 0, [[16, 128], [0, 8], [0, 16], [1, 16]]), op=ALU.is_ge),
                     r=['posf', 'i16a'], w=['eq'])
                P.op('dve', lambda e, posb=posb: e.tensor_tensor(out=eq2[:].rearrange("p h (a b) -> p h a b", b=16) if False else AP(cand, 0, [[2048, 128], [256, 8], [16, 16], [1, 16]]),
                                                                in0=posb, in1=AP(i16b, 0, [[16, 128], [0, 8], [0, 16], [1, 16]]), op=ALU.is_ge),
                     r=['posf', 'i16b', 'pos'], w=['cand'])
                P.op('dve', lambda e: e.tensor_tensor(out=eq[:], in0=eq[:], in1=AP(cand, 0, [[2048, 128], [256, 8], [16, 16], [1, 16]]), op=ALU.subtract), r=['eq', 'cand'], w=['eq'])
                c4 = AP(cand, 0, [[2048, 128], [256, 8], [16, 16], [1, 16]])
                c3 = AP(cand, 0, [[2048, 128], [16, 128], [1, 16]])
                P.op('dve', lambda e, c4=c4: e.tensor_tensor(out=c4, in0=eq[:], in1=AP(ixf, 0, [[256, 128], [32, 8], [0, 16], [1, 16]]), op=ALU.mult), r=['eq', 'ixf'], w=['cand'])
                P.op('dve', lambda e, c3=c3: e.tensor_reduce(out=sel[:, 0, :], in_=c3, axis=AX.X, op=ALU.add), r=['cand'], w=['sel'])
                P.op('dve', lambda e, c4=c4: e.tensor_tensor(out=c4, in0=eq[:], in1=AP(iota16, 0, [[16, 128], [0, 8], [0, 16], [1, 16]]), op=ALU.mult), r=['eq', 'iota16'], w=['cand'])
                P.op('dve', lambda e, c3=c3: e.tensor_reduce(out=paf[:], in_=c3, axis=AX.X, op=ALU.add), r=['cand'], w=['paf'])
                P.op('dve', lambda e: e.scalar_tensor_tensor(out=pbf[:], in0=paf[:], scalar=-16.0, in1=posf[:], op0=ALU.mult, op1=ALU.add), r=['paf', 'posf'], w=['pbf'])
                P.op('dve', lambda e: e.tensor_tensor(out=eq[:], in0=AP(iota16, 0, [[16, 128], [0, 8], [0, 16], [1, 16]]),
                                                      in1=AP(pbf, 0, [[128, 128], [16, 8], [1, 16], [0, 16]]), op=ALU.is_equal), r=['iota16', 'pbf'], w=['eq'])
                P.op('dve', lambda e, c4=c4: e.tensor_tensor(out=c4, in0=eq[:], in1=AP(ixf, 16, [[256, 128], [32, 8], [0, 16], [1, 16]]), op=ALU.mult), r=['eq', 'ixf'], w=['cand'])
                P.op('dve', lambda e, c3=c3: e.tensor_reduce(out=sel[:, 1, :], in_=c3, axis=AX.X, op=ALU.add), r=['cand'], w=['sel'])
                P.op('dve', lambda e: e.tensor_reduce(out=zsum[:], in_=tve[:], axis=AX.X, op=ALU.add), r=['tve'], w=['zsum'])
                P.op('dve', lambda e: e.reciprocal(out=zsum[:], in_=zsum[:]), r=['zsum'], w=['zsum'])
                P.op('dve', lambda e: e.tensor_tensor(out=AP(sel, 256, [[384, 128], [16, 8], [1, 16]]), in0=tve[:], in1=AP(zsum, 0, [[8, 128], [1, 8], [0, 16]]), op=ALU.mult),
                     r=['tve', 'zsum'], w=['sel'])
                P.op('dve', lambda e: e.tensor_copy(out=selb[:], in_=sel[:]), r=['sel'], w=['selb'])

        def tail(blk):
                b, tb_ = divmod(blk, S // NT)
                t0 = tb_ * NT
                par = blk % 2
                h2 = h2s[par]; qT = qTs[par]; scs = scss[par]
                Gs = Gsb[blk % 2]
                gsk = 'Gs%d' % (blk % 2)

                def trs(e):
                    for c3_ in range(3):
                        ins = e.transpose(out=AP(Bk7b, c3_ * 128, [[1024, 128], [1, 128]]), in_=selb[:, c3_, :], identity=identb[:])
                    return ins
                P.op('pe', trs, r=['selb', 'identb'], w=['Bk7b'])
                P.op('act', lambda e: e.copy(out=selT[:], in_=AP(Bk7b, 0, [[1024, 128], [128, 3], [1, 128]])), r=['Bk7b'], w=['selT'])
                for tg in range(4):
                    io_b = AP(iob, 0, [[128, 128], [0, 32], [1, 128]])
                    OJ = OJs[tg % 2]; OI = OIs[tg % 2]; ojk = 'OJ%d' % (tg % 2); oik = 'OI%d' % (tg % 2)
                    P.op('dve', lambda e, io_b=io_b, tg=tg, OJ=OJ: e.tensor_tensor(out=OJ[:], in0=io_b, in1=AP(selT, 128 + tg * 32, [[384, 128], [1, 32], [0, 128]]), op=ALU.is_equal),
                         r=['iob', 'selT'], w=[ojk])
                    P.op('dve', lambda e, io_b=io_b, tg=tg, OI=OI: e.tensor_tensor(out=OI[:], in0=io_b, in1=AP(selT, tg * 32, [[384, 128], [1, 32], [0, 128]]), op=ALU.is_equal),
                         r=['iob', 'selT'], w=[oik])
                    P.op('pool', lambda e, tg=tg, OI=OI: e.tensor_tensor(out=OI[:], in0=OI[:], in1=AP(selT, 256 + tg * 32, [[384, 128], [1, 32], [0, 128]]), op=ALU.mult),
                         r=[oik, 'selT'], w=[oik])
                    for t4 in range(8):
                        pbk = (6, 4, 5, 0, 1, 2, 3)[(tg * 8 + t4) % 7]

                        def gmm(e, t4=t4, pbk=pbk, OJ=OJ, OI=OI):
                            for tq in range(4):
                                tl = t4 * 4 + tq
                                ins = e.matmul(Bk[pbk][:, tq * 128:(tq + 1) * 128], lhsT=OJ[:, tl, :], rhs=OI[:, tl, :], start=True, stop=True)
                            return ins
                        P.op('pe', gmm, r=[ojk, oik], w=[bk(pbk)])
                        P.op('act', lambda e, t4=t4, pbk=pbk, tg=tg, Gs=Gs: e.copy(out=AP(Gs, tg * 32 + t4 * 4, [[128 * NT, 128], [1, 4], [NT, 128]]),
                                                                           in_=AP(Bk[pbk], 0, [[512, 128], [128, 4], [1, 128]])), r=[bk(pbk)], w=[gsk])
                P.dma('sp', G_d[blk], Gs[:], 'gdo%d' % (blk % 2), r=[gsk], w=[gsk, 'Gd%d' % blk])

        nb1 = nblk if not quick else 1
        front(0)
        for blk in range(nb1):
            mid_a(blk)
            gate_(blk)
            if blk + 1 < nb1:
                front(blk + 1)
            mid_b(blk)
            tail(blk)
        P.barrier()

    with ExitStack() as st:
        def SB(name, shape, dt):
            return st.enter_context(nc.sbuf_tensor(name, list(shape), dt))

        def PS(name, shape, dt=F32):
            return st.enter_context(nc.psum_tensor(name, list(shape), dt))
        NTM = 384
        h2e = SB("h2e", [128, 8, NTM], BF16)
        utb = [SB("utb%d" % i, [128, 8, 128], BF16) for i in range(8)]
        vtb = [SB("vtb%d" % i, [128, 1024], BF16) for i in range(8)]
        gq = [SB("gq%d" % i, [128, 3, 4, 128], BF16) for i in range(2)]
        glb = [SB("glb%d" % i, [128, NTM], BF16) for i in range(2)]
        actb = [SB("actb%d" % i, [128, NTM], BF16) for i in range(2)]
        x1b = SB("x1b", [128, D], F32)
        o2 = SB("o2", [128, D], F32)
        sqe = SB("sqe", [128, D], F32)
        sse = SB("sse", [128, 4], F32)
        Bo = [PS("Bo%d" % i, [128, 512], F32) for i in range(6)]
        Bp = [PS("Bp%d" % i, [128, 512], F32) for i in range(2)]
        blocks = []
        for b in range(NB):
            t = 0
            for nt in [384] * 10 + [256]:
                blocks.append((b, t, nt))
                t += nt
        for (b, t0, nt) in (blocks if not quick else blocks[:1]):
            nsub = nt // 128
            tb0 = (b * S + t0) // 128
            P.dma('sp', h2e[:, :, 0:nt], h2T_d[b, :, :, t0:t0 + nt], 'h2e', w=['h2e'])
            def emit_pre(ci):
                sl = ci % 8
                P.dma('sp', utb[sl][:], UTb_d[ci], 'utb%d' % sl, w=['utb%d' % sl])
                P.dma('pool', vtb[sl][:], Vb_d[ci], 'vtb%d' % sl, w=['vtb%d' % sl])
                gsl = (ci // 4) % 2
                if ci % 4 == 0:
                    for sub in range(nsub):
                        P.dma('sp', gq[gsl][:, sub, :, :], G_d[tb0 + sub, :, ci:ci + 4, :], 'gq%d' % gsl, r=['Gd%d' % (tb0 + sub)], w=['gq%d' % gsl])
                pp = ci % 2

                def pmm(e, sl=sl, pp=pp, nt=nt):
                    for kc in range(8):
                        ins = e.matmul(Bp[pp][:, 0:nt], lhsT=utb[sl][:, kc, :], rhs=h2e[:, kc, 0:nt], start=(kc == 0), stop=(kc == 7))
                    return ins
                P.op('pe', pmm, r=['utb%d' % sl, 'h2e'], w=['Bp%d' % pp])
                P.op('act', lambda e, pp=pp, nt=nt: e.activation(out=glb[pp][:, 0:nt], in_=Bp[pp][:, 0:nt], func=AF.Gelu), r=['Bp%d' % pp], w=['glb%d' % pp])
                P.op('dve', lambda e, pp=pp, nt=nt, nsub=nsub, gsl=gsl, ci=ci: e.tensor_tensor(
                    out=AP(actb[pp], 0, [[NTM, 128], [128, nsub], [1, 128]]), in0=AP(glb[pp], 0, [[NTM, 128], [128, nsub], [1, 128]]),
                    in1=AP(gq[gsl], (ci % 4) * 128, [[1536, 128], [512, nsub], [1, 128]]), op=ALU.mult),
                    r=['glb%d' % pp, 'gq%d' % gsl], w=['actb%d' % pp])

            def emit_out(ci):
                sl = ci % 8
                pp = ci % 2
                def omm(e, sl=sl, pp=pp, nsub=nsub, ci=ci):
                    for sub in range(nsub):
                        for hf in range(2):
                            ins = e.matmul(Bo[sub * 2 + hf][:, :], lhsT=actb[pp][:, sub * 128:(sub + 1) * 128], rhs=vtb[sl][:, hf * 512:(hf + 1) * 512],
                                           start=(ci == 0), stop=(ci == 127))
                    return ins
                P.op('pe', omm, r=['actb%d' % pp, 'vtb%d' % sl], w=['Bo'])
            emit_pre(0)
            for ci in range(128):
                if ci + 1 < 128:
                    emit_pre(ci + 1)
                emit_out(ci)
            for sub in range(nsub):
                tt0 = t0 + sub * 128
                P.dma('sp', x1b[:], x1_d[b, tt0:tt0 + 128, :], 'x1b', w=['x1b'])
                for hf in range(2):
                    P.op('dve', lambda e, sub=sub, hf=hf: e.tensor_tensor(out=o2[:, hf * 512:(hf + 1) * 512], in0=Bo[sub * 2 + hf][:, :], in1=x1b[:, hf * 512:(hf + 1) * 512], op=ALU.add),
                         r=['Bo', 'x1b'], w=['o2_%d' % hf])
                P.op('act', lambda e: e.activation(out=sqe[:], in_=o2[:], func=AF.Square, accum_out=sse[:, 0:1]), r=['o2_0', 'o2_1'], w=['sqe', 'ssea'])
                P.op('act', lambda e: e.activation(out=sse[:, 1:2], in_=sse[:, 0:1], func=AF.Sqrt, scale=1.0 / D, bias=epsc[:, 0:1]), r=['ssea', 'epsc'], w=['sseb'])
                P.op('dve', lambda e: e.reciprocal(out=sse[:, 2:3], in_=sse[:, 1:2]), r=['sseb'], w=['ssec'])
                P.op('dve', lambda e: e.scalar_tensor_tensor(out=sqe[:], in0=o2[:], scalar=sse[:, 2:3], in1=gfin_s[:], op0=ALU.mult, op1=ALU.mult),
                     r=['o2_0', 'o2_1', 'ssec', 'gfin', 'sqe'], w=['sqe'])
                P.dma('sp', y[b, tt0:tt0 + 128, :], sqe[:], 'yo', r=['sqe'], w=['yo'])
        P.barrier()
    return nc, P, {}


def host_inputs(inp):
    f = np.float32

    def kmaj(w):
        K, N = w.shape
        return np.ascontiguousarray(w.reshape(K // 128, 128, N).transpose(1, 0, 2)).astype(f)
    com = {}
    com["w1"] = kmaj(inp["w_in"][0])
    com["g1"] = np.ascontiguousarray(inp["norm1_g"][0].reshape(8, 128).T).astype(f)
    com["wout"] = kmaj(inp["w_out"][0])
    com["wglu"] = kmaj(inp["w_glu"][0])
    com["wq"] = kmaj(inp["w_query"][0])
    com["g2"] = np.ascontiguousarray(inp["norm2_g"][0].reshape(8, 128).T).astype(f)
    com["gfin"] = np.ascontiguousarray(np.broadcast_to(inp["final_g"][None, :], (128, D))).astype(f)
    com["wf"] = np.ascontiguousarray(inp["w_fourier"][0].transpose(1, 0, 2)).astype(f)
    c = np.arange(128)
    ang = 2 * np.pi * np.outer(c, c) / 128.0
    sc = 1.0 / math.sqrt(S * 128)
    com["ccsc"] = np.stack([np.cos(ang) * sc, -np.sin(ang) * sc], axis=1).astype(f)
    com["ident"] = np.eye(128, dtype=f)
    s_idx = np.arange(S)
    ks = (np.outer(s_idx, s_idx) % S).astype(np.float64) * (2 * np.pi / S)
    ct = np.cos(ks).astype(f).astype(ml_dtypes.bfloat16)
    stt = np.sin(ks).astype(f).astype(ml_dtypes.bfloat16)
    t = np.stack([ct, stt], axis=0)
    t = t.reshape(2, 32, 128, 16, 256).transpose(3, 2, 0, 1, 4)
    com["tab"] = np.ascontiguousarray(t)
    com["iota128"] = np.ascontiguousarray(np.broadcast_to(np.arange(128, dtype=f)[None, :], (128, 128)))
    com["iota16"] = np.ascontiguousarray(np.broadcast_to(np.arange(16, dtype=f)[None, :], (128, 16)))

    def lamA(arr):
        return np.ascontiguousarray(arr.reshape(2, 4, 4, 2, 64).transpose(3, 4, 1, 2, 0).reshape(128, 32)).astype(f)
    com["lam_are"] = lamA(inp["ssm_a_re"][0])
    com["lam_aim"] = lamA(inp["ssm_a_im"][0])
    com["lam_lst"] = lamA(np.broadcast_to(inp["ssm_log_step"][0][:, :, None], (2, 32, 64)))
    bA = np.zeros((2, 4, 128, 8, 128), f)
    cA = np.zeros((2, 4, 128, 8, 128), f)
    for ri, (bsrc, csrc) in enumerate(((inp["ssm_b_re"][0], inp["ssm_c_re"][0]), (inp["ssm_b_im"][0], inp["ssm_c_im"][0]))):
        for T in range(4):
            for q in range(4):
                for gp in range(2):
                    g = 8 * T + 2 * q + gp
                    for dr in range(2):
                        col = (2 * q + gp) * 16
                        bA[ri, T, gp * 64:(gp + 1) * 64, q * 2 + dr, col:col + 16] = bsrc[dr, g]
                        cA[ri, T, gp * 64:(gp + 1) * 64, q * 2 + dr, col:col + 16] = csrc[dr, g].T
    com["bA"] = bA
    com["cA"] = cA
    com["dD"] = np.ascontiguousarray(inp["ssm_d"][0].reshape(4, 128).T).astype(f)
    eu = inp["expert_u"][0]
    com["uT_in"] = np.ascontiguousarray(eu.reshape(16384, 8, 128).transpose(2, 1, 0)).astype(f)
    com["v_in"] = np.ascontiguousarray(inp["expert_v"][0].reshape(128, 128, 1024)).astype(f)
    sk = inp["sub_keys"][0]
    com["keysT_in"] = np.ascontiguousarray(sk.reshape(16, 128, 128).transpose(2, 0, 1)).astype(f)
    return com


_CACHE = {}


def kernel(**inp):
    if "nc" not in _CACHE:
        _CACHE["nc"] = build()[0]
    nc = _CACHE["nc"]
    com = host_inputs(inp)
    xs = np.ascontiguousarray(inp["x"]).astype(np.float32)
    in_maps = []
    for c in range(8):
        m = dict(com)
        m["x"] = xs[2 * c:2 * c + 2]
        in_maps.append(m)
    res = run_bass_kernel_spmd(nc, in_maps, core_ids=list(range(8)))
    return np.concatenate([np.asarray(r["y"]) for r in res.results], axis=0).astype(np.float32)
```

```python
import math
from contextlib import ExitStack
import numpy as np
import ml_dtypes
import concourse.bass as bass
import concourse.mybir as mybir
from concourse.bass_utils import run_bass_kernel_spmd

F32 = mybir.dt.float32
BF16 = mybir.dt.bfloat16
U32 = mybir.dt.uint32
ALU = mybir.AluOpType
AF = mybir.ActivationFunctionType
AX = mybir.AxisListType

NB = 2
S = 4096
D = 1024
L = 16
NCH = S // L
PADW = 2 * L - 1
EPS = 1e-6
ENG = ['pe', 'act', 'dve', 'pool', 'sp']


class Prog:
    def __init__(s, nc):
        s.nc = nc
        s.e = dict(pe=nc.tensor, act=nc.scalar, dve=nc.vector, pool=nc.gpsimd, sp=nc.sync)
        s.nsem = 0
        s.new_sems()
        s.dsem = {}
        s.lastw = {}
        s.readers = {}
        s.items = {k: [] for k in ENG}

    def new_sems(s):
        if s.nsem > 0:
            return
        s.sem = {}
        for k in ENG:
            s.sem[k] = s.nc.alloc_semaphore("es%d_%s" % (s.nsem, k))
        s.nsem += 1
        s.cnt = {k: 0 for k in ENG}
        s.waited = {}

    def _wait(s, eng, tok):
        kind, name, val = tok
        h = s.sem[name] if kind == 'e' else s.dsem[name][0]
        key = (eng, h.num)
        if s.waited.get(key, 0) >= val:
            return
        s.waited[key] = val
        s.items[eng].append(('w', h, val))

    def _deps(s, eng, r, w):
        deps = []
        for k in r:
            if k in s.lastw:
                deps.append((s.lastw[k], True))
        for k in w:
            if k in s.lastw:
                deps.append((s.lastw[k], False))
            for t in s.readers.get(k, ()):
                deps.append((t, False))
        for tok, raw in deps:
            if tok[0] == 'e' and tok[1] == eng:
                if raw and s.cnt[eng] - tok[2] < 2:
                    s._wait(eng, tok)
                continue
            s._wait(eng, tok)

    def _upd(s, tok, r, w):
        for k in r:
            s.readers.setdefault(k, []).append(tok)
        for k in w:
            s.lastw[k] = tok
            s.readers[k] = []

    def op(s, eng, fn, r=(), w=()):
        s._deps(eng, r, w)
        s.cnt[eng] += 1
        s.items[eng].append(('o', fn, s.sem[eng]))
        s._upd(('e', eng, s.cnt[eng]), r, w)

    def dma(s, eng, out, in_, sem, r=(), w=()):
        s._deps(eng, r, w)
        if sem not in s.dsem:
            if getattr(s, 'free_d', None):
                s.free_d.sort(key=lambda hc: hc[1])
                s.dsem[sem] = s.free_d.pop(0)
            else:
                s.ndsem = getattr(s, 'ndsem', 0) + 1
                s.dsem[sem] = [s.nc.alloc_semaphore("ds%d" % s.ndsem), 0]
        d = s.dsem[sem]
        d[1] += 16
        s.items[eng].append(('d', out, in_, d[0]))
        s._upd(('d', sem, d[1]), r, w)

    def barrier(s, final=False):
        toks = [('e', k, s.cnt[k]) for k in ENG if s.cnt[k] > 0]
        toks += [('d', n, d[1]) for n, d in s.dsem.items() if d[1] > 0]
        for eng in (['sp'] if final else ENG):
            for tok in toks:
                if tok[0] == 'e' and tok[1] == eng:
                    continue
                s._wait(eng, tok)
        s.lastw.clear()
        s.readers.clear()
        s.flush()
        if not hasattr(s, 'free_d'):
            s.free_d = []
        s.free_d.extend(s.dsem.values())
        s.dsem = {}

    def flush(s):
        def replay(items, embed=False):
            def f(e):
                pend = []
                for it in items:
                    if it[0] == 'w':
                        if embed:
                            pend.append(it)
                        else:
                            e.wait_ge(it[1], it[2])
                    elif it[0] == 'o':
                        for p in pend[:-1]:
                            e.wait_ge(p[1], p[2])
                        ins = it[1](e)
                        if pend:
                            ins._wait_ge(pend[-1][1], pend[-1][2])
                        pend = []
                        ins.then_inc(it[2], 1)
                    else:
                        e.dma_start(out=it[1], in_=it[2]).then_inc(it[3], 16)
            return f
        with s.nc.Block() as block:
            for k, dec in (('pe', block.tensor), ('act', block.scalar), ('dve', block.vector), ('pool', block.gpsimd), ('sp', block.sync)):
                if s.items[k]:
                    dec(replay(s.items[k], embed=False))
        s.items = {k: [] for k in ENG}


def AP(t, off, dims):
    return bass.AP(t, off, [list(d) for d in dims])


def build(debug=(), nblk_lim=None, stage=99, upto=99, quick=False):
    nc = bass.Bass("TRN2", target_bir_lowering=False)
    P = Prog(nc)

    def din(name, shape, dt=F32):
        return nc.dram_tensor(name, list(shape), dt, kind="ExternalInput").ap()

    dbg = {}

    def dscr(name, shape, dt=BF16):
        kind = "ExternalOutput" if name in debug else "Internal"
        a = nc.dram_tensor(name, list(shape), dt, kind=kind).ap()
        return a

    x = din("x", [NB, S, D])
    w1 = din("w1", [128, 8, D])
    g1 = din("g1", [128, 8])
    woutw = din("wout", [128, 8, D])
    wglu = din("wglu", [128, 4, 512])
    wq = din("wq", [128, 8, 2048])
    g2 = din("g2", [128, 8])
    gfin = din("gfin", [128, D])
    wf = din("wf", [128, 4, 128])
    ccsc = din("ccsc", [128, 2, 128])
    ident_in = din("ident", [128, 128])
    tab = din("tab", [16, 128, 2, 32, 256], BF16)
    y = nc.dram_tensor("y", [NB, S, D], F32, kind="ExternalOutput").ap()

    zS_d = dscr("zS_d", [NB, 4, 128, S])
    A_d = dscr("A_d", [NB, 32, 128, 1024])
    yF_d = dscr("yF_d", [NB, 4, 128, S])
    yG_d = dscr("yG_d", [NB, 4, 128, S])
    x1_d = dscr("x1_d", [NB, S, D], F32)
    h2T_d = dscr("h2T_d", [NB, 128, 8, S])

    def dump(name, tile, shape, dt, key):
        if name in debug:
            d = nc.dram_tensor(name, list(shape), dt, kind="ExternalOutput").ap()
            P.dma('sp', d, tile, 'dbg_' + name, r=[key], w=['dbg_' + name])

    pst = ExitStack()

    def SBp(name, shape, dt):
        return pst.enter_context(nc.sbuf_tensor(name, list(shape), dt))

    identb = SBp("identb", [128, 128], BF16)
    identf = SBp("identf", [128, 128], F32)
    gfin_s = SBp("gfin_s", [128, D], F32)
    epsc = SBp("epsc", [128, 1], F32)

    P.dma('sp', identf[:], ident_in[:, :], 'c_identf', w=['identf'])
    P.dma('sp', gfin_s[:], gfin[:, :], 'c_gfin', w=['gfin'])
    P.op('dve', lambda e: e.tensor_copy(out=identb[:], in_=identf[:]), r=['identf'], w=['identb'])
    P.op('dve', lambda e: e.memset(epsc[:], EPS), w=['epsc'])

    uT_in = din("uT_in", [128, 8, 16384])
    v_in = din("v_in", [128, 128, 1024])
    UTb_d = dscr("UTb_d", [128, 128, 8, 128])
    Vb_d = dscr("Vb_d", [128, 128, 1024])
    g2s = SBp("g2s", [128, 8], F32)
    P.dma('sp', g2s[:], g2[:, :], 'c_g2s', w=['g2s'])
    e0st = ExitStack()
    uf0 = e0st.enter_context(nc.sbuf_tensor("uf0", [128, 8, 512], F32))
    ub0 = e0st.enter_context(nc.sbuf_tensor("ub0", [128, 8, 512], BF16))
    vf0 = e0st.enter_context(nc.sbuf_tensor("vf0", [128, 4, 1024], F32))
    vb0 = e0st.enter_context(nc.sbuf_tensor("vb0", [128, 4, 1024], BF16))

    def e0_iter(it):
        P.dma('pool', uf0[:], uT_in[:, :, it * 512:(it + 1) * 512], 'uf0', w=['uf0'])
        for kc in range(8):
            P.op('dve', lambda e, kc=kc: e.tensor_scalar(out=ub0[:, kc, :], in0=uf0[:, kc, :], scalar1=g2s[:, kc:kc + 1], scalar2=None, op0=ALU.mult),
                 r=['uf0', 'g2s'], w=['ub0_%d' % kc] + ['ubo0_%d' % i4 for i4 in range(4)])
        for i4 in range(4):
            P.dma('pool', UTb_d[it * 4 + i4], AP(ub0, i4 * 128, [[4096, 128], [512, 8], [1, 128]]),
                  'ubo0', r=['ub0_%d' % kc for kc in range(8)], w=['ubo0_%d' % i4])
        P.dma('pool', vf0[:], v_in[it * 4:(it + 1) * 4].rearrange("i p d -> p i d"), 'vf0', w=['vf0'])
        P.op('act', lambda e: e.copy(out=vb0[:], in_=vf0[:]), r=['vf0'], w=['vb0'])
        P.dma('pool', Vb_d[it * 4:(it + 1) * 4].rearrange("i p d -> p i d"), vb0[:], 'vbo0', r=['vb0'], w=['vb0'])

    with ExitStack() as st:
        def SB(name, shape, dt):
            return st.enter_context(nc.sbuf_tensor(name, list(shape), dt))

        def PS(name, shape, dt=F32):
            return st.enter_context(nc.psum_tensor(name, list(shape), dt))

        w1f = SB("w1f", [128, 8, D], F32)
        w1b = SB("w1b", [128, 8, D], BF16)
        g1s = SB("g1s", [128, 8], F32)
        wfs = SB("wfs", [128, 4, 128], F32)
        ccs = SB("ccs", [128, 2, 128], F32)
        csw = SB("csw", [128, 4, 256], BF16)
        wfb = SB("wfb", [128, 4, 128], BF16)
        ccb = SB("ccb", [128, 2, 128], BF16)
        xs = [SB("xs%d" % i, [128, D], F32) for i in range(2)]
        sq = SB("sq", [128, D], F32)
        ss = [SB("ss%d" % i, [128, 4], F32) for i in range(2)]
        xn = [SB("xn%d" % i, [128, D], BF16) for i in range(2)]
        hT = [SB("hT%d" % i, [128, 8, 128], BF16) for i in range(2)]
        zT = [SB("zT%d" % i, [128, 8, 128], BF16) for i in range(2)]
        Ab = [SB("Ab%d" % i, [128, 1024], BF16) for i in range(2)]
        pT = [PS("pT%d" % i, [128, 8, 128], BF16) for i in range(2)]
        pz = [PS("pz%d" % i, [128, 8, 128], F32) for i in range(1)]
        pA = [PS("pA%d" % i, [128, 1024], F32) for i in range(1)]
        pw = PS("pw", [128, 512], F32)

        P.dma('sp', w1f[:], w1[:, :, :], 'c_w1f', w=['w1f'])
        P.dma('sp', g1s[:], g1[:, :], 'c_g1s', w=['g1s'])
        P.dma('sp', wfs[:], wf[:, :, :], 'c_wfs', w=['wfs'])
        P.dma('sp', ccs[:], ccsc[:, :, :], 'c_ccs', w=['ccs'])
        for kc in range(8):
            P.op('dve', lambda e, kc=kc: e.tensor_scalar(out=w1b[:, kc, :], in0=w1f[:, kc, :], scalar1=g1s[:, kc:kc + 1],
                                                        scalar2=None, op0=ALU.mult), r=['w1f', 'g1s'], w=['w1b'])
        P.op('dve', lambda e: e.tensor_copy(out=wfb[:], in_=wfs[:]), r=['wfs'], w=['wfb'])
        P.op('dve', lambda e: e.tensor_copy(out=ccb[:], in_=ccs[:]), r=['ccs'], w=['ccb'])
        for g in range(4 if stage >= 0 else 0):
            for t in range(2):
                P.op('pe', lambda e, g=g, t=t: e.matmul(pw[:, (g % 2) * 256 + t * 128:(g % 2) * 256 + t * 128 + 128],
                                                        lhsT=ccb[:, t, :], rhs=wfb[:, g, :], start=True, stop=True),
                     r=['ccb', 'wfb'], w=['pw'])
            P.op('dve', lambda e, g=g: e.tensor_copy(out=csw[:, g, :], in_=pw[:, (g % 2) * 256:(g % 2) * 256 + 256]),
                 r=['pw'], w=['csw'])

        nblk = NB * (S // 128) if nblk_lim is None else nblk_lim
        if stage == -2:
            nblk = 0
        for blk in range(nblk):
            b, tb = divmod(blk, S // 128)
            i = blk % 2
            t0 = tb * 128
            if blk % 2 == 0:
                e0_iter(blk // 2)
            P.dma('sp', xs[i][:], x[b, t0:t0 + 128, :], 'xs%d' % i, w=['xs%d' % i])
            P.op('act', lambda e, i=i: e.activation(out=sq[:], in_=xs[i][:], func=AF.Square, accum_out=ss[i][:, 0:1]),
                 r=['xs%d' % i], w=['sq', 'ssa%d' % i])
            P.op('act', lambda e, i=i: e.activation(out=ss[i][:, 1:2], in_=ss[i][:, 0:1], func=AF.Sqrt, scale=1.0 / D, bias=epsc[:, 0:1]),
                 r=['ssa%d' % i, 'epsc'], w=['ssb%d' % i])
            P.op('dve', lambda e, i=i: e.reciprocal(out=ss[i][:, 2:3], in_=ss[i][:, 1:2]), r=['ssb%d' % i], w=['ssc%d' % i])
            P.op('dve', lambda e, i=i: e.tensor_scalar(out=xn[i][:], in0=xs[i][:], scalar1=ss[i][:, 2:3], scalar2=None, op0=ALU.mult),
                 r=['xs%d' % i, 'ssc%d' % i], w=['xn%d' % i])

            if stage < 1:
                continue
            def tr(e, i=i):
                for kc in range(8):
                    ins = e.transpose(out=pT[i][:, kc, :], in_=xn[i][:, kc * 128:(kc + 1) * 128], identity=identb[:])
                return ins
            P.op('pe', tr, r=['xn%d' % i, 'identb'], w=['pT%d' % i])
            P.op('act', lambda e, i=i: e.copy(out=hT[i][:], in_=pT[i][:]), r=['pT%d' % i], w=['hT%d' % i])

            if stage < 2:
                continue
            def zmm(e, i=i):
                for n in range(8):
                    for kc in range(8):
                        ins = e.matmul(pz[0][:, n, :], lhsT=w1b[:, kc, n * 128:(n + 1) * 128], rhs=hT[i][:, kc, :],
                                       start=(kc == 0), stop=(kc == 7))
                return ins
            P.op('pe', zmm, r=['hT%d' % i, 'w1b'], w=['pz'])
            P.op('dve', lambda e, i=i: e.tensor_copy(out=zT[i][:], in_=pz[0][:]), r=['pz'], w=['zT%d' % i])
            if blk == 0:
                dump('d_xn', xn[i][:], [128, D], BF16, 'xn%d' % i)
                dump('d_hT', hT[i][:], [128, 8, 128], BF16, 'hT%d' % i)
                dump('d_zT', zT[i][:], [128, 8, 128], BF16, 'zT%d' % i)
                dump('d_w1b', w1b[:], [128, 8, D], BF16, 'w1b')
            P.dma('sp', zS_d[b, :, :, t0:t0 + 128].rearrange("t p s -> p t s"), zT[i][:, 0:4, :], 'zso%d' % i, r=['zT%d' % i], w=['zso%d' % i])

            if stage < 3:
                continue
            def amm(e, i=i):
                for g in range(4):
                    ins = e.matmul(pA[0][:, g * 256:(g + 1) * 256], lhsT=zT[i][:, 4 + g, :], rhs=csw[:, g, :], start=True, stop=True)
                return ins
            P.op('pe', amm, r=['zT%d' % i, 'csw'], w=['pA'])
            P.op('act', lambda e, i=i: e.copy(out=Ab[i][:], in_=pA[0][:]), r=['pA'], w=['Ab%d' % i])
            P.dma('sp', A_d[b, tb, :, :], Ab[i][:], 'ao%d' % i, r=['Ab%d' % i], w=['ao%d' % i])
        P.barrier()
    e0st.close()
    P.new_sems()
    if upto < 2:
        return nc, P, {}

    with ExitStack() as st:
        def SB(name, shape, dt):
            return st.enter_context(nc.sbuf_tensor(name, list(shape), dt))

        def PS(name, shape, dt=F32):
            return st.enter_context(nc.psum_tensor(name, list(shape), dt))
        Asb = SB("Asb", [128, 32, 1024], BF16)
        tbs = [SB("tb%d" % i, [128, 2, 32, 256], BF16) for i in range(2)]
        yst = [SB("yst%d" % i, [128, 4, 256], BF16) for i in range(2)]
        pf = [PS("pf%d" % i, [128, 512], F32) for i in range(4)]
        for b in range(NB):
            P.dma('sp', Asb[:], A_d[b].rearrange("s p n -> p s n"), 'Asb', w=['Asb'])
            for kt in range(16 if not quick else 1):
                it = (b * 16 + kt) % 2
                P.dma('sp', tbs[it][:], tab[kt], 'tb%d' % it, w=['tb%d' % it])
                for g in range(4):
                    def fmm(e, g=g, it=it):
                        n = 0
                        for cs in range(2):
                            for sc in range(32):
                                ins = e.matmul(pf[g][:, 0:256], lhsT=Asb[:, sc, g * 256 + cs * 128:g * 256 + cs * 128 + 128],
                                               rhs=tbs[it][:, cs, sc, :], start=(n == 0), stop=(n == 63))
                                n += 1
                        return ins
                    P.op('pe', fmm, r=['Asb', 'tb%d' % it], w=['pf%d' % g])
                    if g % 2:
                        P.op('act', lambda e, g=g, it=it: e.copy(out=yst[it][:, g, :], in_=pf[g][:, 0:256]), r=['pf%d' % g], w=['yst%d_%d' % (it, g)])
                    else:
                        P.op('dve', lambda e, g=g, it=it: e.tensor_copy(out=yst[it][:, g, :], in_=pf[g][:, 0:256]), r=['pf%d' % g], w=['yst%d_%d' % (it, g)])
                P.dma('sp', yF_d[b, :, :, kt * 256:(kt + 1) * 256].rearrange("g p s -> p g s"), yst[it][:], 'yfo%d' % it,
                      r=['yst%d_%d' % (it, g) for g in range(4)], w=['yst%d_%d' % (it, g) for g in range(4)])
        P.barrier()
    P.new_sems()
    if upto < 3:
        return nc, P, {}

    lam_are = din("lam_are", [128, 32]); lam_aim = din("lam_aim", [128, 32]); lam_lst = din("lam_lst", [128, 32])
    bA = din("bA", [2, 4, 128, 8, 128]); cA = din("cA", [2, 4, 128, 8, 128]); dD = din("dD", [128, 4])
    WLAG_d = dscr("WLAG_d", [4, 128, 31, 128])
    WIN_d = dscr("WIN_d", [4, 16, 128, 16, 128])
    WOUT_d = dscr("WOUT_d", [4, 16, 128, 16, 128])
    prt = SBp("prt", [128, 17, 32], F32)
    pit = SBp("pit", [128, 17, 32], F32)
    with ExitStack() as st:
        def SB(name, shape, dt):
            return st.enter_context(nc.sbuf_tensor(name, list(shape), dt))

        def PS(name, shape, dt=F32):
            return st.enter_context(nc.psum_tensor(name, list(shape), dt))
        names = ['are', 'aim', 'lst', 'stp', 'ar', 'ai', 'mag', 's16', 'cc', 'sn', 't1', 't2', 't3', 'lr', 'li', 'den', 'nr', 'cr', 'ci']
        T_ = {n: SB("l_" + n, [128, 32], F32) for n in names}
        P.dma('sp', T_['are'][:], lam_are[:, :], 'c_are', w=['are'])
        P.dma('sp', T_['aim'][:], lam_aim[:, :], 'c_aim', w=['aim'])
        P.dma('sp', T_['lst'][:], lam_lst[:, :], 'c_lst', w=['lst'])
        dDs = SB("dDs", [128, 4], F32)
        P.dma('sp', dDs[:], dD[:, :], 'c_dD', w=['dD'])

        def tt(o, a, b_, op, eng='dve'):
            P.op(eng, lambda e: e.tensor_tensor(out=T_[o][:], in0=T_[a][:], in1=T_[b_][:], op=op), r=[a, b_], w=[o])

        def ts(o, a, s1, s2, op0, op1=None):
            if op1 is None:
                P.op('dve', lambda e: e.tensor_scalar(out=T_[o][:], in0=T_[a][:], scalar1=s1, scalar2=None, op0=op0), r=[a], w=[o])
            else:
                P.op('dve', lambda e: e.tensor_scalar(out=T_[o][:], in0=T_[a][:], scalar1=s1, scalar2=s2, op0=op0, op1=op1), r=[a], w=[o])

        def act(o, a, func, scale=1.0):
            P.op('act', lambda e: e.activation(out=T_[o][:], in_=T_[a][:], func=func, scale=scale), r=[a], w=[o])
        act('stp', 'lst', AF.Exp)
        tt('ar', 'are', 'stp', ALU.mult)
        tt('ai', 'aim', 'stp', ALU.mult)
        act('mag', 'ar', AF.Exp)
        act('s16', 'ai', AF.Sin, 1.0 / 16)
        act('sn', 'ai', AF.Sin, 1.0 / 8)
        tt('t1', 's16', 's16', ALU.mult)
        ts('cc', 't1', -2.0, 1.0, ALU.mult, ALU.add)
        for _ in range(3):
            tt('t1', 'cc', 'cc', ALU.mult)
            tt('t2', 'sn', 'sn', ALU.mult)
            tt('t3', 'cc', 'sn', ALU.mult)
            tt('cc', 't1', 't2', ALU.subtract)
            ts('sn', 't3', 2.0, None, ALU.mult)
        tt('lr', 'mag', 'cc', ALU.mult)
        tt('li', 'mag', 'sn', ALU.mult)
        tt('t1', 'are', 'are', ALU.mult)
        tt('t2', 'aim', 'aim', ALU.mult)
        tt('den', 't1', 't2', ALU.add)
        P.op('dve', lambda e: e.reciprocal(out=T_['den'][:], in_=T_['den'][:]), r=['den'], w=['den'])
        ts('nr', 'lr', -1.0, None, ALU.add)
        tt('t1', 'nr', 'are', ALU.mult)
        tt('t2', 'li', 'aim', ALU.mult)
        tt('t3', 't1', 't2', ALU.add)
        tt('cr', 't3', 'den', ALU.mult)
        tt('t1', 'li', 'are', ALU.mult)
        tt('t2', 'nr', 'aim', ALU.mult)
        tt('t3', 't1', 't2', ALU.subtract)
        tt('ci', 't3', 'den', ALU.mult)
        P.op('dve', lambda e: e.memset(prt[:, 0, :], 1.0), w=['prt'])
        P.op('dve', lambda e: e.memset(pit[:, 0, :], 0.0), w=['pit'])
        for k in range(16):
            P.op('dve', lambda e, k=k: e.tensor_tensor(out=T_['t1'][:], in0=prt[:, k, :], in1=T_['lr'][:], op=ALU.mult), r=['prt', 'lr'], w=['t1'])
            P.op('dve', lambda e, k=k: e.tensor_tensor(out=T_['t2'][:], in0=pit[:, k, :], in1=T_['li'][:], op=ALU.mult), r=['pit', 'li'], w=['t2'])
            P.op('dve', lambda e, k=k: e.tensor_tensor(out=T_['t3'][:], in0=prt[:, k, :], in1=T_['li'][:], op=ALU.mult), r=['prt', 'li'], w=['t3'])
            P.op('dve', lambda e, k=k: e.tensor_tensor(out=T_['nr'][:], in0=pit[:, k, :], in1=T_['lr'][:], op=ALU.mult), r=['pit', 'lr'], w=['nr'])
            P.op('dve', lambda e, k=k: e.tensor_tensor(out=prt[:, k + 1, :], in0=T_['t1'][:], in1=T_['t2'][:], op=ALU.subtract), r=['t1', 't2'], w=['prt'])
            P.op('dve', lambda e, k=k: e.tensor_tensor(out=pit[:, k + 1, :], in0=T_['t3'][:], in1=T_['nr'][:], op=ALU.add), r=['t3', 'nr'], w=['pit'])

        bre = SB("bre", [128, 8, 128], F32); bim = SB("bim", [128, 8, 128], F32)
        cre = SB("cre", [128, 8, 128], F32); cim = SB("cim", [128, 8, 128], F32)
        Br = SB("Br", [128, 8, 128], F32); Bi = SB("Bi", [128, 8, 128], F32)
        u1 = SB("u1", [128, 8, 128], F32); u2 = SB("u2", [128, 8, 128], F32)
        v1 = SB("v1", [128, 8, 128], F32); v2 = SB("v2", [128, 8, 128], F32)
        Cxr = SB("Cxr", [128, 8, 128], BF16); Cxi = SB("Cxi", [128, 8, 128], BF16)
        Pkr = [SB("Pkr%d" % i, [128, 8, 128], BF16) for i in range(2)]
        Pki = [SB("Pki%d" % i, [128, 8, 128], BF16) for i in range(2)]
        CLr = [SB("CLr%d" % i, [128, 8, 128], BF16) for i in range(2)]
        CLi = [SB("CLi%d" % i, [128, 8, 128], BF16) for i in range(2)]
        winst = [SB("winst%d" % i, [128, 16, 128], BF16) for i in range(2)]
        wlags = SB("wlags", [128, 31, 128], BF16)
        ddiag = SB("ddiag", [128, 128], F32)
        ptk = [PS("ptk%d" % i, [128, 512], F32) for i in range(2)]
        ptr = [PS("ptr%d" % i, [128, 8, 128], BF16) for i in range(2)]

        def bc(tile, off, pstep):
            return AP(tile, off, [[pstep, 128], [1, 8], [0, 128]])
        for T in range(4):
            P.dma('sp', bre[:], bA[0, T], 'c_bre', w=['bre']); P.dma('sp', bim[:], bA[1, T], 'c_bim', w=['bim'])
            P.dma('sp', cre[:], cA[0, T], 'c_cre', w=['cre']); P.dma('sp', cim[:], cA[1, T], 'c_cim', w=['cim'])
            crb = bc(T_['cr'], T * 8, 32); cib = bc(T_['ci'], T * 8, 32)
            P.op('dve', lambda e, crb=crb: e.tensor_tensor(out=u1[:], in0=bre[:], in1=crb, op=ALU.mult), r=['bre', 'cr'], w=['u1'])
            P.op('dve', lambda e, cib=cib: e.tensor_tensor(out=u2[:], in0=bim[:], in1=cib, op=ALU.mult), r=['bim', 'ci'], w=['u2'])
            P.op('dve', lambda e: e.tensor_tensor(out=Br[:], in0=u1[:], in1=u2[:], op=ALU.subtract), r=['u1', 'u2'], w=['Br'])
            P.op('dve', lambda e, crb=crb: e.tensor_tensor(out=u1[:], in0=bim[:], in1=crb, op=ALU.mult), r=['bim', 'cr'], w=['u1'])
            P.op('dve', lambda e, cib=cib: e.tensor_tensor(out=u2[:], in0=bre[:], in1=cib, op=ALU.mult), r=['bre', 'ci'], w=['u2'])
            P.op('dve', lambda e: e.tensor_tensor(out=Bi[:], in0=u1[:], in1=u2[:], op=ALU.add), r=['u1', 'u2'], w=['Bi'])
            P.op('pool', lambda e: e.tensor_copy(out=Cxr[:], in_=cre[:]), r=['cre'], w=['Cxr'])
            P.op('pool', lambda e: e.tensor_scalar(out=Cxi[:], in0=cim[:], scalar1=-1.0, scalar2=None, op0=ALU.mult), r=['cim'], w=['Cxi'])
            P.op('dve', lambda e, T=T: e.tensor_scalar(out=ddiag[:], in0=identf[:], scalar1=dDs[:, T:T + 1], scalar2=None, op0=ALU.mult),
                 r=['identf', 'dD'], w=['ddiag'])
            for k in range(17):
                i2 = k % 2
                pkr = bc(prt, k * 32 + T * 8, 17 * 32); pki = bc(pit, k * 32 + T * 8, 17 * 32)
                if k <= 15:
                    P.op('dve', lambda e, pkr=pkr: e.tensor_tensor(out=u1[:], in0=Br[:], in1=pkr, op=ALU.mult), r=['Br', 'prt'], w=['u1'])
                    P.op('dve', lambda e, pki=pki: e.tensor_tensor(out=u2[:], in0=Bi[:], in1=pki, op=ALU.mult), r=['Bi', 'pit'], w=['u2'])
                    P.op('dve', lambda e, i2=i2: e.tensor_tensor(out=Pkr[i2][:], in0=u1[:], in1=u2[:], op=ALU.subtract), r=['u1', 'u2'], w=['Pkr%d' % i2])
                    P.op('dve', lambda e, pki=pki: e.tensor_tensor(out=u1[:], in0=Br[:], in1=pki, op=ALU.mult), r=['Br', 'pit'], w=['u1'])
                    P.op('dve', lambda e, pkr=pkr: e.tensor_tensor(out=u2[:], in0=Bi[:], in1=pkr, op=ALU.mult), r=['Bi', 'prt'], w=['u2'])
                    P.op('dve', lambda e, i2=i2: e.tensor_tensor(out=Pki[i2][:], in0=u1[:], in1=u2[:], op=ALU.add), r=['u1', 'u2'], w=['Pki%d' % i2])
                    if k == 0:
                        def tk0(e, i2=i2):
                            n = 0
                            for dr in range(2):
                                for q in range(4):
                                    for (Pt, Ct) in ((Pkr[i2], Cxr), (Pki[i2], Cxi)):
                                        ins = e.matmul(ptk[0][:, 0:128], lhsT=Pt[:, q * 2 + dr, :], rhs=Ct[:, q * 2 + dr, :], start=(n == 0), stop=(n == 15))
                                        n += 1
                            return ins
                        P.op('pe', tk0, r=['Pkr%d' % i2, 'Pki%d' % i2, 'Cxr', 'Cxi'], w=['ptk0'])
                        P.op('dve', lambda e: e.tensor_tensor(out=wlags[:, 15, :], in0=ptk[0][:, 0:128], in1=ddiag[:], op=ALU.add),
                             r=['ptk0', 'ddiag'], w=['wlags'])
                    else:
                        for dr in range(2):
                            def tk(e, i2=i2, dr=dr):
                                n = 0
                                for q in range(4):
                                    for (Pt, Ct) in ((Pkr[i2], Cxr), (Pki[i2], Cxi)):
                                        ins = e.matmul(ptk[dr][:, 0:128], lhsT=Pt[:, q * 2 + dr, :], rhs=Ct[:, q * 2 + dr, :], start=(n == 0), stop=(n == 7))
                                        n += 1
                                return ins
                            P.op('pe', tk, r=['Pkr%d' % i2, 'Pki%d' % i2, 'Cxr', 'Cxi'], w=['ptk%d' % dr])
                            li_ = 15 + k if dr == 0 else 15 - k
                            P.op('act', lambda e, dr=dr, li_=li_: e.copy(out=wlags[:, li_, :], in_=ptk[dr][:, 0:128]), r=['ptk%d' % dr], w=['wlags'])
                    for h in range(2):
                        def trw(e, i2=i2, h=h):
                            for m in range(8):
                                idx = h * 8 + m
                                cb, ri = idx // 2, idx % 2
                                ins = e.transpose(out=ptr[h][:, m, :], in_=(Pkr[i2] if ri == 0 else Pki[i2])[:, cb, :], identity=identb[:])
                            return ins
                        P.op('pe', trw, r=['Pkr%d' % i2, 'Pki%d' % i2, 'identb'], w=['ptr%d' % h])
                        P.op('act', lambda e, i2=i2, h=h: e.copy(out=winst[i2][:, h * 8:(h + 1) * 8, :], in_=ptr[h][:]), r=['ptr%d' % h], w=['winst%d' % i2])
                    P.dma('sp', WIN_d[T, k], winst[i2][:], 'wino%d' % i2, r=['winst%d' % i2], w=['winst%d' % i2])
                if k >= 1:
                    P.op('dve', lambda e, pkr=pkr: e.tensor_tensor(out=u1[:], in0=cre[:], in1=pkr, op=ALU.mult), r=['cre', 'prt'], w=['u1'])
                    P.op('dve', lambda e, pki=pki: e.tensor_tensor(out=u2[:], in0=cim[:], in1=pki, op=ALU.mult), r=['cim', 'pit'], w=['u2'])
                    P.op('dve', lambda e, i2=i2: e.tensor_tensor(out=CLr[i2][:], in0=u1[:], in1=u2[:], op=ALU.subtract), r=['u1', 'u2'], w=['CLr%d' % i2])
                    P.op('pool', lambda e, pki=pki: e.tensor_tensor(out=v1[:], in0=cre[:], in1=pki, op=ALU.mult), r=['cre', 'pit'], w=['v1'])
                    P.op('pool', lambda e, pkr=pkr: e.tensor_tensor(out=v2[:], in0=cim[:], in1=pkr, op=ALU.mult), r=['cim', 'prt'], w=['v2'])
                    P.op('pool', lambda e: e.tensor_tensor(out=v1[:], in0=v1[:], in1=v2[:], op=ALU.add), r=['v1', 'v2'], w=['v1'])
                    P.op('pool', lambda e, i2=i2: e.tensor_scalar(out=CLi[i2][:], in0=v1[:], scalar1=-1.0, scalar2=None, op0=ALU.mult), r=['v1'], w=['CLi%d' % i2])
                    wd = WOUT_d[T, k - 1].rearrange("p (c r) m -> p c r m", r=2)
                    P.dma('sp', wd[:, :, 0, :], CLr[i2][:], 'wouto%d' % i2, r=['CLr%d' % i2], w=['CLr%d' % i2])
                    P.dma('sp', wd[:, :, 1, :], CLi[i2][:], 'woutp%d' % i2, r=['CLi%d' % i2], w=['CLi%d' % i2])
            P.dma('sp', WLAG_d[T], wlags[:], 'wlago', r=['wlags'], w=['wlags'])
        P.barrier()
    P.new_sems()
    if upto < 4:
        return nc, P, {}
    UPW = 7968
    for b in range(NB):
        for T in range(4):
            if quick and (b > 0 or T > 0):
                continue
            with ExitStack() as st:
                def SB(name, shape, dt, b=b, T=T):
                    return st.enter_context(nc.sbuf_tensor("%s_%d_%d" % (name, b, T), list(shape), dt))

                def PS(name, shape, dt=F32, b=b, T=T):
                    return st.enter_context(nc.psum_tensor("%s_%d_%d" % (name, b, T), list(shape), dt))
                zs = SB("zs", [128, S], BF16)
                upad = SB("upad", [128, UPW], BF16)
                wlag = SB("wlag", [128, 31, 128], BF16)
                Hs = [SB("Hs%d" % i, [128, 257, 16], F32) for i in range(2)]
                Xh = SB("Xh", [128, 4, 2, 2, 256], BF16)
                Ssb = SB("Ssb", [128, 2, 256, 8], F32)
                wins = [SB("win%d" % i, [128, 16, 4, 128], BF16) for i in range(2)]
                wout = SB("wout_s", [128, 16, 16, 128], BF16)
                A12 = SB("A12", [128, 2, 16], F32)
                tP = [SB("tP%d" % i, [128, 16], F32) for i in range(2)]
                tQ = [SB("tQ%d" % i, [128, 8], F32) for i in range(2)]
                ygs = [SB("ygs%d" % i, [128, 512], BF16) for i in range(2)]
                ysum = [SB("ysum%d" % i, [128, 512], F32) for i in range(2)]
                crs = SB("crs", [128, 16, 128], F32)
                pss = [PS("pss%d" % i, [128, 512], F32) for i in range(4)]
                py = [PS("py%d" % i, [128, 512], F32) for i in range(2)]

                P.dma('sp', zs[:], zS_d[b, T], 'zs', w=['zs'])
                P.dma('sp', wlag[:], WLAG_d[T], 'wlag', w=['wlag'])
                P.dma('sp', wout[:], WOUT_d[T].rearrange("k p m c -> p k m c"), 'wout', w=['wout'])
                P.op('pool', lambda e: e.memset(upad[:], 0.0), w=['upad'])
                P.op('pool', lambda e: e.tensor_copy(out=AP(upad, 15, [[UPW, 128], [31, 256], [1, 16]]),
                                                     in_=AP(zs, 0, [[S, 128], [16, 256], [1, 16]])), r=['zs'], w=['upad'])
                for dr in range(2):
                    src_r = AP(prt, 16 * 32 + T * 8 + dr, [[17 * 32, 128], [2, 4]])
                    src_i = AP(pit, 16 * 32 + T * 8 + dr, [[17 * 32, 128], [2, 4]])
                    P.op('dve', lambda e, dr=dr, src_r=src_r: e.tensor_copy(out=A12[:, dr, 0:4], in_=src_r), r=['prt'], w=['A12'])
                    P.op('dve', lambda e, dr=dr, src_r=src_r: e.tensor_copy(out=A12[:, dr, 4:8], in_=src_r), r=['prt'], w=['A12'])
                    P.op('dve', lambda e, dr=dr, src_i=src_i: e.tensor_scalar(out=A12[:, dr, 8:12], in0=src_i, scalar1=-1.0, scalar2=None, op0=ALU.mult), r=['pit'], w=['A12'])
                    P.op('dve', lambda e, dr=dr, src_i=src_i: e.tensor_copy(out=A12[:, dr, 12:16], in_=src_i), r=['pit'], w=['A12'])
                P.op('dve', lambda e: e.memset(Hs[0][:], 0.0), w=['H0'])
                P.op('pool', lambda e: e.memset(Hs[1][:], 0.0), w=['H1'])
                for q in range(4):
                    win = wins[q % 2]
                    wkey = 'win%d' % (q % 2)
                    P.dma('sp', win[:], WIN_d[T, :, :, q * 4:(q + 1) * 4, :].rearrange("k p m c -> p k m c"), wkey, w=[wkey])
                    for dr in range(2):
                        for ri in range(2):
                            pi_ = dr * 2 + ri

                            def smm(e, dr=dr, ri=ri, pi_=pi_, win=win):
                                for jp in range(16):
                                    kk = 15 - jp if dr == 0 else jp
                                    ins = e.matmul(pss[pi_][:, 0:256], lhsT=win[:, kk, dr * 2 + ri, :],
                                                   rhs=AP(upad, 15 + jp, [[UPW, 128], [31, 256]]), start=(jp == 0), stop=(jp == 15))
                                return ins
                            P.op('pe', smm, r=[wkey, 'upad'], w=['pss%d' % pi_])
                            P.op('act', lambda e, dr=dr, ri=ri, q=q, pi_=pi_: e.copy(out=AP(Ssb, dr * 2048 + ri * 4 + q, [[4096, 128], [8, 256]]),
                                                                                     in_=pss[pi_][:, 0:256]), r=['pss%d' % pi_], w=['Ssb'])
                for step in range(256 if not quick else 256):
                    for dr, eng in ((0, 'dve'), (1, 'pool')):
                        H = Hs[dr]
                        if dr == 0:
                            src, dst, sc_ = step, step + 1, step
                        else:
                            src, dst, sc_ = 256 - step, 255 - step, 255 - step
                        hk = 'H%d' % dr
                        HW = 257 * 16
                        P.op(eng, lambda e, H=H, src=src, dr=dr, HW=HW: e.tensor_tensor(out=AP(tP[dr], 0, [[16, 128], [8, 2], [1, 8]]),
                                                                                 in0=AP(H, src * 16, [[HW, 128], [4, 2], [1, 8]]),
                                                                                 in1=AP(A12, dr * 16, [[32, 128], [8, 2], [1, 8]]), op=ALU.mult),
                             r=[hk, 'A12'], w=['tP%d' % dr])
                        P.op(eng, lambda e, dr=dr: e.tensor_tensor(out=tQ[dr][:], in0=tP[dr][:, 0:8], in1=tP[dr][:, 8:16], op=ALU.add),
                             r=['tP%d' % dr], w=['tQ%d' % dr])
                        P.op(eng, lambda e, H=H, dst=dst, dr=dr, sc_=sc_, HW=HW: e.tensor_tensor(out=AP(H, dst * 16, [[HW, 128], [8, 2], [1, 8]]),
                                                                                          in0=AP(tQ[dr], 0, [[8, 128], [0, 2], [1, 8]]),
                                                                                          in1=AP(Ssb, dr * 2048 + sc_ * 8, [[4096, 128], [0, 2], [1, 8]]), op=ALU.add),
                             r=['tQ%d' % dr, 'Ssb'], w=[hk])
                P.op('dve', lambda e: e.tensor_copy(out=AP(Xh, 0, [[4096, 128], [1024, 4], [256, 2], [1, 256]]),
                                                    in_=AP(Hs[0], 0, [[257 * 16, 128], [1, 4], [4, 2], [16, 256]])), r=['H0'], w=['Xh'])
                P.op('pool', lambda e: e.tensor_copy(out=AP(Xh, 512, [[4096, 128], [1024, 4], [256, 2], [1, 256]]),
                                                     in_=AP(Hs[1], 16, [[257 * 16, 128], [1, 4], [4, 2], [16, 256]])), r=['H1'], w=['Xh'])
                for half in range(2):
                    h0 = half * 128
                    for g4 in range(4):
                        def cmm(e, g4=g4, h0=h0):
                            for jj in range(4):
                                j = g4 * 4 + jj
                                n = 0
                                for q in range(4):
                                    for dr in range(2):
                                        kidx = j if dr == 0 else 15 - j
                                        for ri in range(2):
                                            ins = e.matmul(pss[g4][:, jj * 128:(jj + 1) * 128], lhsT=wout[:, kidx, q * 4 + dr * 2 + ri, :],
                                                           rhs=Xh[:, q, dr, ri, h0:h0 + 128], start=(n == 0), stop=(n == 15))
                                            n += 1
                            return ins
                        P.op('pe', cmm, r=['wout', 'Xh'], w=['pss%d' % g4])
                        P.op('act', lambda e, g4=g4: e.copy(out=crs[:, g4 * 4:(g4 + 1) * 4, :], in_=pss[g4][:, :]), r=['pss%d' % g4], w=['crs'])
                    for t4 in range(4):
                        tt_ = half * 4 + t4
                        c0 = tt_ * 32
                        ip = tt_ % 2

                        def ymm(e, c0=c0, ip=ip):
                            outv = AP(py[ip], 0, [[512, 128], [16, 32], [1, 16]])
                            for li_ in range(31):
                                dl = li_ - 15
                                ins = e.matmul(outv, lhsT=wlag[:, li_, :], rhs=AP(upad, 15 + c0 * 31 - dl, [[UPW, 128], [31, 32], [1, 16]]),
                                               start=(li_ == 0), stop=(li_ == 30))
                            return ins
                        P.op('pe', ymm, r=['wlag', 'upad'], w=['py%d' % ip])
                        P.op('dve', lambda e, ip=ip, t4=t4: e.tensor_tensor(out=AP(ysum[ip], 0, [[512, 128], [16, 32], [1, 16]]),
                                                                            in0=AP(py[ip], 0, [[512, 128], [16, 32], [1, 16]]),
                                                                            in1=AP(crs, t4 * 32, [[2048, 128], [1, 32], [128, 16]]), op=ALU.add),
                             r=['py%d' % ip, 'crs'], w=['ysum%d' % ip])
                        P.op('act', lambda e, ip=ip: e.activation(out=ygs[ip][:], in_=ysum[ip][:], func=AF.Gelu), r=['ysum%d' % ip], w=['ygs%d' % ip])
                        P.dma('sp', yG_d[b, T, :, tt_ * 512:(tt_ + 1) * 512], ygs[ip][:], 'ygo%d' % ip, r=['ygs%d' % ip], w=['ygs%d' % ip])
                P.barrier()
            P.new_sems()
    if upto < 5:
        return nc, P, {}
    with ExitStack() as st:
        def SB(name, shape, dt):
            return st.enter_context(nc.sbuf_tensor(name, list(shape), dt))

        def PS(name, shape, dt=F32):
            return st.enter_context(nc.psum_tensor(name, list(shape), dt))
        stg = SB("stg", [128, 8, D], F32)
        wglub = SB("wglub", [128, 4, 512], BF16)
        woutb = SB("woutb", [128, 8, D], BF16)
        P.dma('sp', AP(stg, 0, [[8 * D, 128], [1, 2048]]), wglu.rearrange("p t n -> p (t n)"), 'c_wglu', w=['stg'])
        P.op('dve', lambda e: e.tensor_copy(out=wglub[:], in_=AP(stg, 0, [[8 * D, 128], [512, 4], [1, 512]])), r=['stg'], w=['wglub'])
        P.dma('sp', stg[:], woutw[:, :, :], 'c_wout', r=['wglub'], w=['stg'])
        P.op('dve', lambda e: e.tensor_copy(out=woutb[:], in_=stg[:]), r=['stg'], w=['woutb'])
        ygb = [SB("ygb%d" % i, [128, 4, 128], BF16) for i in range(2)]
        yfb = [SB("yfb%d" % i, [128, 4, 128], BF16) for i in range(2)]
        xs = [SB("xsd%d" % i, [128, D], F32) for i in range(2)]
        sg = SB("sg", [128, 4, 128], BF16)
        ysg = [SB("ysg%d" % i, [128, 4, 128], BF16) for i in range(2)]
        x1s = [SB("x1s%d" % i, [128, D], F32) for i in range(2)]
        sq = SB("sqd", [128, D], F32)
        ss = [SB("ssd%d" % i, [128, 4], F32) for i in range(2)]
        xn2 = [SB("xn2%d" % i, [128, D], BF16) for i in range(2)]
        hT2 = [SB("hT2%d" % i, [128, 8, 128], BF16) for i in range(2)]
        pg = PS("pg", [128, 4, 128], F32)
        po = [PS("pod%d" % i, [128, 512], F32) for i in range(2)]
        pT2 = PS("pT2", [128, 8, 128], BF16)
        nblk = NB * (S // 128)
        def d_s1(blk):
                b, tb_ = divmod(blk, S // 128)
                i = blk % 2
                t0 = tb_ * 128
                P.dma('sp', ygb[i][:], yG_d[b, :, :, t0:t0 + 128].rearrange("t p s -> p t s"), 'ygb%d' % i, w=['ygb%d' % i])
                P.dma('sp', yfb[i][:], yF_d[b, :, :, t0:t0 + 128].rearrange("t p s -> p t s"), 'yfb%d' % i, w=['yfb%d' % i])
                P.dma('sp', xs[i][:], x[b, t0:t0 + 128, :], 'xsd%d' % i, w=['xsd%d' % i])

                def glu(e, i=i):
                    for n in range(4):
                        for T in range(4):
                            ins = e.matmul(pg[:, n, :], lhsT=wglub[:, T, n * 128:(n + 1) * 128], rhs=ygb[i][:, T, :], start=(T == 0), stop=(T == 3))
                    return ins
                P.op('pe', glu, r=['wglub', 'ygb%d' % i], w=['pg'])
                P.op('act', lambda e: e.activation(out=sg[:], in_=pg[:], func=AF.Sigmoid), r=['pg'], w=['sg'])
                P.op('dve', lambda e, i=i: e.tensor_tensor(out=ysg[i][:], in0=sg[:], in1=ygb[i][:], op=ALU.mult), r=['sg', 'ygb%d' % i], w=['ysg%d' % i])

        def d_s2(blk):
                b, tb_ = divmod(blk, S // 128)
                i = blk % 2
                t0 = tb_ * 128
                for hf in range(2):
                    def omm(e, i=i, hf=hf):
                        for c8 in range(8):
                            lt = ysg[i][:, c8, :] if c8 < 4 else yfb[i][:, c8 - 4, :]
                            ins = e.matmul(po[hf][:, :], lhsT=lt, rhs=woutb[:, c8, hf * 512:(hf + 1) * 512], start=(c8 == 0), stop=(c8 == 7))
                        return ins
                    P.op('pe', omm, r=['ysg%d' % i, 'yfb%d' % i, 'woutb'], w=['pod%d' % hf])
                    P.op('dve', lambda e, i=i, hf=hf: e.tensor_tensor(out=x1s[i][:, hf * 512:(hf + 1) * 512], in0=po[hf][:, :], in1=xs[i][:, hf * 512:(hf + 1) * 512], op=ALU.add),
                         r=['pod%d' % hf, 'xsd%d' % i], w=['x1s%d_%d' % (i, hf)])
                P.dma('sp', x1_d[b, t0:t0 + 128, :], x1s[i][:], 'x1o%d' % i, r=['x1s%d_0' % i, 'x1s%d_1' % i], w=['x1o%d' % i])
                P.op('act', lambda e, i=i: e.activation(out=sq[:], in_=x1s[i][:], func=AF.Square, accum_out=ss[i][:, 0:1]),
                     r=['x1s%d_0' % i, 'x1s%d_1' % i], w=['sqd', 'ssa%d' % i])
                P.op('act', lambda e, i=i: e.activation(out=ss[i][:, 1:2], in_=ss[i][:, 0:1], func=AF.Sqrt, scale=1.0 / D, bias=epsc[:, 0:1]),
                     r=['ssa%d' % i, 'epsc'], w=['ssb%d' % i])
                P.op('dve', lambda e, i=i: e.reciprocal(out=ss[i][:, 2:3], in_=ss[i][:, 1:2]), r=['ssb%d' % i], w=['ssc%d' % i])
                P.op('dve', lambda e, i=i: e.tensor_scalar(out=xn2[i][:], in0=x1s[i][:], scalar1=ss[i][:, 2:3], scalar2=None, op0=ALU.mult),
                     r=['x1s%d_0' % i, 'x1s%d_1' % i, 'ssc%d' % i], w=['xn2%d' % i])


        def d_s3(blk):
                b, tb_ = divmod(blk, S // 128)
                i = blk % 2
                t0 = tb_ * 128
                def tr2(e, i=i):
                    for kc in range(8):
                        ins = e.transpose(out=pT2[:, kc, :], in_=xn2[i][:, kc * 128:(kc + 1) * 128], identity=identb[:])
                    return ins
                P.op('pe', tr2, r=['xn2%d' % i, 'identb'], w=['pT2'])
                P.op('act', lambda e, i=i: e.copy(out=hT2[i][:], in_=pT2[:]), r=['pT2'], w=['hT2%d' % i])
                P.dma('sp', h2T_d[b, :, :, t0:t0 + 128], hT2[i][:], 'h2o%d' % i, r=['hT2%d' % i], w=['hT2%d' % i])


        nbd = nblk if not quick else 2
        for step in range(nbd + 2):
            if step < nbd:
                d_s1(step)
            if 0 <= step - 1 < nbd:
                d_s2(step - 1)
            if 0 <= step - 2 < nbd:
                d_s3(step - 2)
        P.barrier()
    P.new_sems()
    if upto < 6:
        return nc, P, {}
    keysT_in = din("keysT_in", [128, 16, 128])
    iota128_in = din("iota128", [128, 128])
    iota16_in = din("iota16", [128, 16])
    G_d = dscr("G_d", [64, 128, 128, 128])
    wqb = SBp("wqb", [128, 8, 2048], BF16)
    keysb = SBp("keysb", [128, 16, 128], BF16)
    iota128 = SBp("iota128s", [128, 128], F32)
    iota16 = SBp("iota16s", [128, 16], F32)
    P.dma('sp', iota128[:], iota128_in[:, :], 'c_io128', w=['iota128'])
    P.dma('sp', iota16[:], iota16_in[:, :], 'c_io16', w=['iota16'])
    with ExitStack() as st:
        def SB(name, shape, dt):
            return st.enter_context(nc.sbuf_tensor(name, list(shape), dt))
        stq = SB("stq", [128, 4, 2048], F32)
        kst = SB("kst", [128, 16, 128], F32)
        P.dma('sp', kst[:], keysT_in[:, :, :], 'c_keys', w=['kst'])
        for hq in range(2):
            P.dma('sp', stq[:], wq[:, hq * 4:(hq + 1) * 4, :], 'c_wq', w=['stq'])
            for kk in range(4):
                kc = hq * 4 + kk
                P.op('dve', lambda e, kc=kc, kk=kk: e.tensor_scalar(out=wqb[:, kc, :], in0=stq[:, kk, :], scalar1=g2s[:, kc:kc + 1], scalar2=None, op0=ALU.mult),
                     r=['stq', 'g2s'], w=['wqb'])
        P.op('pool', lambda e: e.tensor_copy(out=keysb[:], in_=kst[:]), r=['kst'], w=['keysb'])
        P.barrier()
    P.new_sems()

    with ExitStack() as st:
        def SB(name, shape, dt):
            return st.enter_context(nc.sbuf_tensor(name, list(shape), dt))

        def PS(name, shape, dt=F32):
            return st.enter_context(nc.psum_tensor(name, list(shape), dt))
        NT = 128
        h2s = [SB("h2_%d" % i, [128, 8, NT], BF16) for i in range(2)]
        qTs = [SB("qT_%d" % i, [128, 16, NT], BF16) for i in range(2)]
        scss = [SB("scs_%d" % i, [128, 16, 128], F32) for i in range(2)]
        scr = SB("scr", [128, 256], F32)
        v16 = SB("v16", [128, 16, 16], F32)
        ix16 = SB("ix16", [128, 16, 16], U32)
        ixf = SB("ixf", [128, 16, 16], F32)
        cand = SB("cand", [128, 8, 256], F32)
        tv = SB("tv", [128, 8, 16], F32)
        tve = SB("tve", [128, 8, 16], F32)
        pos = SB("pos", [128, 8, 16], U32)
        posf = SB("posf", [128, 8, 16], F32)
        paf = SB("paf", [128, 8, 16], F32); pbf = SB("pbf", [128, 8, 16], F32)
        eq = SB("eq", [128, 8, 16, 16], F32)
        i16a = SB("i16a", [128, 16], F32); i16b = SB("i16b", [128, 16], F32)
        sel = SB("sel", [128, 3, 128], F32)
        selb = SB("selb", [128, 3, 128], BF16)
        selT = SB("selT", [128, 3, 128], BF16)
        iob = SB("iob", [128, 128], BF16)
        zsum = SB("zsum", [128, 8], F32)
        OJs = [SB("OJ%d" % i, [128, 32, 128], BF16) for i in range(2)]
        OIs = [SB("OI%d" % i, [128, 32, 128], BF16) for i in range(2)]
        Gsb = [SB("Gs%d" % i, [128, 128, NT], BF16) for i in range(2)]
        Bk = [PS("Bk%d" % i, [128, 512], F32) for i in range(7)]
        Bk7b = PS("Bk7b", [128, 1024], BF16)
        eq2 = cand

        def bk(i):
            return 'Bk%d' % i
        P.op('dve', lambda e: e.tensor_copy(out=iob[:], in_=iota128[:]), r=['iota128'], w=['iob'])
        P.op('dve', lambda e: e.tensor_scalar(out=i16a[:], in0=iota16[:], scalar1=16.0, scalar2=None, op0=ALU.mult), r=['iota16'], w=['i16a'])
        P.op('dve', lambda e: e.tensor_scalar(out=i16b[:], in0=iota16[:], scalar1=16.0, scalar2=16.0, op0=ALU.mult, op1=ALU.add), r=['iota16'], w=['i16b'])
        nblk = NB * S // NT
        def front(blk):
                b, tb_ = divmod(blk, S // NT)
                t0 = tb_ * NT
                par = blk % 2
                h2 = h2s[par]; qT = qTs[par]; scs = scss[par]
                Gs = Gsb[blk % 2]
                gsk = 'Gs%d' % (blk % 2)
                P.dma('sp', h2[:], h2T_d[b, :, :, t0:t0 + NT], 'h2_%d' % par, w=['h2_%d' % par])
                for m in range(16):
                    pb_ = 4 + (m % 2)

                    def qmm(e, m=m, pb_=pb_):
                        for kc in range(8):
                            ins = e.matmul(Bk[pb_][:, 0:NT], lhsT=wqb[:, kc, m * 128:(m + 1) * 128], rhs=h2[:, kc, :], start=(kc == 0), stop=(kc == 7))
                        return ins
                    P.op('pe', qmm, r=['wqb', 'h2_%d' % par], w=[bk(pb_)])
                    P.op('act', lambda e, m=m, pb_=pb_: e.copy(out=qT[:, m, :], in_=Bk[pb_][:, 0:NT]), r=[bk(pb_)], w=['qT_%d' % par])
                for m4 in range(4):
                    def smm2(e, m4=m4):
                        for mm in range(4):
                            m = m4 * 4 + mm
                            ins = e.matmul(Bk[m4][:, mm * 128:(mm + 1) * 128], lhsT=qT[:, m, :], rhs=keysb[:, m, :], start=True, stop=True)
                        return ins
                    P.op('pe', smm2, r=['qT_%d' % par, 'keysb'], w=[bk(m4)])
                    P.op('act', lambda e, m4=m4: e.copy(out=scs[:, m4 * 4:(m4 + 1) * 4, :], in_=Bk[m4][:, :]), r=[bk(m4)], w=['scs_%d' % par])

        def mid_a(blk):
                b, tb_ = divmod(blk, S // NT)
                t0 = tb_ * NT
                par = blk % 2
                h2 = h2s[par]; qT = qTs[par]; scs = scss[par]
                for m in range(16):
                    P.op('dve', lambda e, m=m: e.max(out=v16[:, m, 0:8], in_=scs[:, m, :]), r=['scs_%d' % par], w=['v16'])
                    P.op('dve', lambda e, m=m: e.max_index(out=ix16[:, m, 0:8], in_max=v16[:, m, 0:8], in_values=scs[:, m, :]), r=['scs_%d' % par, 'v16'], w=['ix16'])
                    P.op('dve', lambda e, m=m: e.match_replace(out=scr[:, 0:128], in_to_replace=v16[:, m, 0:8], in_values=scs[:, m, :], imm_value=-1e30),
                         r=['scs_%d' % par, 'v16'], w=['scr'])
                    P.op('dve', lambda e, m=m: e.max(out=v16[:, m, 8:16], in_=scr[:, 0:128]), r=['scr'], w=['v16'])
                    P.op('dve', lambda e, m=m: e.max_index(out=ix16[:, m, 8:16], in_max=v16[:, m, 8:16], in_values=scr[:, 0:128]), r=['scr', 'v16'], w=['ix16'])
                P.op('dve', lambda e: e.tensor_copy(out=ixf[:], in_=ix16[:]), r=['ix16'], w=['ixf'])
                P.op('dve', lambda e: e.tensor_tensor(out=AP(cand, 0, [[2048, 128], [256, 8], [16, 16], [1, 16]]),
                                                      in0=AP(v16, 0, [[256, 128], [32, 8], [1, 16], [0, 16]]),
                                                      in1=AP(v16, 16, [[256, 128], [32, 8], [0, 16], [1, 16]]), op=ALU.add), r=['v16'], w=['cand'])
                for h in range(8):
                    P.op('dve', lambda e, h=h: e.max(out=tv[:, h, 0:8], in_=cand[:, h, :]), r=['cand'], w=['tv'])
                    P.op('dve', lambda e, h=h: e.max_index(out=pos[:, h, 0:8], in_max=tv[:, h, 0:8], in_values=cand[:, h, :]), r=['cand', 'tv'], w=['pos'])
                    P.op('dve', lambda e, h=h: e.match_replace(out=scr[:, 0:256], in_to_replace=tv[:, h, 0:8], in_values=cand[:, h, :], imm_value=-1e30),
                         r=['cand', 'tv'], w=['scr'])
                    P.op('dve', lambda e, h=h: e.max(out=tv[:, h, 8:16], in_=scr[:, 0:256]), r=['scr'], w=['tv'])
                    P.op('dve', lambda e, h=h: e.max_index(out=pos[:, h, 8:16], in_max=tv[:, h, 8:16], in_values=scr[:, 0:256]), r=['scr', 'tv'], w=['pos'])

        def gate_(blk):
                b, tb_ = divmod(blk, S // NT)
                t0 = tb_ * NT
                par = blk % 2
                h2 = h2s[par]; qT = qTs[par]; scs = scss[par]
                P.op('dve', lambda e: e.tensor_tensor(out=tve[:], in0=tv[:], in1=AP(tv, 0, [[128, 128], [16, 8], [0, 16]]), op=ALU.subtract), r=['tv'], w=['tve'])
                P.op('act', lambda e: e.activation(out=tve[:], in_=tve[:], func=AF.Exp), r=['tve'], w=['tve'])

        def mid_b(blk):
                b, tb_ = divmod(blk, S // NT)
                t0 = tb_ * NT
                par = blk % 2
                h2 = h2s[par]; qT = qTs[par]; scs = scss[par]
                P.op('dve', lambda e: e.tensor_copy(out=posf[:], in_=pos[:]), r=['pos'], w=['posf'])
                posb = AP(posf, 0, [[128, 128], [16, 8], [1, 16], [0, 16]])
                P.op('dve', lambda e, posb=posb: e.tensor_tensor(out=eq[:], in0=posb, in1=AP(i16a, 0, [[16, 128], [0, 8], [0, 16], [1, 16]]), op=ALU.is_ge),
                     r=['posf', 'i16a'], w=['eq'])
                P.op('dve', lambda e, posb=posb: e.tensor_tensor(out=eq2[:].rearrange("p h (a b) -> p h a b", b=16) if False else AP(cand, 0, [[2048, 128], [256, 8], [16, 16], [1, 16]]),
                                                                in0=posb, in1=AP(i16b, 0, [[16, 128], [0, 8], [0, 16], [1, 16]]), op=ALU.is_ge),
                     r=['posf', 'i16b', 'pos'], w=['cand'])
                P.op('dve', lambda e: e.tensor_tensor(out=eq[:], in0=eq[:], in1=AP(cand, 0, [[2048, 128], [256, 8], [16, 16], [1, 16]]), op=ALU.subtract), r=['eq', 'cand'], w=['eq'])
                c4 = AP(cand, 0, [[2048, 128], [256, 8], [16, 16], [1, 16]])
                c3 = AP(cand, 0, [[2048, 128], [16, 128], [1, 16]])
                P.op('dve', lambda e, c4=c4: e.tensor_tensor(out=c4, in0=eq[:], in1=AP(ixf, 0, [[256, 128], [32, 8], [0, 16], [1, 16]]), op=ALU.mult), r=['eq', 'ixf'], w=['cand'])
                P.op('dve', lambda e, c3=c3: e.tensor_reduce(out=sel[:, 0, :], in_=c3, axis=AX.X, op=ALU.add), r=['cand'], w=['sel'])
                P.op('dve', lambda e, c4=c4: e.tensor_tensor(out=c4, in0=eq[:], in1=AP(iota16, 0, [[16, 128], [0, 8], [0, 16], [1, 16]]), op=ALU.mult), r=['eq', 'iota16'], w=['cand'])
                P.op('dve', lambda e, c3=c3: e.tensor_reduce(out=paf[:], in_=c3, axis=AX.X, op=ALU.add), r=['cand'], w=['paf'])
                P.op('dve', lambda e: e.scalar_tensor_tensor(out=pbf[:], in0=paf[:], scalar=-16.0, in1=posf[:], op0=ALU.mult, op1=ALU.add), r=['paf', 'posf'], w=['pbf'])
                P.op('dve', lambda e: e.tensor_tensor(out=eq[:], in0=AP(iota16, 0, [[16, 128], [0, 8], [0, 16], [1, 16]]),
                                                      in1=AP(pbf, 0, [[128, 128], [16, 8], [1, 16], [0, 16]]), op=ALU.is_equal), r=['iota16', 'pbf'], w=['eq'])
                P.op('dve', lambda e, c4=c4: e.tensor_tensor(out=c4, in0=eq[:], in1=AP(ixf, 16, [[256, 128], [32, 8], [0, 16], [1, 16]]), op=ALU.mult), r=['eq', 'ixf'], w=['cand'])
                P.op('dve', lambda e, c3=c3: e.tensor_reduce(out=sel[:, 1, :], in_=c3, axis=AX.X, op=ALU.add), r=['cand'], w=['sel'])
                P.op('dve', lambda e: e.tensor_reduce(out=zsum[:], in_=tve[:], axis=AX.X, op=ALU.add), r=['tve'], w=['zsum'])
                P.op('dve', lambda e: e.reciprocal(out=zsum[:], in_=zsum[:]), r=['zsum'], w=['zsum'])
                P.op('dve', lambda e: e.tensor_tensor(out=AP(sel, 256, [[384, 128], [16, 8], [1, 16]]), in0=tve[:], in1=AP(zsum, 0, [[8, 128], [1, 8], [0, 16]]), op=ALU.mult),
                     r=['tve', 'zsum'], w=['sel'])
                P.op('dve', lambda e: e.tensor_copy(out=selb[:], in_=sel[:]), r=['sel'], w=['selb'])

        def tail(blk):
                b, tb_ = divmod(blk, S // NT)
                t0 = tb_ * NT
                par = blk % 2
                h2 = h2s[par]; qT = qTs[par]; scs = scss[par]
                Gs = Gsb[blk % 2]
                gsk = 'Gs%d' % (blk % 2)

                def trs(e):
                    for c3_ in range(3):
                        ins = e.transpose(out=AP(Bk7b, c3_ * 128, [[1024, 128], [1, 128]]), in_=selb[:, c3_, :], identity=identb[:])
                    return ins
                P.op('pe', trs, r=['selb', 'identb'], w=['Bk7b'])
                P.op('act', lambda e: e.copy(out=selT[:], in_=AP(Bk7b, 0, [[1024, 128], [128, 3], [1, 128]])), r=['Bk7b'], w=['selT'])
                for tg in range(4):
                    io_b = AP(iob, 0, [[128, 128], [0, 32], [1, 128]])
                    OJ = OJs[tg % 2]; OI = OIs[tg % 2]; ojk = 'OJ%d' % (tg % 2); oik = 'OI%d' % (tg % 2)
                    P.op('dve', lambda e, io_b=io_b, tg=tg, OJ=OJ: e.tensor_tensor(out=OJ[:], in0=io_b, in1=AP(selT, 128 + tg * 32, [[384, 128], [1, 32], [0, 128]]), op=ALU.is_equal),
                         r=['iob', 'selT'], w=[ojk])
                    P.op('dve', lambda e, io_b=io_b, tg=tg, OI=OI: e.tensor_tensor(out=OI[:], in0=io_b, in1=AP(selT, tg * 32, [[384, 128], [1, 32], [0, 128]]), op=ALU.is_equal),
                         r=['iob', 'selT'], w=[oik])
                    P.op('pool', lambda e, tg=tg, OI=OI: e.tensor_tensor(out=OI[:], in0=OI[:], in1=AP(selT, 256 + tg * 32, [[384, 128], [1, 32], [0, 128]]), op=ALU.mult),
                         r=[oik, 'selT'], w=[oik])
                    for t4 in range(8):
                        pbk = (6, 4, 5, 0, 1, 2, 3)[(tg * 8 + t4) % 7]

                        def gmm(e, t4=t4, pbk=pbk, OJ=OJ, OI=OI):
                            for tq in range(4):
                                tl = t4 * 4 + tq
                                ins = e.matmul(AP(Bk[pbk], tq, [[512, 128], [4, 128]]), lhsT=OJ[:, tl, :], rhs=OI[:, tl, :], start=True, stop=True)
                            return ins
                        P.op('pe', gmm, r=[ojk, oik], w=[bk(pbk)])
                        P.op('act', lambda e, t4=t4, pbk=pbk, tg=tg, Gs=Gs: e.copy(out=AP(Gs, tg * 32 + t4 * 4, [[128 * NT, 128], [NT, 128], [1, 4]]),
                                                                           in_=AP(Bk[pbk], 0, [[512, 128], [4, 128], [1, 4]])), r=[bk(pbk)], w=[gsk])
                P.dma('sp', G_d[blk], Gs[:], 'gdo%d' % (blk % 2), r=[gsk], w=[gsk, 'Gd%d' % blk])

        nb1 = nblk if not quick else 1
        front(0)
        for blk in range(nb1):
            mid_a(blk)
            gate_(blk)
            if blk + 1 < nb1:
                front(blk + 1)
            mid_b(blk)
            tail(blk)
        P.barrier()

    with ExitStack() as st:
        def SB(name, shape, dt):
            return st.enter_context(nc.sbuf_tensor(name, list(shape), dt))

        def PS(name, shape, dt=F32):
            return st.enter_context(nc.psum_tensor(name, list(shape), dt))
        NTM = 384
        h2e = SB("h2e", [128, 8, NTM], BF16)
        utb = [SB("utb%d" % i, [128, 8, 128], BF16) for i in range(8)]
        vtb = [SB("vtb%d" % i, [128, 1024], BF16) for i in range(8)]
        gq = [SB("gq%d" % i, [128, 3, 4, 128], BF16) for i in range(2)]
        glb = [SB("glb%d" % i, [128, NTM], BF16) for i in range(2)]
        actb = [SB("actb%d" % i, [128, NTM], BF16) for i in range(2)]
        x1b = SB("x1b", [128, D], F32)
        o2 = SB("o2", [128, D], F32)
        sqe = SB("sqe", [128, D], F32)
        sse = SB("sse", [128, 4], F32)
        Bo = [PS("Bo%d" % i, [128, 512], F32) for i in range(6)]
        Bp = [PS("Bp%d" % i, [128, 512], F32) for i in range(2)]
        blocks = []
        for b in range(NB):
            t = 0
            for nt in [384] * 10 + [256]:
                blocks.append((b, t, nt))
                t += nt
        for (b, t0, nt) in (blocks if not quick else blocks[:1]):
            nsub = nt // 128
            tb0 = (b * S + t0) // 128
            P.dma('sp', h2e[:, :, 0:nt], h2T_d[b, :, :, t0:t0 + nt], 'h2e', w=['h2e'])
            def emit_pre(ci):
                sl = ci % 8
                P.dma('sp', utb[sl][:], UTb_d[ci], 'utb%d' % sl, w=['utb%d' % sl])
                P.dma('pool', vtb[sl][:], Vb_d[ci], 'vtb%d' % sl, w=['vtb%d' % sl])
                gsl = (ci // 4) % 2
                if ci % 4 == 0:
                    for sub in range(nsub):
                        P.dma('sp', gq[gsl][:, sub, :, :], G_d[tb0 + sub, :, ci:ci + 4, :], 'gq%d' % gsl, r=['Gd%d' % (tb0 + sub)], w=['gq%d' % gsl])
                pp = ci % 2

                def pmm(e, sl=sl, pp=pp, nt=nt):
                    for kc in range(8):
                        ins = e.matmul(Bp[pp][:, 0:nt], lhsT=utb[sl][:, kc, :], rhs=h2e[:, kc, 0:nt], start=(kc == 0), stop=(kc == 7))
                    return ins
                P.op('pe', pmm, r=['utb%d' % sl, 'h2e'], w=['Bp%d' % pp])
                P.op('act', lambda e, pp=pp, nt=nt: e.activation(out=glb[pp][:, 0:nt], in_=Bp[pp][:, 0:nt], func=AF.Gelu), r=['Bp%d' % pp], w=['glb%d' % pp])
                P.op('dve', lambda e, pp=pp, nt=nt, nsub=nsub, gsl=gsl, ci=ci: e.tensor_tensor(
                    out=AP(actb[pp], 0, [[NTM, 128], [128, nsub], [1, 128]]), in0=AP(glb[pp], 0, [[NTM, 128], [128, nsub], [1, 128]]),
                    in1=AP(gq[gsl], (ci % 4) * 128, [[1536, 128], [512, nsub], [1, 128]]), op=ALU.mult),
                    r=['glb%d' % pp, 'gq%d' % gsl], w=['actb%d' % pp])

            def emit_out(ci):
                sl = ci % 8
                pp = ci % 2
                def omm(e, sl=sl, pp=pp, nsub=nsub, ci=ci):
                    for sub in range(nsub):
                        for hf in range(2):
                            ins = e.matmul(Bo[sub * 2 + hf][:, :], lhsT=actb[pp][:, sub * 128:(sub + 1) * 128], rhs=vtb[sl][:, hf * 512:(hf + 1) * 512],
                                           start=(ci == 0), stop=(ci == 127))
                    return ins
                P.op('pe', omm, r=['actb%d' % pp, 'vtb%d' % sl], w=['Bo'])
            emit_pre(0)
            for ci in range(128):
                if ci + 1 < 128:
                    emit_pre(ci + 1)
                emit_out(ci)
            for sub in range(nsub):
                tt0 = t0 + sub * 128
                P.dma('sp', x1b[:], x1_d[b, tt0:tt0 + 128, :], 'x1b', w=['x1b'])
                for hf in range(2):
                    P.op('dve', lambda e, sub=sub, hf=hf: e.tensor_tensor(out=o2[:, hf * 512:(hf + 1) * 512], in0=Bo[sub * 2 + hf][:, :], in1=x1b[:, hf * 512:(hf + 1) * 512], op=ALU.add),
                         r=['Bo', 'x1b'], w=['o2_%d' % hf])
                P.op('act', lambda e: e.activation(out=sqe[:], in_=o2[:], func=AF.Square, accum_out=sse[:, 0:1]), r=['o2_0', 'o2_1'], w=['sqe', 'ssea'])
                P.op('act', lambda e: e.activation(out=sse[:, 1:2], in_=sse[:, 0:1], func=AF.Sqrt, scale=1.0 / D, bias=epsc[:, 0:1]), r=['ssea', 'epsc'], w=['sseb'])
                P.op('dve', lambda e: e.reciprocal(out=sse[:, 2:3], in_=sse[:, 1:2]), r=['sseb'], w=['ssec'])
                P.op('dve', lambda e: e.scalar_tensor_tensor(out=sqe[:], in0=o2[:], scalar=sse[:, 2:3], in1=gfin_s[:], op0=ALU.mult, op1=ALU.mult),
                     r=['o2_0', 'o2_1', 'ssec', 'gfin', 'sqe'], w=['sqe'])
                P.dma('sp', y[b, tt0:tt0 + 128, :], sqe[:], 'yo', r=['sqe'], w=['yo'])
        P.barrier()
    return nc, P, {}


def host_inputs(inp):
    f = np.float32

    def kmaj(w):
        K, N = w.shape
        return np.ascontiguousarray(w.reshape(K // 128, 128, N).transpose(1, 0, 2)).astype(f)
    com = {}
    com["w1"] = kmaj(inp["w_in"][0])
    com["g1"] = np.ascontiguousarray(inp["norm1_g"][0].reshape(8, 128).T).astype(f)
    com["wout"] = kmaj(inp["w_out"][0])
    com["wglu"] = kmaj(inp["w_glu"][0])
    com["wq"] = kmaj(inp["w_query"][0])
    com["g2"] = np.ascontiguousarray(inp["norm2_g"][0].reshape(8, 128).T).astype(f)
    com["gfin"] = np.ascontiguousarray(np.broadcast_to(inp["final_g"][None, :], (128, D))).astype(f)
    com["wf"] = np.ascontiguousarray(inp["w_fourier"][0].transpose(1, 0, 2)).astype(f)
    c = np.arange(128)
    ang = 2 * np.pi * np.outer(c, c) / 128.0
    sc = 1.0 / math.sqrt(S * 128)
    com["ccsc"] = np.stack([np.cos(ang) * sc, -np.sin(ang) * sc], axis=1).astype(f)
    com["ident"] = np.eye(128, dtype=f)
    s_idx = np.arange(S)
    ks = (np.outer(s_idx, s_idx) % S).astype(np.float64) * (2 * np.pi / S)
    ct = np.cos(ks).astype(f).astype(ml_dtypes.bfloat16)
    stt = np.sin(ks).astype(f).astype(ml_dtypes.bfloat16)
    t = np.stack([ct, stt], axis=0)
    t = t.reshape(2, 32, 128, 16, 256).transpose(3, 2, 0, 1, 4)
    com["tab"] = np.ascontiguousarray(t)
    com["iota128"] = np.ascontiguousarray(np.broadcast_to(np.arange(128, dtype=f)[None, :], (128, 128)))
    com["iota16"] = np.ascontiguousarray(np.broadcast_to(np.arange(16, dtype=f)[None, :], (128, 16)))

    def lamA(arr):
        return np.ascontiguousarray(arr.reshape(2, 4, 4, 2, 64).transpose(3, 4, 1, 2, 0).reshape(128, 32)).astype(f)
    com["lam_are"] = lamA(inp["ssm_a_re"][0])
    com["lam_aim"] = lamA(inp["ssm_a_im"][0])
    com["lam_lst"] = lamA(np.broadcast_to(inp["ssm_log_step"][0][:, :, None], (2, 32, 64)))
    bA = np.zeros((2, 4, 128, 8, 128), f)
    cA = np.zeros((2, 4, 128, 8, 128), f)
    for ri, (bsrc, csrc) in enumerate(((inp["ssm_b_re"][0], inp["ssm_c_re"][0]), (inp["ssm_b_im"][0], inp["ssm_c_im"][0]))):
        for T in range(4):
            for q in range(4):
                for gp in range(2):
                    g = 8 * T + 2 * q + gp
                    for dr in range(2):
                        col = (2 * q + gp) * 16
                        bA[ri, T, gp * 64:(gp + 1) * 64, q * 2 + dr, col:col + 16] = bsrc[dr, g]
                        cA[ri, T, gp * 64:(gp + 1) * 64, q * 2 + dr, col:col + 16] = csrc[dr, g].T
    com["bA"] = bA
    com["cA"] = cA
    com["dD"] = np.ascontiguousarray(inp["ssm_d"][0].reshape(4, 128).T).astype(f)
    eu = inp["expert_u"][0]
    com["uT_in"] = np.ascontiguousarray(eu.reshape(16384, 8, 128).transpose(2, 1, 0)).astype(f)
    com["v_in"] = np.ascontiguousarray(inp["expert_v"][0].reshape(128, 128, 1024)).astype(f)
    sk = inp["sub_keys"][0]
    com["keysT_in"] = np.ascontiguousarray(sk.reshape(16, 128, 128).transpose(2, 0, 1)).astype(f)
    return com


_CACHE = {}


def kernel(**inp):
    if "nc" not in _CACHE:
        _CACHE["nc"] = build()[0]
    nc = _CACHE["nc"]
    com = host_inputs(inp)
    xs = np.ascontiguousarray(inp["x"]).astype(np.float32)
    in_maps = []
    for c in range(8):
        m = dict(com)
        m["x"] = xs[2 * c:2 * c + 2]
        in_maps.append(m)
    res = run_bass_kernel_spmd(nc, in_maps, core_ids=list(range(8)))
    return np.concatenate([np.asarray(r["y"]) for r in res.results], axis=0).astype(np.float32)
```

```python
import math
from contextlib import ExitStack
import numpy as np
import ml_dtypes
import concourse.bass as bass
import concourse.mybir as mybir
from concourse.bass_utils import run_bass_kernel_spmd

F32 = mybir.dt.float32
BF16 = mybir.dt.bfloat16
U32 = mybir.dt.uint32
ALU = mybir.AluOpType
AF = mybir.ActivationFunctionType
AX = mybir.AxisListType

NB = 2
S = 4096
D = 1024
L = 16
NCH = S // L
PADW = 2 * L - 1
EPS = 1e-6
ENG = ['pe', 'act', 'dve', 'pool', 'sp']


class Prog:
    def __init__(s, nc):
        s.nc = nc
        s.e = dict(pe=nc.tensor, act=nc.scalar, dve=nc.vector, pool=nc.gpsimd, sp=nc.sync)
        s.nsem = 0
        s.new_sems()
        s.dsem = {}
        s.lastw = {}
        s.readers = {}
        s.items = {k: [] for k in ENG}

    def new_sems(s):
        if s.nsem > 0:
            return
        s.sem = {}
        for k in ENG:
            s.sem[k] = s.nc.alloc_semaphore("es%d_%s" % (s.nsem, k))
        s.nsem += 1
        s.cnt = {k: 0 for k in ENG}
        s.waited = {}

    def _wait(s, eng, tok):
        kind, name, val = tok
        h = s.sem[name] if kind == 'e' else s.dsem[name][0]
        key = (eng, h.num)
        if s.waited.get(key, 0) >= val:
            return
        s.waited[key] = val
        s.items[eng].append(('w', h, val))

    def _deps(s, eng, r, w):
        deps = []
        for k in r:
            if k in s.lastw:
                deps.append((s.lastw[k], True))
        for k in w:
            if k in s.lastw:
                deps.append((s.lastw[k], False))
            for t in s.readers.get(k, ()):
                deps.append((t, False))
        for tok, raw in deps:
            if tok[0] == 'e' and tok[1] == eng:
                if raw and s.cnt[eng] - tok[2] < 2:
                    s._wait(eng, tok)
                continue
            s._wait(eng, tok)

    def _upd(s, tok, r, w):
        for k in r:
            s.readers.setdefault(k, []).append(tok)
        for k in w:
            s.lastw[k] = tok
            s.readers[k] = []

    def op(s, eng, fn, r=(), w=()):
        s._deps(eng, r, w)
        s.cnt[eng] += 1
        s.items[eng].append(('o', fn, s.sem[eng]))
        s._upd(('e', eng, s.cnt[eng]), r, w)

    def dma(s, eng, out, in_, sem, r=(), w=()):
        s._deps(eng, r, w)
        if sem not in s.dsem:
            if getattr(s, 'free_d', None):
                s.free_d.sort(key=lambda hc: hc[1])
                s.dsem[sem] = s.free_d.pop(0)
            else:
                s.ndsem = getattr(s, 'ndsem', 0) + 1
                s.dsem[sem] = [s.nc.alloc_semaphore("ds%d" % s.ndsem), 0]
        d = s.dsem[sem]
        d[1] += 16
        s.items[eng].append(('d', out, in_, d[0]))
        s._upd(('d', sem, d[1]), r, w)

    def barrier(s, final=False):
        toks = [('e', k, s.cnt[k]) for k in ENG if s.cnt[k] > 0]
        toks += [('d', n, d[1]) for n, d in s.dsem.items() if d[1] > 0]
        for eng in (['sp'] if final else ENG):
            for tok in toks:
                if tok[0] == 'e' and tok[1] == eng:
                    continue
                s._wait(eng, tok)
        s.lastw.clear()
        s.readers.clear()
        s.flush()
        if not hasattr(s, 'free_d'):
            s.free_d = []
        s.free_d.extend(s.dsem.values())
        s.dsem = {}

    def flush(s):
        def replay(items, embed=False):
            def f(e):
                pend = []
                for it in items:
                    if it[0] == 'w':
                        if embed:
                            pend.append(it)
                        else:
                            e.wait_ge(it[1], it[2])
                    elif it[0] == 'o':
                        for p in pend[:-1]:
                            e.wait_ge(p[1], p[2])
                        ins = it[1](e)
                        if pend:
                            ins._wait_ge(pend[-1][1], pend[-1][2])
                        pend = []
                        ins.then_inc(it[2], 1)
                    else:
                        e.dma_start(out=it[1], in_=it[2]).then_inc(it[3], 16)
            return f
        with s.nc.Block() as block:
            for k, dec in (('pe', block.tensor), ('act', block.scalar), ('dve', block.vector), ('pool', block.gpsimd), ('sp', block.sync)):
                if s.items[k]:
                    dec(replay(s.items[k], embed=False))
        s.items = {k: [] for k in ENG}


def AP(t, off, dims):
    return bass.AP(t, off, [list(d) for d in dims])


def build(debug=(), nblk_lim=None, stage=99, upto=99, quick=False):
    nc = bass.Bass("TRN2", target_bir_lowering=False)
    P = Prog(nc)

    def din(name, shape, dt=F32):
        return nc.dram_tensor(name, list(shape), dt, kind="ExternalInput").ap()

    dbg = {}

    def dscr(name, shape, dt=BF16):
        kind = "ExternalOutput" if name in debug else "Internal"
        a = nc.dram_tensor(name, list(shape), dt, kind=kind).ap()
        return a

    x = din("x", [NB, S, D])
    w1 = din("w1", [128, 8, D])
    g1 = din("g1", [128, 8])
    woutw = din("wout", [128, 8, D])
    wglu = din("wglu", [128, 4, 512])
    wq = din("wq", [128, 8, 2048])
    g2 = din("g2", [128, 8])
    gfin = din("gfin", [128, D])
    wf = din("wf", [128, 4, 128])
    ccsc = din("ccsc", [128, 2, 128])
    ident_in = din("ident", [128, 128])
    tab = din("tab", [16, 128, 2, 32, 256], BF16)
    y = nc.dram_tensor("y", [NB, S, D], F32, kind="ExternalOutput").ap()

    zS_d = dscr("zS_d", [NB, 4, 128, S])
    A_d = dscr("A_d", [NB, 32, 128, 1024])
    yF_d = dscr("yF_d", [NB, 4, 128, S])
    yG_d = dscr("yG_d", [NB, 4, 128, S])
    x1_d = dscr("x1_d", [NB, S, D], F32)
    h2T_d = dscr("h2T_d", [NB, 128, 8, S])

    def dump(name, tile, shape, dt, key):
        if name in debug:
            d = nc.dram_tensor(name, list(shape), dt, kind="ExternalOutput").ap()
            P.dma('sp', d, tile, 'dbg_' + name, r=[key], w=['dbg_' + name])

    pst = ExitStack()

    def SBp(name, shape, dt):
        return pst.enter_context(nc.sbuf_tensor(name, list(shape), dt))

    identb = SBp("identb", [128, 128], BF16)
    identf = SBp("identf", [128, 128], F32)
    gfin_s = SBp("gfin_s", [128, D], F32)
    epsc = SBp("epsc", [128, 1], F32)

    P.dma('sp', identf[:], ident_in[:, :], 'c_identf', w=['identf'])
    P.dma('sp', gfin_s[:], gfin[:, :], 'c_gfin', w=['gfin'])
    P.op('dve', lambda e: e.tensor_copy(out=identb[:], in_=identf[:]), r=['identf'], w=['identb'])
    P.op('dve', lambda e: e.memset(epsc[:], EPS), w=['epsc'])

    uT_in = din("uT_in", [128, 8, 16384])
    v_in = din("v_in", [128, 128, 1024])
    UTb_d = dscr("UTb_d", [128, 128, 8, 128])
    Vb_d = dscr("Vb_d", [128, 128, 1024])
    g2s = SBp("g2s", [128, 8], F32)
    P.dma('sp', g2s[:], g2[:, :], 'c_g2s', w=['g2s'])
    e0st = ExitStack()
    uf0 = e0st.enter_context(nc.sbuf_tensor("uf0", [128, 8, 512], F32))
    ub0 = e0st.enter_context(nc.sbuf_tensor("ub0", [128, 8, 512], BF16))
    vf0 = e0st.enter_context(nc.sbuf_tensor("vf0", [128, 4, 1024], F32))
    vb0 = e0st.enter_context(nc.sbuf_tensor("vb0", [128, 4, 1024], BF16))

    def e0_iter(it):
        P.dma('pool', uf0[:], uT_in[:, :, it * 512:(it + 1) * 512], 'uf0', w=['uf0'])
        for kc in range(8):
            P.op('dve', lambda e, kc=kc: e.tensor_scalar(out=ub0[:, kc, :], in0=uf0[:, kc, :], scalar1=g2s[:, kc:kc + 1], scalar2=None, op0=ALU.mult),
                 r=['uf0', 'g2s'], w=['ub0_%d' % kc] + ['ubo0_%d' % i4 for i4 in range(4)])
        for i4 in range(4):
            P.dma('pool', UTb_d[it * 4 + i4], AP(ub0, i4 * 128, [[4096, 128], [512, 8], [1, 128]]),
                  'ubo0', r=['ub0_%d' % kc for kc in range(8)], w=['ubo0_%d' % i4])
        P.dma('pool', vf0[:], v_in[it * 4:(it + 1) * 4].rearrange("i p d -> p i d"), 'vf0', w=['vf0'])
        P.op('act', lambda e: e.copy(out=vb0[:], in_=vf0[:]), r=['vf0'], w=['vb0'])
        P.dma('pool', Vb_d[it * 4:(it + 1) * 4].rearrange("i p d -> p i d"), vb0[:], 'vbo0', r=['vb0'], w=['vb0'])

    with ExitStack() as st:
        def SB(name, shape, dt):
            return st.enter_context(nc.sbuf_tensor(name, list(shape), dt))

        def PS(name, shape, dt=F32):
            return st.enter_context(nc.psum_tensor(name, list(shape), dt))

        w1f = SB("w1f", [128, 8, D], F32)
        w1b = SB("w1b", [128, 8, D], BF16)
        g1s = SB("g1s", [128, 8], F32)
        wfs = SB("wfs", [128, 4, 128], F32)
        ccs = SB("ccs", [128, 2, 128], F32)
        csw = SB("csw", [128, 4, 256], BF16)
        wfb = SB("wfb", [128, 4, 128], BF16)
        ccb = SB("ccb", [128, 2, 128], BF16)
        xs = [SB("xs%d" % i, [128, D], F32) for i in range(2)]
        sq = SB("sq", [128, D], F32)
        ss = [SB("ss%d" % i, [128, 4], F32) for i in range(2)]
        xn = [SB("xn%d" % i, [128, D], BF16) for i in range(2)]
        hT = [SB("hT%d" % i, [128, 8, 128], BF16) for i in range(2)]
        zT = [SB("zT%d" % i, [128, 8, 128], BF16) for i in range(2)]
        Ab = [SB("Ab%d" % i, [128, 1024], BF16) for i in range(2)]
        pT = [PS("pT%d" % i, [128, 8, 128], BF16) for i in range(2)]
        pz = [PS("pz%d" % i, [128, 8, 128], F32) for i in range(1)]
        pA = [PS("pA%d" % i, [128, 1024], F32) for i in range(1)]
        pw = PS("pw", [128, 512], F32)

        P.dma('sp', w1f[:], w1[:, :, :], 'c_w1f', w=['w1f'])
        P.dma('sp', g1s[:], g1[:, :], 'c_g1s', w=['g1s'])
        P.dma('sp', wfs[:], wf[:, :, :], 'c_wfs', w=['wfs'])
        P.dma('sp', ccs[:], ccsc[:, :, :], 'c_ccs', w=['ccs'])
        for kc in range(8):
            P.op('dve', lambda e, kc=kc: e.tensor_scalar(out=w1b[:, kc, :], in0=w1f[:, kc, :], scalar1=g1s[:, kc:kc + 1],
                                                        scalar2=None, op0=ALU.mult), r=['w1f', 'g1s'], w=['w1b'])
        P.op('dve', lambda e: e.tensor_copy(out=wfb[:], in_=wfs[:]), r=['wfs'], w=['wfb'])
        P.op('dve', lambda e: e.tensor_copy(out=ccb[:], in_=ccs[:]), r=['ccs'], w=['ccb'])
        for g in range(4 if stage >= 0 else 0):
            for t in range(2):
                P.op('pe', lambda e, g=g, t=t: e.matmul(pw[:, (g % 2) * 256 + t * 128:(g % 2) * 256 + t * 128 + 128],
                                                        lhsT=ccb[:, t, :], rhs=wfb[:, g, :], start=True, stop=True),
                     r=['ccb', 'wfb'], w=['pw'])
            P.op('dve', lambda e, g=g: e.tensor_copy(out=csw[:, g, :], in_=pw[:, (g % 2) * 256:(g % 2) * 256 + 256]),
                 r=['pw'], w=['csw'])

        nblk = NB * (S // 128) if nblk_lim is None else nblk_lim
        if stage == -2:
            nblk = 0
        for blk in range(nblk):
            b, tb = divmod(blk, S // 128)
            i = blk % 2
            t0 = tb * 128
            if blk % 2 == 0:
                e0_iter(blk // 2)
            P.dma('sp', xs[i][:], x[b, t0:t0 + 128, :], 'xs%d' % i, w=['xs%d' % i])
            P.op('act', lambda e, i=i: e.activation(out=sq[:], in_=xs[i][:], func=AF.Square, accum_out=ss[i][:, 0:1]),
                 r=['xs%d' % i], w=['sq', 'ssa%d' % i])
            P.op('act', lambda e, i=i: e.activation(out=ss[i][:, 1:2], in_=ss[i][:, 0:1], func=AF.Sqrt, scale=1.0 / D, bias=epsc[:, 0:1]),
                 r=['ssa%d' % i, 'epsc'], w=['ssb%d' % i])
            P.op('dve', lambda e, i=i: e.reciprocal(out=ss[i][:, 2:3], in_=ss[i][:, 1:2]), r=['ssb%d' % i], w=['ssc%d' % i])
            P.op('dve', lambda e, i=i: e.tensor_scalar(out=xn[i][:], in0=xs[i][:], scalar1=ss[i][:, 2:3], scalar2=None, op0=ALU.mult),
                 r=['xs%d' % i, 'ssc%d' % i], w=['xn%d' % i])

            if stage < 1:
                continue
            def tr(e, i=i):
                for kc in range(8):
                    ins = e.transpose(out=pT[i][:, kc, :], in_=xn[i][:, kc * 128:(kc + 1) * 128], identity=identb[:])
                return ins
            P.op('pe', tr, r=['xn%d' % i, 'identb'], w=['pT%d' % i])
            P.op('act', lambda e, i=i: e.copy(out=hT[i][:], in_=pT[i][:]), r=['pT%d' % i], w=['hT%d' % i])

            if stage < 2:
                continue
            def zmm(e, i=i):
                for n in range(8):
                    for kc in range(8):
                        ins = e.matmul(pz[0][:, n, :], lhsT=w1b[:, kc, n * 128:(n + 1) * 128], rhs=hT[i][:, kc, :],
                                       start=(kc == 0), stop=(kc == 7))
                return ins
            P.op('pe', zmm, r=['hT%d' % i, 'w1b'], w=['pz'])
            P.op('dve', lambda e, i=i: e.tensor_copy(out=zT[i][:], in_=pz[0][:]), r=['pz'], w=['zT%d' % i])
            if blk == 0:
                dump('d_xn', xn[i][:], [128, D], BF16, 'xn%d' % i)
                dump('d_hT', hT[i][:], [128, 8, 128], BF16, 'hT%d' % i)
                dump('d_zT', zT[i][:], [128, 8, 128], BF16, 'zT%d' % i)
                dump('d_w1b', w1b[:], [128, 8, D], BF16, 'w1b')
            P.dma('sp', zS_d[b, :, :, t0:t0 + 128].rearrange("t p s -> p t s"), zT[i][:, 0:4, :], 'zso%d' % i, r=['zT%d' % i], w=['zso%d' % i])

            if stage < 3:
                continue
            def amm(e, i=i):
                for g in range(4):
                    ins = e.matmul(pA[0][:, g * 256:(g + 1) * 256], lhsT=zT[i][:, 4 + g, :], rhs=csw[:, g, :], start=True, stop=True)
                return ins
            P.op('pe', amm, r=['zT%d' % i, 'csw'], w=['pA'])
            P.op('act', lambda e, i=i: e.copy(out=Ab[i][:], in_=pA[0][:]), r=['pA'], w=['Ab%d' % i])
            P.dma('sp', A_d[b, tb, :, :], Ab[i][:], 'ao%d' % i, r=['Ab%d' % i], w=['ao%d' % i])
        P.barrier()
    e0st.close()
    P.new_sems()
    if upto < 2:
        return nc, P, {}

    with ExitStack() as st:
        def SB(name, shape, dt):
            return st.enter_context(nc.sbuf_tensor(name, list(shape), dt))

        def PS(name, shape, dt=F32):
            return st.enter_context(nc.psum_tensor(name, list(shape), dt))
        Asb = SB("Asb", [128, 32, 1024], BF16)
        tbs = [SB("tb%d" % i, [128, 2, 32, 256], BF16) for i in range(2)]
        yst = [SB("yst%d" % i, [128, 4, 256], BF16) for i in range(2)]
        pf = [PS("pf%d" % i, [128, 512], F32) for i in range(4)]
        for b in range(NB):
            P.dma('sp', Asb[:], A_d[b].rearrange("s p n -> p s n"), 'Asb', w=['Asb'])
            for kt in range(16 if not quick else 1):
                it = (b * 16 + kt) % 2
                P.dma('sp', tbs[it][:], tab[kt], 'tb%d' % it, w=['tb%d' % it])
                for g in range(4):
                    def fmm(e, g=g, it=it):
                        n = 0
                        for cs in range(2):
                            for sc in range(32):
                                ins = e.matmul(pf[g][:, 0:256], lhsT=Asb[:, sc, g * 256 + cs * 128:g * 256 + cs * 128 + 128],
                                               rhs=tbs[it][:, cs, sc, :], start=(n == 0), stop=(n == 63))
                                n += 1
                        return ins
                    P.op('pe', fmm, r=['Asb', 'tb%d' % it], w=['pf%d' % g])
                    if g % 2:
                        P.op('act', lambda e, g=g, it=it: e.copy(out=yst[it][:, g, :], in_=pf[g][:, 0:256]), r=['pf%d' % g], w=['yst%d_%d' % (it, g)])
                    else:
                        P.op('dve', lambda e, g=g, it=it: e.tensor_copy(out=yst[it][:, g, :], in_=pf[g][:, 0:256]), r=['pf%d' % g], w=['yst%d_%d' % (it, g)])
                P.dma('sp', yF_d[b, :, :, kt * 256:(kt + 1) * 256].rearrange("g p s -> p g s"), yst[it][:], 'yfo%d' % it,
                      r=['yst%d_%d' % (it, g) for g in range(4)], w=['yst%d_%d' % (it, g) for g in range(4)])
        P.barrier()
    P.new_sems()
    if upto < 3:
        return nc, P, {}

    lam_are = din("lam_are", [128, 32]); lam_aim = din("lam_aim", [128, 32]); lam_lst = din("lam_lst", [128, 32])
    bA = din("bA", [2, 4, 128, 8, 128]); cA = din("cA", [2, 4, 128, 8, 128]); dD = din("dD", [128, 4])
    WLAG_d = dscr("WLAG_d", [4, 128, 31, 128])
    WIN_d = dscr("WIN_d", [4, 16, 128, 16, 128])
    WOUT_d = dscr("WOUT_d", [4, 16, 128, 16, 128])
    prt = SBp("prt", [128, 17, 32], F32)
    pit = SBp("pit", [128, 17, 32], F32)
    with ExitStack() as st:
        def SB(name, shape, dt):
            return st.enter_context(nc.sbuf_tensor(name, list(shape), dt))

        def PS(name, shape, dt=F32):
            return st.enter_context(nc.psum_tensor(name, list(shape), dt))
        names = ['are', 'aim', 'lst', 'stp', 'ar', 'ai', 'mag', 's16', 'cc', 'sn', 't1', 't2', 't3', 'lr', 'li', 'den', 'nr', 'cr', 'ci']
        T_ = {n: SB("l_" + n, [128, 32], F32) for n in names}
        P.dma('sp', T_['are'][:], lam_are[:, :], 'c_are', w=['are'])
        P.dma('sp', T_['aim'][:], lam_aim[:, :], 'c_aim', w=['aim'])
        P.dma('sp', T_['lst'][:], lam_lst[:, :], 'c_lst', w=['lst'])
        dDs = SB("dDs", [128, 4], F32)
        P.dma('sp', dDs[:], dD[:, :], 'c_dD', w=['dD'])

        def tt(o, a, b_, op, eng='dve'):
            P.op(eng, lambda e: e.tensor_tensor(out=T_[o][:], in0=T_[a][:], in1=T_[b_][:], op=op), r=[a, b_], w=[o])

        def ts(o, a, s1, s2, op0, op1=None):
            if op1 is None:
                P.op('dve', lambda e: e.tensor_scalar(out=T_[o][:], in0=T_[a][:], scalar1=s1, scalar2=None, op0=op0), r=[a], w=[o])
            else:
                P.op('dve', lambda e: e.tensor_scalar(out=T_[o][:], in0=T_[a][:], scalar1=s1, scalar2=s2, op0=op0, op1=op1), r=[a], w=[o])

        def act(o, a, func, scale=1.0):
            P.op('act', lambda e: e.activation(out=T_[o][:], in_=T_[a][:], func=func, scale=scale), r=[a], w=[o])
        act('stp', 'lst', AF.Exp)
        tt('ar', 'are', 'stp', ALU.mult)
        tt('ai', 'aim', 'stp', ALU.mult)
        act('mag', 'ar', AF.Exp)
        act('s16', 'ai', AF.Sin, 1.0 / 16)
        act('sn', 'ai', AF.Sin, 1.0 / 8)
        tt('t1', 's16', 's16', ALU.mult)
        ts('cc', 't1', -2.0, 1.0, ALU.mult, ALU.add)
        for _ in range(3):
            tt('t1', 'cc', 'cc', ALU.mult)
            tt('t2', 'sn', 'sn', ALU.mult)
            tt('t3', 'cc', 'sn', ALU.mult)
            tt('cc', 't1', 't2', ALU.subtract)
            ts('sn', 't3', 2.0, None, ALU.mult)
        tt('lr', 'mag', 'cc', ALU.mult)
        tt('li', 'mag', 'sn', ALU.mult)
        tt('t1', 'are', 'are', ALU.mult)
        tt('t2', 'aim', 'aim', ALU.mult)
        tt('den', 't1', 't2', ALU.add)
        P.op('dve', lambda e: e.reciprocal(out=T_['den'][:], in_=T_['den'][:]), r=['den'], w=['den'])
        ts('nr', 'lr', -1.0, None, ALU.add)
        tt('t1', 'nr', 'are', ALU.mult)
        tt('t2', 'li', 'aim', ALU.mult)
        tt('t3', 't1', 't2', ALU.add)
        tt('cr', 't3', 'den', ALU.mult)
        tt('t1', 'li', 'are', ALU.mult)
        tt('t2', 'nr', 'aim', ALU.mult)
        tt('t3', 't1', 't2', ALU.subtract)
        tt('ci', 't3', 'den', ALU.mult)
        P.op('dve', lambda e: e.memset(prt[:, 0, :], 1.0), w=['prt'])
        P.op('dve', lambda e: e.memset(pit[:, 0, :], 0.0), w=['pit'])
        for k in range(16):
            P.op('dve', lambda e, k=k: e.tensor_tensor(out=T_['t1'][:], in0=prt[:, k, :], in1=T_['lr'][:], op=ALU.mult), r=['prt', 'lr'], w=['t1'])
            P.op('dve', lambda e, k=k: e.tensor_tensor(out=T_['t2'][:], in0=pit[:, k, :], in1=T_['li'][:], op=ALU.mult), r=['pit', 'li'], w=['t2'])
            P.op('dve', lambda e, k=k: e.tensor_tensor(out=T_['t3'][:], in0=prt[:, k, :], in1=T_['li'][:], op=ALU.mult), r=['prt', 'li'], w=['t3'])
            P.op('dve', lambda e, k=k: e.tensor_tensor(out=T_['nr'][:], in0=pit[:, k, :], in1=T_['lr'][:], op=ALU.mult), r=['pit', 'lr'], w=['nr'])
            P.op('dve', lambda e, k=k: e.tensor_tensor(out=prt[:, k + 1, :], in0=T_['t1'][:], in1=T_['t2'][:], op=ALU.subtract), r=['t1', 't2'], w=['prt'])
            P.op('dve', lambda e, k=k: e.tensor_tensor(out=pit[:, k + 1, :], in0=T_['t3'][:], in1=T_['nr'][:], op=ALU.add), r=['t3', 'nr'], w=['pit'])

        bre = SB("bre", [128, 8, 128], F32); bim = SB("bim", [128, 8, 128], F32)
        cre = SB("cre", [128, 8, 128], F32); cim = SB("cim", [128, 8, 128], F32)
        Br = SB("Br", [128, 8, 128], F32); Bi = SB("Bi", [128, 8, 128], F32)
        u1 = SB("u1", [128, 8, 128], F32); u2 = SB("u2", [128, 8, 128], F32)
        v1 = SB("v1", [128, 8, 128], F32); v2 = SB("v2", [128, 8, 128], F32)
        Cxr = SB("Cxr", [128, 8, 128], BF16); Cxi = SB("Cxi", [128, 8, 128], BF16)
        Pkr = [SB("Pkr%d" % i, [128, 8, 128], BF16) for i in range(2)]
        Pki = [SB("Pki%d" % i, [128, 8, 128], BF16) for i in range(2)]
        CLr = [SB("CLr%d" % i, [128, 8, 128], BF16) for i in range(2)]
        CLi = [SB("CLi%d" % i, [128, 8, 128], BF16) for i in range(2)]
        winst = [SB("winst%d" % i, [128, 16, 128], BF16) for i in range(2)]
        wlags = SB("wlags", [128, 31, 128], BF16)
        ddiag = SB("ddiag", [128, 128], F32)
        ptk = [PS("ptk%d" % i, [128, 512], F32) for i in range(2)]
        ptr = [PS("ptr%d" % i, [128, 8, 128], BF16) for i in range(2)]

        def bc(tile, off, pstep):
            return AP(tile, off, [[pstep, 128], [1, 8], [0, 128]])
        for T in range(4):
            P.dma('sp', bre[:], bA[0, T], 'c_bre', w=['bre']); P.dma('sp', bim[:], bA[1, T], 'c_bim', w=['bim'])
            P.dma('sp', cre[:], cA[0, T], 'c_cre', w=['cre']); P.dma('sp', cim[:], cA[1, T], 'c_cim', w=['cim'])
            crb = bc(T_['cr'], T * 8, 32); cib = bc(T_['ci'], T * 8, 32)
            P.op('dve', lambda e, crb=crb: e.tensor_tensor(out=u1[:], in0=bre[:], in1=crb, op=ALU.mult), r=['bre', 'cr'], w=['u1'])
            P.op('dve', lambda e, cib=cib: e.tensor_tensor(out=u2[:], in0=bim[:], in1=cib, op=ALU.mult), r=['bim', 'ci'], w=['u2'])
            P.op('dve', lambda e: e.tensor_tensor(out=Br[:], in0=u1[:], in1=u2[:], op=ALU.subtract), r=['u1', 'u2'], w=['Br'])
            P.op('dve', lambda e, crb=crb: e.tensor_tensor(out=u1[:], in0=bim[:], in1=crb, op=ALU.mult), r=['bim', 'cr'], w=['u1'])
            P.op('dve', lambda e, cib=cib: e.tensor_tensor(out=u2[:], in0=bre[:], in1=cib, op=ALU.mult), r=['bre', 'ci'], w=['u2'])
            P.op('dve', lambda e: e.tensor_tensor(out=Bi[:], in0=u1[:], in1=u2[:], op=ALU.add), r=['u1', 'u2'], w=['Bi'])
            P.op('pool', lambda e: e.tensor_copy(out=Cxr[:], in_=cre[:]), r=['cre'], w=['Cxr'])
            P.op('pool', lambda e: e.tensor_scalar(out=Cxi[:], in0=cim[:], scalar1=-1.0, scalar2=None, op0=ALU.mult), r=['cim'], w=['Cxi'])
            P.op('dve', lambda e, T=T: e.tensor_scalar(out=ddiag[:], in0=identf[:], scalar1=dDs[:, T:T + 1], scalar2=None, op0=ALU.mult),
                 r=['identf', 'dD'], w=['ddiag'])
            for k in range(17):
                i2 = k % 2
                pkr = bc(prt, k * 32 + T * 8, 17 * 32); pki = bc(pit, k * 32 + T * 8, 17 * 32)
                if k <= 15:
                    P.op('dve', lambda e, pkr=pkr: e.tensor_tensor(out=u1[:], in0=Br[:], in1=pkr, op=ALU.mult), r=['Br', 'prt'], w=['u1'])
                    P.op('dve', lambda e, pki=pki: e.tensor_tensor(out=u2[:], in0=Bi[:], in1=pki, op=ALU.mult), r=['Bi', 'pit'], w=['u2'])
                    P.op('dve', lambda e, i2=i2: e.tensor_tensor(out=Pkr[i2][:], in0=u1[:], in1=u2[:], op=ALU.subtract), r=['u1', 'u2'], w=['Pkr%d' % i2])
                    P.op('dve', lambda e, pki=pki: e.tensor_tensor(out=u1[:], in0=Br[:], in1=pki, op=ALU.mult), r=['Br', 'pit'], w=['u1'])
                    P.op('dve', lambda e, pkr=pkr: e.tensor_tensor(out=u2[:], in0=Bi[:], in1=pkr, op=ALU.mult), r=['Bi', 'prt'], w=['u2'])
                    P.op('dve', lambda e, i2=i2: e.tensor_tensor(out=Pki[i2][:], in0=u1[:], in1=u2[:], op=ALU.add), r=['u1', 'u2'], w=['Pki%d' % i2])
                    if k == 0:
                        def tk0(e, i2=i2):
                            n = 0
                            for dr in range(2):
                                for q in range(4):
                                    for (Pt, Ct) in ((Pkr[i2], Cxr), (Pki[i2], Cxi)):
                                        ins = e.matmul(ptk[0][:, 0:128], lhsT=Pt[:, q * 2 + dr, :], rhs=Ct[:, q * 2 + dr, :], start=(n == 0), stop=(n == 15))
                                        n += 1
                            return ins
                        P.op('pe', tk0, r=['Pkr%d' % i2, 'Pki%d' % i2, 'Cxr', 'Cxi'], w=['ptk0'])
                        P.op('dve', lambda e: e.tensor_tensor(out=wlags[:, 15, :], in0=ptk[0][:, 0:128], in1=ddiag[:], op=ALU.add),
                             r=['ptk0', 'ddiag'], w=['wlags'])
                    else:
                        for dr in range(2):
                            def tk(e, i2=i2, dr=dr):
                                n = 0
                                for q in range(4):
                                    for (Pt, Ct) in ((Pkr[i2], Cxr), (Pki[i2], Cxi)):
                                        ins = e.matmul(ptk[dr][:, 0:128], lhsT=Pt[:, q * 2 + dr, :], rhs=Ct[:, q * 2 + dr, :], start=(n == 0), stop=(n == 7))
                                        n += 1
                                return ins
                            P.op('pe', tk, r=['Pkr%d' % i2, 'Pki%d' % i2, 'Cxr', 'Cxi'], w=['ptk%d' % dr])
                            li_ = 15 + k if dr == 0 else 15 - k
                            P.op('act', lambda e, dr=dr, li_=li_: e.copy(out=wlags[:, li_, :], in_=ptk[dr][:, 0:128]), r=['ptk%d' % dr], w=['wlags'])
                    for h in range(2):
                        def trw(e, i2=i2, h=h):
                            for m in range(8):
                                idx = h * 8 + m
                                cb, ri = idx // 2, idx % 2
                                ins = e.transpose(out=ptr[h][:, m, :], in_=(Pkr[i2] if ri == 0 else Pki[i2])[:, cb, :], identity=identb[:])
                            return ins
                        P.op('pe', trw, r=['Pkr%d' % i2, 'Pki%d' % i2, 'identb'], w=['ptr%d' % h])
                        P.op('act', lambda e, i2=i2, h=h: e.copy(out=winst[i2][:, h * 8:(h + 1) * 8, :], in_=ptr[h][:]), r=['ptr%d' % h], w=['winst%d' % i2])
                    P.dma('sp', WIN_d[T, k], winst[i2][:], 'wino%d' % i2, r=['winst%d' % i2], w=['winst%d' % i2])
                if k >= 1:
                    P.op('dve', lambda e, pkr=pkr: e.tensor_tensor(out=u1[:], in0=cre[:], in1=pkr, op=ALU.mult), r=['cre', 'prt'], w=['u1'])
                    P.op('dve', lambda e, pki=pki: e.tensor_tensor(out=u2[:], in0=cim[:], in1=pki, op=ALU.mult), r=['cim', 'pit'], w=['u2'])
                    P.op('dve', lambda e, i2=i2: e.tensor_tensor(out=CLr[i2][:], in0=u1[:], in1=u2[:], op=ALU.subtract), r=['u1', 'u2'], w=['CLr%d' % i2])
                    P.op('pool', lambda e, pki=pki: e.tensor_tensor(out=v1[:], in0=cre[:], in1=pki, op=ALU.mult), r=['cre', 'pit'], w=['v1'])
                    P.op('pool', lambda e, pkr=pkr: e.tensor_tensor(out=v2[:], in0=cim[:], in1=pkr, op=ALU.mult), r=['cim', 'prt'], w=['v2'])
                    P.op('pool', lambda e: e.tensor_tensor(out=v1[:], in0=v1[:], in1=v2[:], op=ALU.add), r=['v1', 'v2'], w=['v1'])
                    P.op('pool', lambda e, i2=i2: e.tensor_scalar(out=CLi[i2][:], in0=v1[:], scalar1=-1.0, scalar2=None, op0=ALU.mult), r=['v1'], w=['CLi%d' % i2])
                    wd = WOUT_d[T, k - 1].rearrange("p (c r) m -> p c r m", r=2)
                    P.dma('sp', wd[:, :, 0, :], CLr[i2][:], 'wouto%d' % i2, r=['CLr%d' % i2], w=['CLr%d' % i2])
                    P.dma('sp', wd[:, :, 1, :], CLi[i2][:], 'woutp%d' % i2, r=['CLi%d' % i2], w=['CLi%d' % i2])
            P.dma('sp', WLAG_d[T], wlags[:], 'wlago', r=['wlags'], w=['wlags'])
        P.barrier()
    P.new_sems()
    if upto < 4:
        return nc, P, {}
    UPW = 7968
    for b in range(NB):
        for T in range(4):
            if quick and (b > 0 or T > 0):
                continue
            with ExitStack() as st:
                def SB(name, shape, dt, b=b, T=T):
                    return st.enter_context(nc.sbuf_tensor("%s_%d_%d" % (name, b, T), list(shape), dt))

                def PS(name, shape, dt=F32, b=b, T=T):
                    return st.enter_context(nc.psum_tensor("%s_%d_%d" % (name, b, T), list(shape), dt))
                zs = SB("zs", [128, S], BF16)
                upad = SB("upad", [128, UPW], BF16)
                wlag = SB("wlag", [128, 31, 128], BF16)
                Hs = [SB("Hs%d" % i, [128, 257, 16], F32) for i in range(2)]
                Xh = SB("Xh", [128, 4, 2, 2, 256], BF16)
                Ssb = SB("Ssb", [128, 2, 256, 8], F32)
                wins = [SB("win%d" % i, [128, 16, 4, 128], BF16) for i in range(2)]
                wout = SB("wout_s", [128, 16, 16, 128], BF16)
                A12 = SB("A12", [128, 2, 16], F32)
                tP = [SB("tP%d" % i, [128, 16], F32) for i in range(2)]
                tQ = [SB("tQ%d" % i, [128, 8], F32) for i in range(2)]
                ygs = [SB("ygs%d" % i, [128, 512], BF16) for i in range(2)]
                ysum = [SB("ysum%d" % i, [128, 512], F32) for i in range(2)]
                crs = SB("crs", [128, 16, 128], F32)
                pss = [PS("pss%d" % i, [128, 512], F32) for i in range(4)]
                py = [PS("py%d" % i, [128, 512], F32) for i in range(2)]

                P.dma('sp', zs[:], zS_d[b, T], 'zs', w=['zs'])
                P.dma('sp', wlag[:], WLAG_d[T], 'wlag', w=['wlag'])
                P.dma('sp', wout[:], WOUT_d[T].rearrange("k p m c -> p k m c"), 'wout', w=['wout'])
                P.op('pool', lambda e: e.memset(upad[:], 0.0), w=['upad'])
                P.op('pool', lambda e: e.tensor_copy(out=AP(upad, 15, [[UPW, 128], [31, 256], [1, 16]]),
                                                     in_=AP(zs, 0, [[S, 128], [16, 256], [1, 16]])), r=['zs'], w=['upad'])
                for dr in range(2):
                    src_r = AP(prt, 16 * 32 + T * 8 + dr, [[17 * 32, 128], [2, 4]])
                    src_i = AP(pit, 16 * 32 + T * 8 + dr, [[17 * 32, 128], [2, 4]])
                    P.op('dve', lambda e, dr=dr, src_r=src_r: e.tensor_copy(out=A12[:, dr, 0:4], in_=src_r), r=['prt'], w=['A12'])
                    P.op('dve', lambda e, dr=dr, src_r=src_r: e.tensor_copy(out=A12[:, dr, 4:8], in_=src_r), r=['prt'], w=['A12'])
                    P.op('dve', lambda e, dr=dr, src_i=src_i: e.tensor_scalar(out=A12[:, dr, 8:12], in0=src_i, scalar1=-1.0, scalar2=None, op0=ALU.mult), r=['pit'], w=['A12'])
                    P.op('dve', lambda e, dr=dr, src_i=src_i: e.tensor_copy(out=A12[:, dr, 12:16], in_=src_i), r=['pit'], w=['A12'])
                P.op('dve', lambda e: e.memset(Hs[0][:], 0.0), w=['H0'])
                P.op('pool', lambda e: e.memset(Hs[1][:], 0.0), w=['H1'])
                for q in range(4):
                    win = wins[q % 2]
                    wkey = 'win%d' % (q % 2)
                    P.dma('sp', win[:], WIN_d[T, :, :, q * 4:(q + 1) * 4, :].rearrange("k p m c -> p k m c"), wkey, w=[wkey])
                    for dr in range(2):
                        for ri in range(2):
                            pi_ = dr * 2 + ri

                            def smm(e, dr=dr, ri=ri, pi_=pi_, win=win):
                                for jp in range(16):
                                    kk = 15 - jp if dr == 0 else jp
                                    ins = e.matmul(pss[pi_][:, 0:256], lhsT=win[:, kk, dr * 2 + ri, :],
                                                   rhs=AP(upad, 15 + jp, [[UPW, 128], [31, 256]]), start=(jp == 0), stop=(jp == 15))
                                return ins
                            P.op('pe', smm, r=[wkey, 'upad'], w=['pss%d' % pi_])
                            P.op('act', lambda e, dr=dr, ri=ri, q=q, pi_=pi_: e.copy(out=AP(Ssb, dr * 2048 + ri * 4 + q, [[4096, 128], [8, 256]]),
                                                                                     in_=pss[pi_][:, 0:256]), r=['pss%d' % pi_], w=['Ssb'])
                for step in range(256 if not quick else 256):
                    for dr, eng in ((0, 'dve'), (1, 'pool')):
                        H = Hs[dr]
                        if dr == 0:
                            src, dst, sc_ = step, step + 1, step
                        else:
                            src, dst, sc_ = 256 - step, 255 - step, 255 - step
                        hk = 'H%d' % dr
                        HW = 257 * 16
                        P.op(eng, lambda e, H=H, src=src, dr=dr, HW=HW: e.tensor_tensor(out=AP(tP[dr], 0, [[16, 128], [8, 2], [1, 8]]),
                                                                                 in0=AP(H, src * 16, [[HW, 128], [4, 2], [1, 8]]),
                                                                                 in1=AP(A12, dr * 16, [[32, 128], [8, 2], [1, 8]]), op=ALU.mult),
                             r=[hk, 'A12'], w=['tP%d' % dr])
                        P.op(eng, lambda e, dr=dr: e.tensor_tensor(out=tQ[dr][:], in0=tP[dr][:, 0:8], in1=tP[dr][:, 8:16], op=ALU.add),
                             r=['tP%d' % dr], w=['tQ%d' % dr])
                        P.op(eng, lambda e, H=H, dst=dst, dr=dr, sc_=sc_, HW=HW: e.tensor_tensor(out=AP(H, dst * 16, [[HW, 128], [8, 2], [1, 8]]),
                                                                                          in0=AP(tQ[dr], 0, [[8, 128], [0, 2], [1, 8]]),
                                                                                          in1=AP(Ssb, dr * 2048 + sc_ * 8, [[4096, 128], [0, 2], [1, 8]]), op=ALU.add),
                             r=['tQ%d' % dr, 'Ssb'], w=[hk])
                P.op('dve', lambda e: e.tensor_copy(out=AP(Xh, 0, [[4096, 128], [1024, 4], [256, 2], [1, 256]]),
                                                    in_=AP(Hs[0], 0, [[257 * 16, 128], [1, 4], [4, 2], [16, 256]])), r=['H0'], w=['Xh'])
                P.op('pool', lambda e: e.tensor_copy(out=AP(Xh, 512, [[4096, 128], [1024, 4], [256, 2], [1, 256]]),
                                                     in_=AP(Hs[1], 16, [[257 * 16, 128], [1, 4], [4, 2], [16, 256]])), r=['H1'], w=['Xh'])
                for half in range(2):
                    h0 = half * 128
                    for g4 in range(4):
                        def cmm(e, g4=g4, h0=h0):
                            for jj in range(4):
                                j = g4 * 4 + jj
                                n = 0
                                for q in range(4):
                                    for dr in range(2):
                                        kidx = j if dr == 0 else 15 - j
                                        for ri in range(2):
                                            ins = e.matmul(pss[g4][:, jj * 128:(jj + 1) * 128], lhsT=wout[:, kidx, q * 4 + dr * 2 + ri, :],
                                                           rhs=Xh[:, q, dr, ri, h0:h0 + 128], start=(n == 0), stop=(n == 15))
                                            n += 1
                            return ins
                        P.op('pe', cmm, r=['wout', 'Xh'], w=['pss%d' % g4])
                        P.op('act', lambda e, g4=g4: e.copy(out=crs[:, g4 * 4:(g4 + 1) * 4, :], in_=pss[g4][:, :]), r=['pss%d' % g4], w=['crs'])
                    for t4 in range(4):
                        tt_ = half * 4 + t4
                        c0 = tt_ * 32
                        ip = tt_ % 2

                        def ymm(e, c0=c0, ip=ip):
                            outv = AP(py[ip], 0, [[512, 128], [16, 32], [1, 16]])
                            for li_ in range(31):
                                dl = li_ - 15
                                ins = e.matmul(outv, lhsT=wlag[:, li_, :], rhs=AP(upad, 15 + c0 * 31 - dl, [[UPW, 128], [31, 32], [1, 16]]),
                                               start=(li_ == 0), stop=(li_ == 30))
                            return ins
                        P.op('pe', ymm, r=['wlag', 'upad'], w=['py%d' % ip])
                        P.op('dve', lambda e, ip=ip, t4=t4: e.tensor_tensor(out=AP(ysum[ip], 0, [[512, 128], [16, 32], [1, 16]]),
                                                                            in0=AP(py[ip], 0, [[512, 128], [16, 32], [1, 16]]),
                                                                            in1=AP(crs, t4 * 32, [[2048, 128], [1, 32], [128, 16]]), op=ALU.add),
                             r=['py%d' % ip, 'crs'], w=['ysum%d' % ip])
                        P.op('act', lambda e, ip=ip: e.activation(out=ygs[ip][:], in_=ysum[ip][:], func=AF.Gelu), r=['ysum%d' % ip], w=['ygs%d' % ip])
                        P.dma('sp', yG_d[b, T, :, tt_ * 512:(tt_ + 1) * 512], ygs[ip][:], 'ygo%d' % ip, r=['ygs%d' % ip], w=['ygs%d' % ip])
                P.barrier()
            P.new_sems()
    if upto < 5:
        return nc, P, {}
    with ExitStack() as st:
        def SB(name, shape, dt):
            return st.enter_context(nc.sbuf_tensor(name, list(shape), dt))

        def PS(name, shape, dt=F32):
            return st.enter_context(nc.psum_tensor(name, list(shape), dt))
        stg = SB("stg", [128, 8, D], F32)
        wglub = SB("wglub", [128, 4, 512], BF16)
        woutb = SB("woutb", [128, 8, D], BF16)
        P.dma('sp', AP(stg, 0, [[8 * D, 128], [1, 2048]]), wglu.rearrange("p t n -> p (t n)"), 'c_wglu', w=['stg'])
        P.op('dve', lambda e: e.tensor_copy(out=wglub[:], in_=AP(stg, 0, [[8 * D, 128], [512, 4], [1, 512]])), r=['stg'], w=['wglub'])
        P.dma('sp', stg[:], woutw[:, :, :], 'c_wout', r=['wglub'], w=['stg'])
        P.op('dve', lambda e: e.tensor_copy(out=woutb[:], in_=stg[:]), r=['stg'], w=['woutb'])
        ygb = [SB("ygb%d" % i, [128, 4, 128], BF16) for i in range(2)]
        yfb = [SB("yfb%d" % i, [128, 4, 128], BF16) for i in range(2)]
        xs = [SB("xsd%d" % i, [128, D], F32) for i in range(2)]
        sg = SB("sg", [128, 4, 128], BF16)
        ysg = [SB("ysg%d" % i, [128, 4, 128], BF16) for i in range(2)]
        x1s = [SB("x1s%d" % i, [128, D], F32) for i in range(2)]
        sq = SB("sqd", [128, D], F32)
        ss = [SB("ssd%d" % i, [128, 4], F32) for i in range(2)]
        xn2 = [SB("xn2%d" % i, [128, D], BF16) for i in range(2)]
        hT2 = [SB("hT2%d" % i, [128, 8, 128], BF16) for i in range(2)]
        pg = PS("pg", [128, 4, 128], F32)
        po = [PS("pod%d" % i, [128, 512], F32) for i in range(2)]
        pT2 = PS("pT2", [128, 8, 128], BF16)
        nblk = NB * (S // 128)
        def d_s1(blk):
                b, tb_ = divmod(blk, S // 128)
                i = blk % 2
                t0 = tb_ * 128
                P.dma('sp', ygb[i][:], yG_d[b, :, :, t0:t0 + 128].rearrange("t p s -> p t s"), 'ygb%d' % i, w=['ygb%d' % i])
                P.dma('sp', yfb[i][:], yF_d[b, :, :, t0:t0 + 128].rearrange("t p s -> p t s"), 'yfb%d' % i, w=['yfb%d' % i])
                P.dma('sp', xs[i][:], x[b, t0:t0 + 128, :], 'xsd%d' % i, w=['xsd%d' % i])

                def glu(e, i=i):
                    for n in range(4):
                        for T in range(4):
                            ins = e.matmul(pg[:, n, :], lhsT=wglub[:, T, n * 128:(n + 1) * 128], rhs=ygb[i][:, T, :], start=(T == 0), stop=(T == 3))
                    return ins
                P.op('pe', glu, r=['wglub', 'ygb%d' % i], w=['pg'])
                P.op('act', lambda e: e.activation(out=sg[:], in_=pg[:], func=AF.Sigmoid), r=['pg'], w=['sg'])
                P.op('dve', lambda e, i=i: e.tensor_tensor(out=ysg[i][:], in0=sg[:], in1=ygb[i][:], op=ALU.mult), r=['sg', 'ygb%d' % i], w=['ysg%d' % i])

        def d_s2(blk):
                b, tb_ = divmod(blk, S // 128)
                i = blk % 2
                t0 = tb_ * 128
                for hf in range(2):
                    def omm(e, i=i, hf=hf):
                        for c8 in range(8):
                            lt = ysg[i][:, c8, :] if c8 < 4 else yfb[i][:, c8 - 4, :]
                            ins = e.matmul(po[hf][:, :], lhsT=lt, rhs=woutb[:, c8, hf * 512:(hf + 1) * 512], start=(c8 == 0), stop=(c8 == 7))
                        return ins
                    P.op('pe', omm, r=['ysg%d' % i, 'yfb%d' % i, 'woutb'], w=['pod%d' % hf])
                    P.op('dve', lambda e, i=i, hf=hf: e.tensor_tensor(out=x1s[i][:, hf * 512:(hf + 1) * 512], in0=po[hf][:, :], in1=xs[i][:, hf * 512:(hf + 1) * 512], op=ALU.add),
                         r=['pod%d' % hf, 'xsd%d' % i], w=['x1s%d_%d' % (i, hf)])
                P.dma('sp', x1_d[b, t0:t0 + 128, :], x1s[i][:], 'x1o%d' % i, r=['x1s%d_0' % i, 'x1s%d_1' % i], w=['x1o%d' % i])
                P.op('act', lambda e, i=i: e.activation(out=sq[:], in_=x1s[i][:], func=AF.Square, accum_out=ss[i][:, 0:1]),
                     r=['x1s%d_0' % i, 'x1s%d_1' % i], w=['sqd', 'ssa%d' % i])
                P.op('act', lambda e, i=i: e.activation(out=ss[i][:, 1:2], in_=ss[i][:, 0:1], func=AF.Sqrt, scale=1.0 / D, bias=epsc[:, 0:1]),
                     r=['ssa%d' % i, 'epsc'], w=['ssb%d' % i])
                P.op('dve', lambda e, i=i: e.reciprocal(out=ss[i][:, 2:3], in_=ss[i][:, 1:2]), r=['ssb%d' % i], w=['ssc%d' % i])
                P.op('dve', lambda e, i=i: e.tensor_scalar(out=xn2[i][:], in0=x1s[i][:], scalar1=ss[i][:, 2:3], scalar2=None, op0=ALU.mult),
                     r=['x1s%d_0' % i, 'x1s%d_1' % i, 'ssc%d' % i], w=['xn2%d' % i])


        def d_s3(blk):
                b, tb_ = divmod(blk, S // 128)
                i = blk % 2
                t0 = tb_ * 128
                def tr2(e, i=i):
                    for kc in range(8):
                        ins = e.transpose(out=pT2[:, kc, :], in_=xn2[i][:, kc * 128:(kc + 1) * 128], identity=identb[:])
                    return ins
                P.op('pe', tr2, r=['xn2%d' % i, 'identb'], w=['pT2'])
                P.op('act', lambda e, i=i: e.copy(out=hT2[i][:], in_=pT2[:]), r=['pT2'], w=['hT2%d' % i])
                P.dma('sp', h2T_d[b, :, :, t0:t0 + 128], hT2[i][:], 'h2o%d' % i, r=['hT2%d' % i], w=['hT2%d' % i])


        nbd = nblk if not quick else 2
        for step in range(nbd + 2):
            if step < nbd:
                d_s1(step)
            if 0 <= step - 1 < nbd:
                d_s2(step - 1)
            if 0 <= step - 2 < nbd:
                d_s3(step - 2)
        P.barrier()
    P.new_sems()
    if upto < 6:
        return nc, P, {}
    keysT_in = din("keysT_in", [128, 16, 128])
    iota128_in = din("iota128", [128, 128])
    iota16_in = din("iota16", [128, 16])
    G_d = dscr("G_d", [64, 128, 128, 128])
    wqb = SBp("wqb", [128, 8, 2048], BF16)
    keysb = SBp("keysb", [128, 16, 128], BF16)
    iota128 = SBp("iota128s", [128, 128], F32)
    iota16 = SBp("iota16s", [128, 16], F32)
    P.dma('sp', iota128[:], iota128_in[:, :], 'c_io128', w=['iota128'])
    P.dma('sp', iota16[:], iota16_in[:, :], 'c_io16', w=['iota16'])
    with ExitStack() as st:
        def SB(name, shape, dt):
            return st.enter_context(nc.sbuf_tensor(name, list(shape), dt))
        stq = SB("stq", [128, 4, 2048], F32)
        kst = SB("kst", [128, 16, 128], F32)
        P.dma('sp', kst[:], keysT_in[:, :, :], 'c_keys', w=['kst'])
        for hq in range(2):
            P.dma('sp', stq[:], wq[:, hq * 4:(hq + 1) * 4, :], 'c_wq', w=['stq'])
            for kk in range(4):
                kc = hq * 4 + kk
                P.op('dve', lambda e, kc=kc, kk=kk: e.tensor_scalar(out=wqb[:, kc, :], in0=stq[:, kk, :], scalar1=g2s[:, kc:kc + 1], scalar2=None, op0=ALU.mult),
                     r=['stq', 'g2s'], w=['wqb'])
        P.op('pool', lambda e: e.tensor_copy(out=keysb[:], in_=kst[:]), r=['kst'], w=['keysb'])
        P.barrier()
    P.new_sems()

    with ExitStack() as st:
        def SB(name, shape, dt):
            return st.enter_context(nc.sbuf_tensor(name, list(shape), dt))

        def PS(name, shape, dt=F32):
            return st.enter_context(nc.psum_tensor(name, list(shape), dt))
        NT = 128
        h2s = [SB("h2_%d" % i, [128, 8, NT], BF16) for i in range(2)]
        qTs = [SB("qT_%d" % i, [128, 16, NT], BF16) for i in range(2)]
        scss = [SB("scs_%d" % i, [128, 16, 128], F32) for i in range(2)]
        scr = SB("scr", [128, 256], F32)
        v16 = SB("v16", [128, 16, 16], F32)
        ix16 = SB("ix16", [128, 16, 16], U32)
        ixf = SB("ixf", [128, 16, 16], F32)
        cand = SB("cand", [128, 8, 256], F32)
        tv = SB("tv", [128, 8, 16], F32)
        tve = SB("tve", [128, 8, 16], F32)
        pos = SB("pos", [128, 8, 16], U32)
        posf = SB("posf", [128, 8, 16], F32)
        paf = SB("paf", [128, 8, 16], F32); pbf = SB("pbf", [128, 8, 16], F32)
        eq = SB("eq", [128, 8, 16, 16], F32)
        i16a = SB("i16a", [128, 16], F32); i16b = SB("i16b", [128, 16], F32)
        sel = SB("sel", [128, 3, 128], F32)
        selb = SB("selb", [128, 3, 128], BF16)
        selT = SB("selT", [128, 3, 128], BF16)
        iob = SB("iob", [128, 128], BF16)
        selTn = SB("selTn", [128, 128], F32)
        zsum = SB("zsum", [128, 8], F32)
        OJs = [SB("OJ%d" % i, [128, 32, 128], BF16) for i in range(2)]
        OIs = [SB("OI%d" % i, [128, 32, 128], BF16) for i in range(2)]
        Gsb = [SB("Gs%d" % i, [128, 128, NT], BF16) for i in range(2)]
        Bk = [PS("Bk%d" % i, [128, 512], F32) for i in range(7)]
        Bk7b = PS("Bk7b", [128, 1024], BF16)
        eq2 = cand

        def bk(i):
            return 'Bk%d' % i
        P.op('dve', lambda e: e.tensor_copy(out=iob[:], in_=iota128[:]), r=['iota128'], w=['iob'])
        P.op('dve', lambda e: e.tensor_scalar(out=i16a[:], in0=iota16[:], scalar1=16.0, scalar2=None, op0=ALU.mult), r=['iota16'], w=['i16a'])
        P.op('dve', lambda e: e.tensor_scalar(out=i16b[:], in0=iota16[:], scalar1=16.0, scalar2=16.0, op0=ALU.mult, op1=ALU.add), r=['iota16'], w=['i16b'])
        nblk = NB * S // NT
        def front(blk):
                b, tb_ = divmod(blk, S // NT)
                t0 = tb_ * NT
                par = blk % 2
                h2 = h2s[par]; qT = qTs[par]; scs = scss[par]
                Gs = Gsb[blk % 2]
                gsk = 'Gs%d' % (blk % 2)
                P.dma('sp', h2[:], h2T_d[b, :, :, t0:t0 + NT], 'h2_%d' % par, w=['h2_%d' % par])
                for m in range(16):
                    pb_ = 4 + (m % 2)

                    def qmm(e, m=m, pb_=pb_):
                        for kc in range(8):
                            ins = e.matmul(Bk[pb_][:, 0:NT], lhsT=wqb[:, kc, m * 128:(m + 1) * 128], rhs=h2[:, kc, :], start=(kc == 0), stop=(kc == 7))
                        return ins
                    P.op('pe', qmm, r=['wqb', 'h2_%d' % par], w=[bk(pb_)])
                    P.op('act', lambda e, m=m, pb_=pb_: e.copy(out=qT[:, m, :], in_=Bk[pb_][:, 0:NT]), r=[bk(pb_)], w=['qT_%d' % par])
                for m4 in range(4):
                    def smm2(e, m4=m4):
                        for mm in range(4):
                            m = m4 * 4 + mm
                            ins = e.matmul(Bk[m4][:, mm * 128:(mm + 1) * 128], lhsT=qT[:, m, :], rhs=keysb[:, m, :], start=True, stop=True)
                        return ins
                    P.op('pe', smm2, r=['qT_%d' % par, 'keysb'], w=[bk(m4)])
                    P.op('act', lambda e, m4=m4: e.copy(out=scs[:, m4 * 4:(m4 + 1) * 4, :], in_=Bk[m4][:, :]), r=[bk(m4)], w=['scs_%d' % par])

        def mid_a(blk):
                b, tb_ = divmod(blk, S // NT)
                t0 = tb_ * NT
                par = blk % 2
                h2 = h2s[par]; qT = qTs[par]; scs = scss[par]
                for m in range(16):
                    P.op('dve', lambda e, m=m: e.max(out=v16[:, m, 0:8], in_=scs[:, m, :]), r=['scs_%d' % par], w=['v16'])
                    P.op('dve', lambda e, m=m: e.max_index(out=ix16[:, m, 0:8], in_max=v16[:, m, 0:8], in_values=scs[:, m, :]), r=['scs_%d' % par, 'v16'], w=['ix16'])
                    P.op('dve', lambda e, m=m: e.match_replace(out=scr[:, 0:128], in_to_replace=v16[:, m, 0:8], in_values=scs[:, m, :], imm_value=-1e30),
                         r=['scs_%d' % par, 'v16'], w=['scr'])
                    P.op('dve', lambda e, m=m: e.max(out=v16[:, m, 8:16], in_=scr[:, 0:128]), r=['scr'], w=['v16'])
                    P.op('dve', lambda e, m=m: e.max_index(out=ix16[:, m, 8:16], in_max=v16[:, m, 8:16], in_values=scr[:, 0:128]), r=['scr', 'v16'], w=['ix16'])
                P.op('dve', lambda e: e.tensor_copy(out=ixf[:], in_=ix16[:]), r=['ix16'], w=['ixf'])
                P.op('dve', lambda e: e.tensor_tensor(out=AP(cand, 0, [[2048, 128], [256, 8], [16, 16], [1, 16]]),
                                                      in0=AP(v16, 0, [[256, 128], [32, 8], [1, 16], [0, 16]]),
                                                      in1=AP(v16, 16, [[256, 128], [32, 8], [0, 16], [1, 16]]), op=ALU.add), r=['v16'], w=['cand'])
                for h in range(8):
                    P.op('dve', lambda e, h=h: e.max(out=tv[:, h, 0:8], in_=cand[:, h, :]), r=['cand'], w=['tv'])
                    P.op('dve', lambda e, h=h: e.max_index(out=pos[:, h, 0:8], in_max=tv[:, h, 0:8], in_values=cand[:, h, :]), r=['cand', 'tv'], w=['pos'])
                    P.op('dve', lambda e, h=h: e.match_replace(out=scr[:, 0:256], in_to_replace=tv[:, h, 0:8], in_values=cand[:, h, :], imm_value=-1e30),
                         r=['cand', 'tv'], w=['scr'])
                    P.op('dve', lambda e, h=h: e.max(out=tv[:, h, 8:16], in_=scr[:, 0:256]), r=['scr'], w=['tv'])
                    P.op('dve', lambda e, h=h: e.max_index(out=pos[:, h, 8:16], in_max=tv[:, h, 8:16], in_values=scr[:, 0:256]), r=['scr', 'tv'], w=['pos'])

        def gate_(blk):
                b, tb_ = divmod(blk, S // NT)
                t0 = tb_ * NT
                par = blk % 2
                h2 = h2s[par]; qT = qTs[par]; scs = scss[par]
                P.op('dve', lambda e: e.tensor_tensor(out=tve[:], in0=tv[:], in1=AP(tv, 0, [[128, 128], [16, 8], [0, 16]]), op=ALU.subtract), r=['tv'], w=['tve'])
                P.op('act', lambda e: e.activation(out=tve[:], in_=tve[:], func=AF.Exp), r=['tve'], w=['tve'])

        def mid_b(blk):
                b, tb_ = divmod(blk, S // NT)
                t0 = tb_ * NT
                par = blk % 2
                h2 = h2s[par]; qT = qTs[par]; scs = scss[par]
                P.op('dve', lambda e: e.tensor_copy(out=posf[:], in_=pos[:]), r=['pos'], w=['posf'])
                posb = AP(posf, 0, [[128, 128], [16, 8], [1, 16], [0, 16]])
                P.op('dve', lambda e, posb=posb: e.tensor_tensor(out=eq[:], in0=posb, in1=AP(i16a, 0, [[16, 128], [0, 8], [0, 16], [1, 16]]), op=ALU.is_ge),
                     r=['posf', 'i16a'], w=['eq'])
                P.op('dve', lambda e, posb=posb: e.tensor_tensor(out=eq2[:].rearrange("p h (a b) -> p h a b", b=16) if False else AP(cand, 0, [[2048, 128], [256, 8], [16, 16], [1, 16]]),
                                                                in0=posb, in1=AP(i16b, 0, [[16, 128], [0, 8], [0, 16], [1, 16]]), op=ALU.is_ge),
                     r=['posf', 'i16b', 'pos'], w=['cand'])
                P.op('dve', lambda e: e.tensor_tensor(out=eq[:], in0=eq[:], in1=AP(cand, 0, [[2048, 128], [256, 8], [16, 16], [1, 16]]), op=ALU.subtract), r=['eq', 'cand'], w=['eq'])
                c4 = AP(cand, 0, [[2048, 128], [256, 8], [16, 16], [1, 16]])
                c3 = AP(cand, 0, [[2048, 128], [16, 128], [1, 16]])
                P.op('dve', lambda e, c4=c4: e.tensor_tensor(out=c4, in0=eq[:], in1=AP(ixf, 0, [[256, 128], [32, 8], [0, 16], [1, 16]]), op=ALU.mult), r=['eq', 'ixf'], w=['cand'])
                P.op('dve', lambda e, c3=c3: e.tensor_reduce(out=sel[:, 0, :], in_=c3, axis=AX.X, op=ALU.add), r=['cand'], w=['sel'])
                P.op('dve', lambda e, c4=c4: e.tensor_tensor(out=c4, in0=eq[:], in1=AP(iota16, 0, [[16, 128], [0, 8], [0, 16], [1, 16]]), op=ALU.mult), r=['eq', 'iota16'], w=['cand'])
                P.op('dve', lambda e, c3=c3: e.tensor_reduce(out=paf[:], in_=c3, axis=AX.X, op=ALU.add), r=['cand'], w=['paf'])
                P.op('dve', lambda e: e.scalar_tensor_tensor(out=pbf[:], in0=paf[:], scalar=-16.0, in1=posf[:], op0=ALU.mult, op1=ALU.add), r=['paf', 'posf'], w=['pbf'])
                P.op('dve', lambda e: e.tensor_tensor(out=eq[:], in0=AP(iota16, 0, [[16, 128], [0, 8], [0, 16], [1, 16]]),
                                                      in1=AP(pbf, 0, [[128, 128], [16, 8], [1, 16], [0, 16]]), op=ALU.is_equal), r=['iota16', 'pbf'], w=['eq'])
                P.op('dve', lambda e, c4=c4: e.tensor_tensor(out=c4, in0=eq[:], in1=AP(ixf, 16, [[256, 128], [32, 8], [0, 16], [1, 16]]), op=ALU.mult), r=['eq', 'ixf'], w=['cand'])
                P.op('dve', lambda e, c3=c3: e.tensor_reduce(out=sel[:, 1, :], in_=c3, axis=AX.X, op=ALU.add), r=['cand'], w=['sel'])
                P.op('dve', lambda e: e.tensor_reduce(out=zsum[:], in_=tve[:], axis=AX.X, op=ALU.add), r=['tve'], w=['zsum'])
                P.op('dve', lambda e: e.reciprocal(out=zsum[:], in_=zsum[:]), r=['zsum'], w=['zsum'])
                P.op('dve', lambda e: e.tensor_tensor(out=AP(sel, 256, [[384, 128], [16, 8], [1, 16]]), in0=tve[:], in1=AP(zsum, 0, [[8, 128], [1, 8], [0, 16]]), op=ALU.mult),
                     r=['tve', 'zsum'], w=['sel'])
                P.op('dve', lambda e: e.tensor_copy(out=selb[:], in_=sel[:]), r=['sel'], w=['selb'])

        def tail(blk):
                b, tb_ = divmod(blk, S // NT)
                t0 = tb_ * NT
                par = blk % 2
                h2 = h2s[par]; qT = qTs[par]; scs = scss[par]
                Gs = Gsb[blk % 2]
                gsk = 'Gs%d' % (blk % 2)

                def trs(e):
                    for c3_ in range(3):
                        ins = e.transpose(out=AP(Bk7b, c3_ * 128, [[1024, 128], [1, 128]]), in_=selb[:, c3_, :], identity=identb[:])
                    return ins
                P.op('pe', trs, r=['selb', 'identb'], w=['Bk7b'])
                P.op('act', lambda e: e.copy(out=selT[:], in_=AP(Bk7b, 0, [[1024, 128], [128, 3], [1, 128]])), r=['Bk7b'], w=['selT'])
                P.op('act', lambda e: e.activation(out=selTn[:], in_=AP(Bk7b, 128, [[1024, 128], [1, 128]]), func=AF.Copy, scale=-1.0), r=['Bk7b'], w=['selTn'])
                for tg in range(4):
                    io_b = AP(iob, 0, [[128, 128], [0, 32], [1, 128]])
                    OJ = OJs[tg % 2]; OI = OIs[tg % 2]; ojk = 'OJ%d' % (tg % 2); oik = 'OI%d' % (tg % 2)
                    for tl in range(32):
                        P.op('act', lambda e, tl=tl, tg=tg, OJ=OJ: e.activation(out=OJ[:, tl, :], in_=iota128[:], func=AF.Abs,
                                                                              bias=selTn[:, tg * 32 + tl:tg * 32 + tl + 1]),
                             r=['iota128', 'selTn'], w=[ojk])
                    P.op('act', lambda e, OJ=OJ: e.activation(out=OJ[:], in_=OJ[:], func=AF.Relu, scale=-1.0, bias=1.0), r=[ojk], w=[ojk])
                    P.op('dve', lambda e, io_b=io_b, tg=tg, OI=OI: e.tensor_tensor(out=OI[:], in0=io_b, in1=AP(selT, tg * 32, [[384, 128], [1, 32], [0, 128]]), op=ALU.is_equal),
                         r=['iob', 'selT'], w=[oik])
                    P.op('pool', lambda e, tg=tg, OI=OI: e.tensor_tensor(out=OI[:], in0=OI[:], in1=AP(selT, 256 + tg * 32, [[384, 128], [1, 32], [0, 128]]), op=ALU.mult),
                         r=[oik, 'selT'], w=[oik])
                    for t4 in range(8):
                        pbk = (6, 4, 5, 0, 1, 2, 3)[(tg * 8 + t4) % 7]

                        def gmm(e, t4=t4, pbk=pbk, OJ=OJ, OI=OI):
                            for tq in range(4):
                                tl = t4 * 4 + tq
                                ins = e.matmul(AP(Bk[pbk], tq, [[512, 128], [4, 128]]), lhsT=OJ[:, tl, :], rhs=OI[:, tl, :], start=True, stop=True)
                            return ins
                        P.op('pe', gmm, r=[ojk, oik], w=[bk(pbk)])
                        P.op('act', lambda e, t4=t4, pbk=pbk, tg=tg, Gs=Gs: e.copy(out=AP(Gs, tg * 32 + t4 * 4, [[128 * NT, 128], [NT, 128], [1, 4]]),
                                                                           in_=AP(Bk[pbk], 0, [[512, 128], [4, 128], [1, 4]])), r=[bk(pbk)], w=[gsk])
                P.dma('sp', G_d[blk], Gs[:], 'gdo%d' % (blk % 2), r=[gsk], w=[gsk, 'Gd%d' % blk])

        nb1 = nblk if not quick else 1
        front(0)
        for blk in range(nb1):
            mid_a(blk)
            gate_(blk)
            if blk + 1 < nb1:
                front(blk + 1)
            mid_b(blk)
            tail(blk)
        P.barrier()

    with ExitStack() as st:
        def SB(name, shape, dt):
            return st.enter_context(nc.sbuf_tensor(name, list(shape), dt))

        def PS(name, shape, dt=F32):
            return st.enter_context(nc.psum_tensor(name, list(shape), dt))
        NTM = 384
        h2e = SB("h2e", [128, 8, NTM], BF16)
        utb = [SB("utb%d" % i, [128, 8, 128], BF16) for i in range(8)]
        vtb = [SB("vtb%d" % i, [128, 1024], BF16) for i in range(8)]
        gq = [SB("gq%d" % i, [128, 3, 4, 128], BF16) for i in range(2)]
        glb = [SB("glb%d" % i, [128, NTM], BF16) for i in range(2)]
        actb = [SB("actb%d" % i, [128, NTM], BF16) for i in range(2)]
        x1b = SB("x1b", [128, D], F32)
        o2 = SB("o2", [128, D], F32)
        sqe = SB("sqe", [128, D], F32)
        sse = SB("sse", [128, 4], F32)
        Bo = [PS("Bo%d" % i, [128, 512], F32) for i in range(6)]
        Bp = [PS("Bp%d" % i, [128, 512], F32) for i in range(2)]
        blocks = []
        for b in range(NB):
            t = 0
            for nt in [384] * 10 + [256]:
                blocks.append((b, t, nt))
                t += nt
        for (b, t0, nt) in (blocks if not quick else blocks[:1]):
            nsub = nt // 128
            tb0 = (b * S + t0) // 128
            P.dma('sp', h2e[:, :, 0:nt], h2T_d[b, :, :, t0:t0 + nt], 'h2e', w=['h2e'])
            def emit_pre(ci):
                sl = ci % 8
                P.dma('sp', utb[sl][:], UTb_d[ci], 'utb%d' % sl, w=['utb%d' % sl])
                P.dma('pool', vtb[sl][:], Vb_d[ci], 'vtb%d' % sl, w=['vtb%d' % sl])
                gsl = (ci // 4) % 2
                if ci % 4 == 0:
                    for sub in range(nsub):
                        P.dma('sp', gq[gsl][:, sub, :, :], G_d[tb0 + sub, :, ci:ci + 4, :], 'gq%d' % gsl, r=['Gd%d' % (tb0 + sub)], w=['gq%d' % gsl])
                pp = ci % 2

                def pmm(e, sl=sl, pp=pp, nt=nt):
                    for kc in range(8):
                        ins = e.matmul(Bp[pp][:, 0:nt], lhsT=utb[sl][:, kc, :], rhs=h2e[:, kc, 0:nt], start=(kc == 0), stop=(kc == 7))
                    return ins
                P.op('pe', pmm, r=['utb%d' % sl, 'h2e'], w=['Bp%d' % pp])
                P.op('act', lambda e, pp=pp, nt=nt: e.activation(out=glb[pp][:, 0:nt], in_=Bp[pp][:, 0:nt], func=AF.Gelu), r=['Bp%d' % pp], w=['glb%d' % pp])
                P.op('dve', lambda e, pp=pp, nt=nt, nsub=nsub, gsl=gsl, ci=ci: e.tensor_tensor(
                    out=AP(actb[pp], 0, [[NTM, 128], [128, nsub], [1, 128]]), in0=AP(glb[pp], 0, [[NTM, 128], [128, nsub], [1, 128]]),
                    in1=AP(gq[gsl], (ci % 4) * 128, [[1536, 128], [512, nsub], [1, 128]]), op=ALU.mult),
                    r=['glb%d' % pp, 'gq%d' % gsl], w=['actb%d' % pp])

            def emit_out(ci):
                sl = ci % 8
                pp = ci % 2
                def omm(e, sl=sl, pp=pp, nsub=nsub, ci=ci):
                    for sub in range(nsub):
                        for hf in range(2):
                            ins = e.matmul(Bo[sub * 2 + hf][:, :], lhsT=actb[pp][:, sub * 128:(sub + 1) * 128], rhs=vtb[sl][:, hf * 512:(hf + 1) * 512],
                                           start=(ci == 0), stop=(ci == 127))
                    return ins
                P.op('pe', omm, r=['actb%d' % pp, 'vtb%d' % sl], w=['Bo'])
            emit_pre(0)
            for ci in range(128):
                if ci + 1 < 128:
                    emit_pre(ci + 1)
                emit_out(ci)
            for sub in range(nsub):
                tt0 = t0 + sub * 128
                P.dma('sp', x1b[:], x1_d[b, tt0:tt0 + 128, :], 'x1b', w=['x1b'])
                for hf in range(2):
                    P.op('dve', lambda e, sub=sub, hf=hf: e.tensor_tensor(out=o2[:, hf * 512:(hf + 1) * 512], in0=Bo[sub * 2 + hf][:, :], in1=x1b[:, hf * 512:(hf + 1) * 512], op=ALU.add),
                         r=['Bo', 'x1b'], w=['o2_%d' % hf])
                P.op('act', lambda e: e.activation(out=sqe[:], in_=o2[:], func=AF.Square, accum_out=sse[:, 0:1]), r=['o2_0', 'o2_1'], w=['sqe', 'ssea'])
                P.op('act', lambda e: e.activation(out=sse[:, 1:2], in_=sse[:, 0:1], func=AF.Sqrt, scale=1.0 / D, bias=epsc[:, 0:1]), r=['ssea', 'epsc'], w=['sseb'])
                P.op('dve', lambda e: e.reciprocal(out=sse[:, 2:3], in_=sse[:, 1:2]), r=['sseb'], w=['ssec'])
                P.op('dve', lambda e: e.scalar_tensor_tensor(out=sqe[:], in0=o2[:], scalar=sse[:, 2:3], in1=gfin_s[:], op0=ALU.mult, op1=ALU.mult),
                     r=['o2_0', 'o2_1', 'ssec', 'gfin', 'sqe'], w=['sqe'])
                P.dma('sp', y[b, tt0:tt0 + 128, :], sqe[:], 'yo', r=['sqe'], w=['yo'])
        P.barrier()
    return nc, P, {}


def host_inputs(inp):
    f = np.float32

    def kmaj(w):
        K, N = w.shape
        return np.ascontiguousarray(w.reshape(K // 128, 128, N).transpose(1, 0, 2)).astype(f)
    com = {}
    com["w1"] = kmaj(inp["w_in"][0])
    com["g1"] = np.ascontiguousarray(inp["norm1_g"][0].reshape(8, 128).T).astype(f)
    com["wout"] = kmaj(inp["w_out"][0])
    com["wglu"] = kmaj(inp["w_glu"][0])
    com["wq"] = kmaj(inp["w_query"][0])
    com["g2"] = np.ascontiguousarray(inp["norm2_g"][0].reshape(8, 128).T).astype(f)
    com["gfin"] = np.ascontiguousarray(np.broadcast_to(inp["final_g"][None, :], (128, D))).astype(f)
    com["wf"] = np.ascontiguousarray(inp["w_fourier"][0].transpose(1, 0, 2)).astype(f)
    c = np.arange(128)
    ang = 2 * np.pi * np.outer(c, c) / 128.0
    sc = 1.0 / math.sqrt(S * 128)
    com["ccsc"] = np.stack([np.cos(ang) * sc, -np.sin(ang) * sc], axis=1).astype(f)
    com["ident"] = np.eye(128, dtype=f)
    s_idx = np.arange(S)
    ks = (np.outer(s_idx, s_idx) % S).astype(np.float64) * (2 * np.pi / S)
    ct = np.cos(ks).astype(f).astype(ml_dtypes.bfloat16)
    stt = np.sin(ks).astype(f).astype(ml_dtypes.bfloat16)
    t = np.stack([ct, stt], axis=0)
    t = t.reshape(2, 32, 128, 16, 256).transpose(3, 2, 0, 1, 4)
    com["tab"] = np.ascontiguousarray(t)
    com["iota128"] = np.ascontiguousarray(np.broadcast_to(np.arange(128, dtype=f)[None, :], (128, 128)))
    com["iota16"] = np.ascontiguousarray(np.broadcast_to(np.arange(16, dtype=f)[None, :], (128, 16)))

    def lamA(arr):
        return np.ascontiguousarray(arr.reshape(2, 4, 4, 2, 64).transpose(3, 4, 1, 2, 0).reshape(128, 32)).astype(f)
    com["lam_are"] = lamA(inp["ssm_a_re"][0])
    com["lam_aim"] = lamA(inp["ssm_a_im"][0])
    com["lam_lst"] = lamA(np.broadcast_to(inp["ssm_log_step"][0][:, :, None], (2, 32, 64)))
    bA = np.zeros((2, 4, 128, 8, 128), f)
    cA = np.zeros((2, 4, 128, 8, 128), f)
    for ri, (bsrc, csrc) in enumerate(((inp["ssm_b_re"][0], inp["ssm_c_re"][0]), (inp["ssm_b_im"][0], inp["ssm_c_im"][0]))):
        for T in range(4):
            for q in range(4):
                for gp in range(2):
                    g = 8 * T + 2 * q + gp
                    for dr in range(2):
                        col = (2 * q + gp) * 16
                        bA[ri, T, gp * 64:(gp + 1) * 64, q * 2 + dr, col:col + 16] = bsrc[dr, g]
                        cA[ri, T, gp * 64:(gp + 1) * 64, q * 2 + dr, col:col + 16] = csrc[dr, g].T
    com["bA"] = bA
    com["cA"] = cA
    com["dD"] = np.ascontiguousarray(inp["ssm_d"][0].reshape(4, 128).T).astype(f)
    eu = inp["expert_u"][0]
    com["uT_in"] = np.ascontiguousarray(eu.reshape(16384, 8, 128).transpose(2, 1, 0)).astype(f)
    com["v_in"] = np.ascontiguousarray(inp["expert_v"][0].reshape(128, 128, 1024)).astype(f)
    sk = inp["sub_keys"][0]
    com["keysT_in"] = np.ascontiguousarray(sk.reshape(16, 128, 128).transpose(2, 0, 1)).astype(f)
    return com


_CACHE = {}


def kernel(**inp):
    if "nc" not in _CACHE:
        _CACHE["nc"] = build()[0]
    nc = _CACHE["nc"]
    com = host_inputs(inp)
    xs = np.ascontiguousarray(inp["x"]).astype(np.float32)
    in_maps = []
    for c in range(8):
        m = dict(com)
        m["x"] = xs[2 * c:2 * c + 2]
        in_maps.append(m)
    res = run_bass_kernel_spmd(nc, in_maps, core_ids=list(range(8)))
    return np.concatenate([np.asarray(r["y"]) for r in res.results], axis=0).astype(np.float32)
```
